# Optimizing a Trainium2 kernel written in Bass

```python
import math
import jax, jax.numpy as jnp
from jax import lax
import numpy as np

D_MODEL = 2048
BATCH = 2
SEQ = 4096
DEPTH = 4

GRID_W = 64
CTX_LEN = 256
D_MIX = D_MODEL
H_NA = 8
DH_NA = D_MIX // 2 // H_NA
WIN_R = 8
WIN_C = 16
H_M = 4
DV_M = D_MIX // 2 // H_M
DK_M = DV_M // 2
CHUNK = 128
ROPE_BASE = 10000.0
N_EXPERTS = 8
TOP_K = 2
D_FF = ((8 * D_MODEL // 3 + 255) // 256) * 256
N_DENSE = (DEPTH + 1) // 2
N_MOE = DEPTH // 2
EPS = 1e-6
PROJ_SIZES = [H_NA * DH_NA] * 3 + [H_M * DK_M] * 2 + [H_M * DV_M] * 2 + [4 * H_M]
N_IN = sum(PROJ_SIZES)
PROJ_OFFSETS = [sum(PROJ_SIZES[: i + 1]) for i in range(len(PROJ_SIZES) - 1)]

kernel_name = "hybrid_mlstm_natten_moe_dit"


def rms_norm(t, g):
    tf = t.astype(jnp.float32)
    y = tf * lax.rsqrt(jnp.mean(tf * tf, axis=-1, keepdims=True) + EPS)
    return (y * g.astype(jnp.float32)).astype(t.dtype)


def modulate(h, shift, scale):
    return h * (1 + scale) + shift


def heads(t, n_heads):
    b, l, _ = t.shape
    return t.reshape(b, l, n_heads, -1).transpose(0, 2, 1, 3)


def merge_heads(t):
    b, h, l, d = t.shape
    return t.transpose(0, 2, 1, 3).reshape(b, l, h * d)


def flip(t):
    return jnp.flip(t, axis=2)


def axial_rope(n_tok, dim):
    t = jnp.arange(n_tok)
    row = (t // GRID_W).astype(jnp.float32)
    col = (t % GRID_W).astype(jnp.float32)
    quarter = dim // 4
    inv = ROPE_BASE ** (-jnp.arange(quarter, dtype=jnp.float32) / quarter)
    ang = jnp.concatenate([row[:, None] * inv, col[:, None] * inv], axis=-1)
    return jnp.cos(ang), jnp.sin(ang)


def apply_rope(t, cos, sin):
    te, to = t[..., 0::2], t[..., 1::2]
    cos = cos.astype(t.dtype)
    sin = sin.astype(t.dtype)
    return jnp.stack([te * cos - to * sin, te * sin + to * cos], axis=-1).reshape(t.shape)


def mlstm_scan(q, k, v, logi, logf, state, with_h=True):
    b_, h_, l_, _ = q.shape
    nc = l_ // CHUNK

    def chunks(a):
        a = a.reshape(b_, h_, nc, CHUNK, *a.shape[3:])
        return jnp.moveaxis(a, 2, 0)

    causal = jnp.tril(jnp.ones((CHUNK, CHUNK), dtype=bool))

    def step(carry, xs):
        C, n, m = carry
        qb, kb, vb, ib, fb = xs
        bcum = jnp.cumsum(fb, axis=-1)
        b_last = bcum[..., -1]
        d_end = b_last[..., None] - bcum + ib
        m_new = jnp.maximum(b_last + m, jnp.max(d_end, axis=-1))
        w_end = jnp.exp(d_end - m_new[..., None])
        decay = jnp.exp(b_last + m - m_new)
        C_new = decay[..., None, None] * C + jnp.einsum("bhs,bhsd,bhsv->bhdv", w_end, kb, vb)
        n_new = decay[..., None] * n + jnp.einsum("bhs,bhsd->bhd", w_end, kb)
        if not with_h:
            return (C_new, n_new, m_new), None
        d_mat = jnp.where(causal, bcum[..., :, None] - bcum[..., None, :] + ib[..., None, :], -jnp.inf)
        m_in = bcum + m[..., None]
        m_t = jnp.maximum(m_in, jnp.max(d_mat, axis=-1))
        s = jnp.einsum("bhtd,bhsd->bhts", qb, kb) * jnp.exp(d_mat - m_t[..., None])
        a_in = jnp.exp(m_in - m_t)
        num = jnp.einsum("bhts,bhsv->bhtv", s, vb) + a_in[..., None] * jnp.einsum("bhtd,bhdv->bhtv", qb, C)
        den = jnp.sum(s, axis=-1) + a_in * jnp.einsum("bhtd,bhd->bht", qb, n)
        h = num / jnp.maximum(jnp.abs(den), jnp.exp(-m_t))[..., None]
        return (C_new, n_new, m_new), h

    state, h = lax.scan(step, state, tuple(chunks(a) for a in (q, k, v, logi, logf)))
    if not with_h:
        return None, state
    h = jnp.moveaxis(h, 0, 2).reshape(b_, h_, l_, -1)
    return h, state


def mlstm_inputs(p, gate_b, rope):
    q = heads(p[3], H_M).astype(jnp.float32)
    k = heads(p[4], H_M).astype(jnp.float32)
    if rope is not None:
        q = apply_rope(q, rope[0], rope[1])
        k = apply_rope(k, rope[0], rope[1])
    k = k * (DK_M ** -0.5)
    v = heads(p[5], H_M).astype(jnp.float32)
    b, l, _ = p[7].shape
    g = (p[7] + gate_b).astype(jnp.float32).reshape(b, l, 4, H_M).transpose(2, 0, 3, 1)
    gates = (g[0], jax.nn.log_sigmoid(g[1]), g[2], jax.nn.log_sigmoid(g[3]))
    return q, k, v, p[6], gates


def mlstm_output(h, o, m_norm_g):
    h = rms_norm(h, m_norm_g.reshape(H_M, 1, DV_M))
    return merge_heads(h).astype(o.dtype) * jax.nn.sigmoid(o)


def zero_state(b):
    return (jnp.zeros((b, H_M, DK_M, DV_M), jnp.float32),
            jnp.zeros((b, H_M, DK_M), jnp.float32),
            jnp.zeros((b, H_M), jnp.float32))


def neighbourhood_attention(q, k, v, kc, vc, rpb, rows):
    b, h, l, dh = q.shape
    wr = min(WIN_R, rows)
    scale = dh ** -0.5
    r_idx = jnp.arange(rows)
    row_start = jnp.clip(r_idx - wr // 2, 0, rows - wr)
    band_rows = row_start[:, None] + jnp.arange(wr)[None, :]
    q_g = q.reshape(b, h, rows, GRID_W, dh)
    k_band = k.reshape(b, h, rows, GRID_W, dh)[:, :, band_rows].reshape(b, h, rows, wr * GRID_W, dh)
    v_band = v.reshape(b, h, rows, GRID_W, dh)[:, :, band_rows].reshape(b, h, rows, wr * GRID_W, dh)
    c_idx = jnp.arange(GRID_W)
    col_start = jnp.clip(c_idx - WIN_C // 2, 0, GRID_W - WIN_C)
    col_ok = (c_idx[None, :] >= col_start[:, None]) & (c_idx[None, :] < col_start[:, None] + WIN_C)
    mask = jnp.broadcast_to(col_ok[:, None, :], (GRID_W, wr, GRID_W)).reshape(GRID_W, wr * GRID_W)
    dr = band_rows - r_idx[:, None] + (WIN_R - 1)
    dc = jnp.clip(c_idx[None, :] - c_idx[:, None], -(WIN_C - 1), WIN_C - 1) + (WIN_C - 1)
    bias = rpb[:, dr[:, None, :, None], dc[None, :, None, :]]
    bias = bias.reshape(h, rows, GRID_W, wr * GRID_W).astype(jnp.float32)
    s_band = jnp.einsum("bhrqd,bhrkd->bhrqk", q_g, k_band).astype(jnp.float32) * scale + bias[None]
    s_band = jnp.where(mask, s_band, -jnp.inf)
    s_ctx = jnp.einsum("bhrqd,bhcd->bhrqc", q_g, kc).astype(jnp.float32) * scale
    p = jax.nn.softmax(jnp.concatenate([s_band, s_ctx], axis=-1), axis=-1).astype(v.dtype)
    p_band, p_ctx = p[..., : wr * GRID_W], p[..., wr * GRID_W:]
    out = jnp.einsum("bhrqk,bhrkd->bhrqd", p_band, v_band) + jnp.einsum("bhrqc,bhcd->bhrqd", p_ctx, vc)
    return merge_heads(out.reshape(b, h, l, dh))


def context_attention(q, k, v):
    s = jnp.einsum("bhqd,bhkd->bhqk", q, k).astype(jnp.float32) * (q.shape[-1] ** -0.5)
    p = jax.nn.softmax(s, axis=-1).astype(v.dtype)
    return merge_heads(jnp.einsum("bhqk,bhkd->bhqd", p, v))


def token_mixer(hx, hc, rope, w_in, w_out, gate_b, na_q_g, na_k_g, na_rpb, m_norm_g, with_ctx_out):
    b, l, _ = hx.shape
    rows = l // GRID_W
    px = jnp.split(hx @ w_in, PROJ_OFFSETS, axis=-1)
    pc = jnp.split(hc @ w_in, PROJ_OFFSETS, axis=-1)
    qx = rms_norm(heads(px[0], H_NA), na_q_g)
    kx = rms_norm(heads(px[1], H_NA), na_k_g)
    vx = heads(px[2], H_NA)
    kc = rms_norm(heads(pc[1], H_NA), na_k_g)
    vc = heads(pc[2], H_NA)
    na_x = neighbourhood_attention(qx, kx, vx, kc, vc, na_rpb, rows)
    qmc, kmc, vmc, omc, gc = mlstm_inputs(pc, gate_b, None)
    qmx, kmx, vmx, omx, gx = mlstm_inputs(px, gate_b, rope)
    z = zero_state(b)
    hcf, st_f = mlstm_scan(qmc, kmc, vmc, gc[0], gc[1], z, with_h=with_ctx_out)
    hcb, st_b = mlstm_scan(flip(qmc), flip(kmc), flip(vmc), flip(gc[2]), flip(gc[3]), z, with_h=with_ctx_out)
    hxf, _ = mlstm_scan(qmx, kmx, vmx, gx[0], gx[1], st_f)
    hxb, _ = mlstm_scan(flip(qmx), flip(kmx), flip(vmx), flip(gx[2]), flip(gx[3]), st_b)
    m_x = mlstm_output(hxf + flip(hxb), omx, m_norm_g)
    out_x = jnp.concatenate([na_x, m_x], axis=-1) @ w_out
    if not with_ctx_out:
        return out_x, None
    qc = rms_norm(heads(pc[0], H_NA), na_q_g)
    na_c = context_attention(qc, kc, vc)
    m_c = mlstm_output(hcf + flip(hcb), omc, m_norm_g)
    out_c = jnp.concatenate([na_c, m_c], axis=-1) @ w_out
    return out_x, out_c


def swiglu(h, wg, wu, wd):
    return (jax.nn.silu(h @ wg) * (h @ wu)) @ wd


def moe_swiglu(h, router, wg, wu, wd):
    logits = (h @ router).astype(jnp.float32)
    top_val, top_idx = lax.top_k(logits, TOP_K)
    top_w = jax.nn.softmax(top_val, axis=-1)
    gates = jnp.sum(jax.nn.one_hot(top_idx, N_EXPERTS, dtype=jnp.float32) * top_w[..., None], axis=-2)
    out = jnp.zeros_like(h)
    for e in range(N_EXPERTS):
        out = out + gates[..., e:e + 1].astype(h.dtype) * swiglu(h, wg[e], wu[e], wd[e])
    return out


def setup_inputs(seed: int = 0) -> dict:
    key = jax.random.key(seed)
    ks = jax.random.split(key, 24)
    f32 = jnp.float32
    nrm = lambda k, shape, s: jax.random.normal(k, shape, f32) * s
    d = D_MODEL
    gate_b = jnp.concatenate([
        nrm(ks[9], (DEPTH, H_M), 0.1),
        3.0 + 3.0 * jax.random.uniform(ks[10], (DEPTH, H_M), f32),
        nrm(ks[11], (DEPTH, H_M), 0.1),
        3.0 + 3.0 * jax.random.uniform(ks[12], (DEPTH, H_M), f32),
    ], axis=-1)
    return {
        "x": nrm(ks[0], (BATCH, SEQ, d), 1.0),
        "c": nrm(ks[1], (BATCH, d), 1.0),
        "ctx": nrm(ks[2], (BATCH, CTX_LEN, d), 1.0),
        "c_ctx": nrm(ks[3], (d,), 1.0),
        "ada_w": nrm(ks[4], (DEPTH, d, 6 * d), 0.5 * d ** -0.5),
        "ada_b": nrm(ks[5], (DEPTH, 6 * d), 0.02),
        "norm1_g": 1.0 + nrm(ks[6], (DEPTH, d), 0.1),
        "norm2_g": 1.0 + nrm(ks[7], (DEPTH, d), 0.1),
        "w_in": nrm(ks[8], (DEPTH, d, N_IN), d ** -0.5),
        "gate_b": gate_b,
        "na_q_g": 1.0 + nrm(ks[13], (DEPTH, DH_NA), 0.1),
        "na_k_g": 1.0 + nrm(ks[14], (DEPTH, DH_NA), 0.1),
        "na_rpb": nrm(ks[15], (DEPTH, H_NA, 2 * WIN_R - 1, 2 * WIN_C - 1), 0.1),
        "m_norm_g": 1.0 + nrm(ks[16], (DEPTH, H_M * DV_M), 0.1),
        "w_out": nrm(ks[17], (DEPTH, D_MIX, d), D_MIX ** -0.5),
        "ffn_w_gate": nrm(ks[18], (N_DENSE, d, D_FF), d ** -0.5),
        "ffn_w_up": nrm(ks[19], (N_DENSE, d, D_FF), d ** -0.5),
        "ffn_w_down": nrm(ks[20], (N_DENSE, D_FF, d), D_FF ** -0.5),
        "moe_router": nrm(ks[21], (N_MOE, d, N_EXPERTS), d ** -0.5),
        "moe_w_gate": nrm(ks[22], (N_MOE, N_EXPERTS, d, D_FF), d ** -0.5),
        "moe_w_up": nrm(jax.random.fold_in(ks[22], 1), (N_MOE, N_EXPERTS, d, D_FF), d ** -0.5),
        "moe_w_down": nrm(ks[23], (N_MOE, N_EXPERTS, D_FF, d), D_FF ** -0.5),
    }


def reference(x, c, ctx, c_ctx, ada_w, ada_b, norm1_g, norm2_g, w_in, gate_b, na_q_g, na_k_g,
              na_rpb, m_norm_g, w_out, ffn_w_gate, ffn_w_up, ffn_w_down, moe_router,
              moe_w_gate, moe_w_up, moe_w_down):
    L = x.shape[1]
    rope = axial_rope(L, DK_M)
    cond_x = jax.nn.silu(c)
    cond_c = jax.nn.silu(c_ctx)[None]
    for layer in range(DEPTH):
        last = layer == DEPTH - 1
        mod_x = jnp.split((cond_x @ ada_w[layer] + ada_b[layer])[:, None, :], 6, axis=-1)
        mod_c = jnp.split((cond_c @ ada_w[layer] + ada_b[layer])[:, None, :], 6, axis=-1)
        hx = modulate(rms_norm(x, norm1_g[layer]), mod_x[0], mod_x[1])
        hc = modulate(rms_norm(ctx, norm1_g[layer]), mod_c[0], mod_c[1])
        mix_x, mix_c = token_mixer(hx, hc, rope, w_in[layer], w_out[layer], gate_b[layer],
                                   na_q_g[layer], na_k_g[layer], na_rpb[layer], m_norm_g[layer],
                                   with_ctx_out=not last)
        x = x + mod_x[2] * mix_x
        if not last:
            ctx = ctx + mod_c[2] * mix_c
        h2 = modulate(rms_norm(x, norm2_g[layer]), mod_x[3], mod_x[4])
        if not last:
            hc2 = modulate(rms_norm(ctx, norm2_g[layer]), mod_c[3], mod_c[4])
            h2 = jnp.concatenate([h2, hc2], axis=1)
        idx = layer // 2
        if layer % 2 == 0:
            f = swiglu(h2, ffn_w_gate[idx], ffn_w_up[idx], ffn_w_down[idx])
        else:
            f = moe_swiglu(h2, moe_router[idx], moe_w_gate[idx], moe_w_up[idx], moe_w_down[idx])
        x = x + mod_x[5] * f[:, :L]
        if not last:
            ctx = ctx + mod_c[5] * f[:, L:]
    return x
```

```python
import numpy as np
import concourse.bass as bass
import concourse.mybir as mybir
from concourse.bass_utils import run_bass_kernel_spmd

F32 = mybir.dt.float32
BF16 = mybir.dt.bfloat16
ALU = mybir.AluOpType
AF = mybir.ActivationFunctionType
AX = mybir.AxisListType

D = 2048
KC = D // 128
B = 2
L = 4096
CTX = 256
TB = L + CTX
DEPTH = 4
DFF = 5632
FC = DFF // 128
NE = 8
EPS = 1e-6
TOK = 1088


class Buf:
    def __init__(self, name, t=None, psum=False):
        self.name = name
        self.t = t
        self.psum = psum
        self.w = None
        self.r = []
        self.sem = None
        self.dma_total = 0

    def __getitem__(self, k):
        return self.t[k]


class Em:
    ENG = ("pe", "act", "dve", "pool", "sp")

    def __init__(self, nc):
        self.nc = nc
        self.eng = {"pe": nc.tensor, "act": nc.scalar, "dve": nc.vector, "pool": nc.gpsimd, "sp": nc.sync}
        self.sem = {k: nc.alloc_semaphore("sem_" + k) for k in self.ENG}
        self.cnt = {k: 0 for k in self.ENG}
        self.obs = {k: {} for k in self.ENG}
        self.out_bufs = []
        self.guards = []
        self.free_sems = []
        self.scope_bufs = []
        self.persist_bufs = []
        self.in_scope = False
        self.mark = 0
        self.uid = 0

    def _nm(self, name):
        self.uid += 1
        return f"{name}_{self.uid}"

    def sb(self, name, shape, dtype=F32):
        g = self.nc.sbuf_tensor(self._nm(name), list(shape), dtype)
        t = g.__enter__()
        self.guards.append(g)
        b = Buf(name, t)
        b.scoped = self.in_scope
        return b

    def ps(self, name, shape, dtype=F32):
        g = self.nc.psum_tensor(self._nm(name), list(shape), dtype)
        t = g.__enter__()
        self.guards.append(g)
        b = Buf(name, t, psum=True)
        b.scoped = self.in_scope
        return b

    def dram(self, name, shape, dtype, kind="Internal"):
        t = self.nc.dram_tensor(name, list(shape), dtype, kind=kind)
        b = Buf(name, t.ap())
        b.scoped = False
        if kind == "ExternalOutput":
            self.out_bufs.append(b)
        return b

    def _get_sem(self, dst):
        if dst.sem is None:
            if self.free_sems:
                dst.sem, dst.dma_total = self.free_sems.pop()
            else:
                dst.sem = self.nc.alloc_semaphore(self._nm("dsem"))
            (self.scope_bufs if getattr(dst, "scoped", False) else self.persist_bufs).append(dst)

    def scope_begin(self):
        self.in_scope = True
        self.mark = len(self.guards)
        self.scope_bufs = []

    def barrier(self):
        evs = [(self.sem[k], self.cnt[k]) for k in self.ENG if self.cnt[k] > 0]
        evs += [(b.sem, b.dma_total) for b in self.scope_bufs + self.persist_bufs if b.dma_total > 0]
        for e in self.ENG:
            self._wait(e, evs)

    def scope_end(self):
        self.barrier()
        for b in self.scope_bufs:
            self.free_sems.append((b.sem, b.dma_total))
        self.scope_bufs = []
        while len(self.guards) > self.mark:
            self.guards.pop().__exit__(None, None, None)
        self.in_scope = False

    def pool_(self, name, shape, dtype, n):
        bufs = [self.sb(f"{name}{i}", shape, dtype) for i in range(n)]
        st = {"i": 0}

        def nxt():
            b = bufs[st["i"] % n]
            st["i"] += 1
            return b
        return nxt

    def _wait(self, e, evs):
        need = {}
        for ev in evs:
            if ev is None:
                continue
            s, v = ev
            k = s.num
            if k not in need or need[k][1] < v:
                need[k] = (s, v)
        for k, (s, v) in need.items():
            if e == "pe" and s is self.sem["pe"]:
                continue
            if self.obs[e].get(k, 0) >= v:
                continue
            self.eng[e].wait_ge(s, v)
            self.obs[e][k] = v

    def op(self, e, fn, reads=(), writes=(), signal=True):
        evs = []
        for b in reads:
            evs.append(b.w)
            if b.psum:
                evs.extend(b.r)
        for b in writes:
            evs.append(b.w)
            evs.extend(b.r)
        self._wait(e, evs)
        ins = fn(self.eng[e])
        if signal:
            self.cnt[e] += 1
            ins.then_inc(self.sem[e], 1)
            ev = (self.sem[e], self.cnt[e])
        else:
            ev = (self.sem[e], self.cnt[e] + 1)
        for b in writes:
            b.w = ev
            b.r = []
        for b in reads:
            if b not in writes:
                b.r = [x for x in b.r if x[0] is not ev[0]] + [ev]
        return ins

    def dma(self, q, out_ap, in_ap, dst, src):
        self._get_sem(dst)
        evs = [src.w]
        evs.extend(dst.r)
        if dst.w is not None and dst.w[0] is not dst.sem:
            evs.append(dst.w)
        self._wait(q, evs)
        ins = self.eng[q].dma_start(out=out_ap, in_=in_ap)
        ins.then_inc(dst.sem, 16)
        dst.dma_total += 16
        ev = (dst.sem, dst.dma_total)
        dst.w = ev
        dst.r = []
        src.r = [x for x in src.r if x[0] is not ev[0]] + [ev]
        return ins

    def cc(self, kind, op, groups, out_ap, in_ap, dst, src):
        self._get_sem(dst)
        evs = [src.w, dst.w]
        evs.extend(dst.r)
        evs.extend(src.r)
        self._wait("pool", evs)
        ins = self.eng["pool"].collective_compute(kind, op, replica_groups=groups, ins=[in_ap.opt()], outs=[out_ap.opt()])
        ins.then_inc(dst.sem)
        dst.dma_total += 1
        ev = (dst.sem, dst.dma_total)
        dst.w = ev
        dst.r = []
        src.r = [x for x in src.r if x[0] is not ev[0]] + [ev]
        return ins

    def finish(self):
        evs = [b.w for b in self.out_bufs]
        self._wait("sp", evs)
        self._wait("sp", [(self.sem[k], self.cnt[k]) for k in ("pe", "act", "dve", "pool") if self.cnt[k] > 0])


def tiles_of(n, t=512):
    return [(s, min(t, n - s)) for s in range(0, n, t)]


class Consts:
    def __init__(self, em):
        self.ones_bf = em.sb("c_ones_bf", [128, 128], BF16)
        self.ones_f = em.sb("c_ones_f", [128, 128], F32)
        self.ident_f = em.sb("c_ident_f", [128, 128], F32)
        self.ident_bf = em.sb("c_ident_bf", [128, 128], BF16)
        self.eps = em.sb("c_eps", [128, 1], F32)
        self.one = em.sb("c_one", [128, 1], F32)
        em.op("pool", lambda e: e.memset(self.ones_f[:], 1.0), writes=[self.ones_f])
        em.op("pool", lambda e: e.memset(self.ones_bf[:], 1.0), writes=[self.ones_bf])
        em.op("pool", lambda e: e.memset(self.eps[:], EPS), writes=[self.eps])
        em.op("pool", lambda e: e.memset(self.one[:], 1.0), writes=[self.one])
        em.op("pool", lambda e: e.memset(self.ident_f[:], 0.0), writes=[self.ident_f])
        em.op("pool", lambda e: e.affine_select(out=self.ident_f[:], in_=self.ident_f[:], pattern=[[-1, 128]],
                                                 compare_op=ALU.not_equal, fill=1.0, base=0, channel_multiplier=1),
              reads=[self.ident_f], writes=[self.ident_f])
        em.op("dve", lambda e: e.tensor_copy(out=self.ident_bf[:], in_=self.ident_f[:]), reads=[self.ident_f],
              writes=[self.ident_bf])


class PsumRing:
    def __init__(self, em, n=8):
        self.banks = [em.ps(f"psb{i}", [128, 512], F32) for i in range(n)]
        self.i = 0

    def next(self):
        b = self.banks[self.i % len(self.banks)]
        self.i += 1
        return b


def mm_acc(em, pbank, out_ap, pairs, reads, sparse=True):
    n = len(pairs)
    for i, (l, r) in enumerate(pairs):
        em.op("pe", lambda e, l=l, r=r, i=i: e.matmul(out_ap, lhsT=l, rhs=r, start=(i == 0), stop=(i == n - 1)),
              reads=reads, writes=[pbank], signal=(i == n - 1) or not sparse)


def make_affine(em, a_out, g, scale_ap, a_buf_reads):
    em.op("dve", lambda e: e.scalar_tensor_tensor(out=a_out, in0=scale_ap, scalar=1.0, in1=g, op0=ALU.add, op1=ALU.mult),
          reads=a_buf_reads, writes=[a_buf_reads[0]])


def norm_mod(em, cst, psr, x, out, par, tiles, pools, hf_cb=None):
    sq_p, r_p, tmp_p, hf_p = pools
    for ti, (s, n, is_c) in enumerate(tiles):
        pb = psr.next()
        sqs = []
        for k in range(KC):
            sq = sq_p()
            em.op("act", lambda e, sq=sq, k=k: e.activation(out=sq[:, 0:n], in_=x[:, k, s:s + n], func=AF.Square),
                  reads=[x], writes=[sq])
            em.op("pe", lambda e, sq=sq, k=k: e.matmul(pb[:, 0:n], lhsT=cst.ones_bf[:], rhs=sq[:, 0:n], start=(k == 0),
                                                       stop=(k == KC - 1)),
                  reads=[sq, cst.ones_bf], writes=[pb])
        r = r_p()
        em.op("act", lambda e: e.activation(out=r[:, 0:n], in_=pb[:, 0:n], func=AF.Sqrt, bias=cst.eps[:, 0:1], scale=1.0 / D),
              reads=[pb, cst.eps], writes=[r])
        em.op("dve", lambda e: e.reciprocal(out=r[:, 0:n], in_=r[:, 0:n]), reads=[r], writes=[r])
        hf = hf_p() if hf_cb is not None else None
        o = 2 if is_c else 0
        for k in range(KC):
            if hf is None:
                tmp = tmp_p()
                tv = tmp[:, 0:n]
                tb = tmp
            else:
                tv = hf[:, k, 0:n]
                tb = hf
            em.op("dve", lambda e, k=k, tv=tv: e.tensor_tensor(out=tv, in0=x[:, k, s:s + n], in1=r[:, 0:n], op=ALU.mult),
                  reads=[x, r], writes=[tb])
            if hf is None:
                em.op("act", lambda e, k=k, tv=tv: e.activation(out=out[:, k, s:s + n], in_=tv, func=AF.Identity,
                                                                scale=par[:, o, k:k + 1], bias=par[:, o + 1, k:k + 1]),
                      reads=[tb, par], writes=[out])
            else:
                em.op("act", lambda e, k=k, tv=tv: e.activation(out=tv, in_=tv, func=AF.Identity,
                                                                scale=par[:, o, k:k + 1], bias=par[:, o + 1, k:k + 1]),
                      reads=[tb, par], writes=[tb])
                em.op("pool", lambda e, k=k, tv=tv: e.tensor_copy(out=out[:, k, s:s + n], in_=tv), reads=[tb], writes=[out])
        if hf_cb is not None:
            hf_cb(ti, s, n, hf)


def load_par(em, par, mod, g, split_shift, split_scale):
    for c in range(2):
        em.op("dve", lambda e, c=c: e.scalar_tensor_tensor(out=par[:, 2 * c, :], in0=mod[:, c, split_scale * 16:split_scale * 16 + 16],
                                                           scalar=1.0, in1=g[:], op0=ALU.add, op1=ALU.mult),
              reads=[mod, g], writes=[par])
        em.op("dve", lambda e, c=c: e.tensor_copy(out=par[:, 2 * c + 1, :], in_=mod[:, c, split_shift * 16:split_shift * 16 + 16]),
              reads=[mod], writes=[par])


TOK_TILES = [(0, 512, False), (512, 512, False), (1024, 64, True)]


def norm_pools(em, with_hf=False, hf_n=256):
    sq_p = em.pool_("nm_sq", [128, 512], BF16, 3)
    r_p = em.pool_("nm_r", [128, 512], F32, 2)
    tmp_p = em.pool_("nm_tmp", [128, 512], F32, 3)
    hf_p = em.pool_("nm_hf", [128, KC, hf_n], F32, 1) if with_hf else None
    return (sq_p, r_p, tmp_p, hf_p)


def build_M():
    nc = bass.Bass("TRN2", target_bir_lowering=False)
    em = Em(nc)
    condT = em.dram("condT", [128, KC, 3], F32, kind="ExternalInput")
    adaw = em.dram("adaw", [DEPTH, D, 1536], F32, kind="ExternalInput")
    adab = em.dram("adab", [128, DEPTH, 12], F32, kind="ExternalInput")
    mod = em.dram("mod", [128, DEPTH, 12, 3], F32, kind="ExternalOutput")
    psr = PsumRing(em, 4)
    cf = em.sb("cf", [128, KC, 3], F32)
    cb = em.sb("cb", [128, KC, 3], BF16)
    bt = em.sb("bt", [128, DEPTH, 12], F32)
    res = em.sb("res", [128, DEPTH, 12, 3], F32)
    wp = em.pool_("adaw_sb", [128, KC, 1536], BF16, 2)
    em.dma("sp", cf[:], condT[:], cf, condT)
    em.dma("sp", bt[:], adab[:], bt, adab)
    em.op("act", lambda e: e.activation(out=cb[:], in_=cf[:], func=AF.Silu), reads=[cf], writes=[cb])
    for l in range(DEPTH):
        w = wp()
        em.dma("pool", w[:], adaw[l].rearrange("(k p) f -> p k f", p=128), w, adaw)
        pb = psr.next()
        for j in range(12):
            mm_acc(em, pb, pb[:, 3 * j:3 * j + 3], [(w[:, k, 128 * j:128 * j + 128], cb[:, k, :]) for k in range(KC)], [w, cb])
        em.op("dve", lambda e, l=l: e.tensor_tensor(out=res[:, l, :, :], in0=pb[:, 0:36].rearrange("p (j c) -> p j c", c=3),
                                                    in1=bt[:, l, :].unsqueeze(2).to_broadcast([128, 12, 3]), op=ALU.add),
              reads=[pb, bt], writes=[res])
    em.dma("sp", mod[:], res[:], mod, res)
    em.finish()
    return nc


def build_A0():
    nc = bass.Bass("TRN2", target_bir_lowering=False)
    em = Em(nc)
    xT = em.dram("xT", [128, KC, TOK], F32, kind="ExternalInput")
    modd = em.dram("mod", [128, 2, 96], F32, kind="ExternalInput")
    g1d = em.dram("g1", [128, KC], F32, kind="ExternalInput")
    hxT = em.dram("hxT", [128, KC, TOK], BF16, kind="ExternalOutput")
    cst = Consts(em)
    psr = PsumRing(em, 4)
    x = em.sb("x", [128, KC, TOK], F32)
    h = em.sb("h", [128, KC, TOK], BF16)
    mod = em.sb("modsb", [128, 2, 96], F32)
    g1 = em.sb("g1sb", [128, KC], F32)
    par = em.sb("par", [128, 4, KC], F32)
    em.dma("sp", x[:], xT[:], x, xT)
    em.dma("sp", mod[:], modd[:], mod, modd)
    em.dma("sp", g1[:], g1d[:], g1, g1d)
    load_par(em, par, mod, g1, 0, 1)
    norm_mod(em, cst, psr, x, h, par, TOK_TILES, norm_pools(em))
    em.dma("sp", hxT[:], h[:], hxT, h)
    em.finish()
    return nc


_PROGS = {}


def _prog(name, builder, *args):
    key = (name,) + tuple(args)
    if key not in _PROGS:
        _PROGS[key] = builder(*args)
    return _PROGS[key]


def _run(nc, in_maps):
    res = run_bass_kernel_spmd(nc, in_maps, core_ids=list(range(len(in_maps))))
    return res.results


def fm(v):
    v = np.asarray(v)
    lead = v.shape[:-1]
    r = v.reshape(lead + (KC, 128))
    r = np.moveaxis(r, -1, 0)
    r = np.moveaxis(r, -1, 1)
    return np.ascontiguousarray(r)


def host_M(c, c_ctx, ada_w, ada_b):
    cond = np.stack([c[0], c[1], c_ctx], axis=0)
    condT = fm(cond)
    in_maps = []
    for i in range(8):
        cols = np.concatenate([np.arange(128 * (12 * i), 128 * (12 * i + 12))])
        aw = np.ascontiguousarray(ada_w[:, :, cols])
        ab = ada_b[:, cols].reshape(DEPTH, 12, 128).transpose(2, 0, 1)
        in_maps.append({"condT": condT, "adaw": aw, "adab": np.ascontiguousarray(ab)})
    res = _run(_prog("M", build_M), in_maps)
    mod = np.concatenate([r["mod"] for r in res], axis=2)
    return [np.ascontiguousarray(mod[:, l]) for l in range(DEPTH)]


def mod_for_core(modl, b):
    return np.ascontiguousarray(np.stack([modl[:, :, b], modl[:, :, 2]], axis=1))


def shard_tokens(x, ctx):
    outs = []
    for b in range(B):
        for j in range(4):
            t = np.concatenate([x[b, 1024 * j:1024 * (j + 1)], ctx[b, 64 * j:64 * (j + 1)]], axis=0)
            outs.append(np.ascontiguousarray(t.reshape(TOK, KC, 128).transpose(2, 1, 0)))
    return outs


NT128 = 9


def build_C():
    nc = bass.Bass("TRN2", target_bir_lowering=False)
    em = Em(nc)
    xT = em.dram("xT", [128, KC, TOK], F32, kind="ExternalInput")
    mixT = em.dram("mixT", [128, KC, TOK], BF16, kind="ExternalInput")
    wout = em.dram("wout", [D, D], F32, kind="ExternalInput")
    modd = em.dram("mod", [128, 2, 96], F32, kind="ExternalInput")
    n2gd = em.dram("n2g", [128, KC], F32, kind="ExternalInput")
    routd = em.dram("router", [128, KC, NE], F32, kind="ExternalInput")
    xo = em.dram("xo", [128, KC, TOK], F32, kind="ExternalOutput")
    h2T = em.dram("h2T", [128, KC, TOK], BF16, kind="ExternalOutput")
    gout = em.dram("gates", [128, NT128, NE], F32, kind="ExternalOutput")
    cst = Consts(em)
    psr = PsumRing(em, 6)
    x = em.sb("x", [128, KC, TOK], F32)
    mix = em.sb("mix", [128, KC, TOK], BF16)
    h2 = em.sb("h2", [128, KC, TOK], BF16)
    mod = em.sb("modsb", [128, 2, 96], F32)
    n2g = em.sb("n2gsb", [128, KC], F32)
    rout = em.sb("routsb", [128, KC, NE], F32)
    par = em.sb("par", [128, 4, KC], F32)
    gates = em.sb("gatessb", [128, NT128, NE], F32)
    em.dma("sp", x[:], xT[:], x, xT)
    em.dma("sp", mix[:], mixT[:], mix, mixT)
    em.dma("sp", mod[:], modd[:], mod, modd)
    em.dma("sp", n2g[:], n2gd[:], n2g, n2gd)
    em.dma("sp", rout[:], routd[:], rout, routd)
    em.op("pool", lambda e: e.memset(gates[:], 0.0), writes=[gates])
    wp = em.pool_("wout_sb", [128, KC, 512], BF16, 2)
    for cg in range(4):
        w = wp()
        em.dma("pool", w[:], wout[:, 512 * cg:512 * cg + 512].rearrange("(k p) f -> p k f", p=128), w, wout)
        for dc in range(4):
            d = 4 * cg + dc
            for (s, n, is_c) in TOK_TILES:
                pb = psr.next()
                mm_acc(em, pb, pb[:, 0:n], [(w[:, k, 128 * dc:128 * dc + 128], mix[:, k, s:s + n]) for k in range(KC)], [w, mix])
                gcol = mod[:, 1 if is_c else 0, 32 + d:32 + d + 1]
                em.op("dve", lambda e, pb=pb, d=d, s=s, n=n, gcol=gcol: e.scalar_tensor_tensor(
                    out=x[:, d, s:s + n], in0=pb[:, 0:n], scalar=gcol, in1=x[:, d, s:s + n], op0=ALU.mult, op1=ALU.add),
                    reads=[pb, mod, x], writes=[x])
    em.dma("sp", xo[:], x[:], xo, x)
    load_par(em, par, mod, n2g, 3, 4)
    small = em.pool_("rt_small", [128, 8], F32, 12)
    lgp = em.pool_("rt_lg", [128, NE], F32, 3)

    def router_cb(ti, s, n, hf):
        for j in range(0, n, 128):
            m = min(128, n - j)
            t128 = (s + j) // 128
            pb = psr.next()
            mm_acc(em, pb, pb[0:m, 0:NE], [(hf[:, k, j:j + m], rout[:, k, :]) for k in range(KC)], [hf, rout])
            lg = lgp()
            em.op("act", lambda e: e.activation(out=lg[0:m, :], in_=pb[0:m, 0:NE], func=AF.Copy), reads=[pb], writes=[lg])
            m1 = small(); eq = small(); lg2 = small(); m2 = small(); sel = small(); nm1 = small(); ex = small(); den = small()
            em.op("dve", lambda e: e.reduce_max(out=m1[0:m, 0:1], in_=lg[0:m, :], axis=AX.X), reads=[lg], writes=[m1])
            em.op("dve", lambda e: e.tensor_scalar(out=eq[0:m, :], in0=lg[0:m, :], scalar1=m1[0:m, 0:1], scalar2=None, op0=ALU.is_equal),
                  reads=[lg, m1], writes=[eq])
            em.op("dve", lambda e: e.scalar_tensor_tensor(out=lg2[0:m, :], in0=eq[0:m, :], scalar=-1e30, in1=lg[0:m, :], op0=ALU.mult, op1=ALU.add),
                  reads=[eq, lg], writes=[lg2])
            em.op("dve", lambda e: e.reduce_max(out=m2[0:m, 0:1], in_=lg2[0:m, :], axis=AX.X), reads=[lg2], writes=[m2])
            em.op("dve", lambda e: e.tensor_scalar(out=sel[0:m, :], in0=lg[0:m, :], scalar1=m2[0:m, 0:1], scalar2=None, op0=ALU.is_ge),
                  reads=[lg, m2], writes=[sel])
            em.op("dve", lambda e: e.tensor_scalar(out=nm1[0:m, 0:1], in0=m1[0:m, 0:1], scalar1=-1.0, scalar2=None, op0=ALU.mult),
                  reads=[m1], writes=[nm1])
            em.op("act", lambda e: e.activation(out=ex[0:m, :], in_=lg[0:m, :], func=AF.Exp, bias=nm1[0:m, 0:1], scale=1.0),
                  reads=[lg, nm1], writes=[ex])
            em.op("dve", lambda e: e.tensor_tensor(out=ex[0:m, :], in0=ex[0:m, :], in1=sel[0:m, :], op=ALU.mult), reads=[ex, sel], writes=[ex])
            em.op("dve", lambda e: e.reduce_sum(out=den[0:m, 0:1], in_=ex[0:m, :], axis=AX.X), reads=[ex], writes=[den])
            em.op("dve", lambda e: e.reciprocal(out=den[0:m, 0:1], in_=den[0:m, 0:1]), reads=[den], writes=[den])
            em.op("dve", lambda e: e.tensor_scalar(out=gates[0:m, t128, :], in0=ex[0:m, :], scalar1=den[0:m, 0:1], scalar2=None, op0=ALU.mult),
                  reads=[ex, den], writes=[gates])

    c_tiles = [(0, 256, False), (256, 256, False), (512, 256, False), (768, 256, False), (1024, 64, True)]
    norm_mod(em, cst, psr, x, h2, par, c_tiles, norm_pools(em, with_hf=True), hf_cb=router_cb)
    em.dma("sp", h2T[:], h2[:], h2T, h2)
    em.dma("sp", gout[:], gates[:], gout, gates)
    em.finish()
    return nc


NTOK_ALL = 8 * TOK


def build_D(F, GC):
    nc = bass.Bass("TRN2", target_bir_lowering=False)
    em = Em(nc)
    NG = F // (128 * GC)
    h2T = em.dram("h2T", [128, KC, NTOK_ALL], BF16, kind="ExternalInput")
    grow = em.dram("grow", [1, NTOK_ALL], F32, kind="ExternalInput")
    wg = em.dram("wg", [D, F], F32, kind="ExternalInput")
    wu = em.dram("wu", [D, F], F32, kind="ExternalInput")
    wd = em.dram("wd", [F, D], F32, kind="ExternalInput")
    yT = em.dram("yT", [128, KC, NTOK_ALL], F32, kind="ExternalOutput")
    psr = PsumRing(em, 8)
    hp = em.pool_("h_sb", [128, KC, 512], BF16, 2)
    gp = em.pool_("g_sb", [128, 512], F32, 2)
    wgp = em.pool_("wg_sb", [128, KC, 128 * GC], BF16, 2)
    wup = em.pool_("wu_sb", [128, KC, 128 * GC], BF16, 2)
    wdp = em.pool_("wd_sb", [128, GC, D], BF16, 2)
    actp = em.pool_("act_sb", [128, GC, 512], BF16, 2)
    sgp = em.pool_("sg_sb", [128, 512], F32, 3)
    yacc = em.sb("yacc", [128, KC, 512], F32)
    for tt in range(NTOK_ALL // 512):
        s = 512 * tt
        h = hp()
        em.dma("sp", h[:], h2T[:, :, s:s + 512], h, h2T)
        g = gp()
        em.dma("sp", g[:], grow[0:1, s:s + 512].partition_broadcast(128), g, grow)
        for gi in range(NG):
            wgs = wgp(); wus = wup(); wds = wdp()
            c0 = 128 * GC * gi
            em.dma("pool", wgs[:], wg[:, c0:c0 + 128 * GC].rearrange("(k p) f -> p k f", p=128), wgs, wg)
            em.dma("pool", wus[:], wu[:, c0:c0 + 128 * GC].rearrange("(k p) f -> p k f", p=128), wus, wu)
            em.dma("pool", wds[:], wd[c0:c0 + 128 * GC, :].rearrange("(c p) f -> p c f", p=128), wds, wd)
            act = actp()
            for c in range(GC):
                pg = psr.next(); pu = psr.next()
                mm_acc(em, pg, pg[:, :], [(wgs[:, k, 128 * c:128 * c + 128], h[:, k, :]) for k in range(KC)], [wgs, h])
                mm_acc(em, pu, pu[:, :], [(wus[:, k, 128 * c:128 * c + 128], h[:, k, :]) for k in range(KC)], [wus, h])
                sg = sgp()
                em.op("act", lambda e, pg=pg, sg=sg: e.activation(out=sg[:], in_=pg[:, :], func=AF.Silu), reads=[pg], writes=[sg])
                em.op("dve", lambda e, pu=pu, sg=sg, c=c: e.tensor_tensor(out=act[:, c, :], in0=pu[:, :], in1=sg[:], op=ALU.mult),
                      reads=[pu, sg], writes=[act])
            for d in range(KC):
                pd = psr.next()
                mm_acc(em, pd, pd[:, :], [(wds[:, c, 128 * d:128 * d + 128], act[:, c, :]) for c in range(GC)], [wds, act])
                if gi == 0:
                    em.op("act", lambda e, pd=pd, d=d: e.activation(out=yacc[:, d, :], in_=pd[:, :], func=AF.Copy), reads=[pd], writes=[yacc])
                else:
                    em.op("dve", lambda e, pd=pd, d=d: e.tensor_tensor(out=yacc[:, d, :], in0=pd[:, :], in1=yacc[:, d, :], op=ALU.add),
                          reads=[pd, yacc], writes=[yacc])
        for d in range(KC):
            em.op("pool", lambda e, d=d: e.tensor_tensor(out=yacc[:, d, :], in0=yacc[:, d, :], in1=g[:], op=ALU.mult), reads=[yacc, g], writes=[yacc])
        em.dma("sp", yT[:, :, s:s + 512], yacc[:], yT, yacc)
    em.finish()
    return nc


def build_E():
    nc = bass.Bass("TRN2", target_bir_lowering=False)
    em = Em(nc)
    xT = em.dram("xT", [128, KC, TOK], F32, kind="ExternalInput")
    yp = em.dram("yp", [NE, 128, KC, TOK], F32, kind="ExternalInput")
    modd = em.dram("mod", [128, 2, 96], F32, kind="ExternalInput")
    modnd = em.dram("modn", [128, 2, 96], F32, kind="ExternalInput")
    g1d = em.dram("g1n", [128, KC], F32, kind="ExternalInput")
    xo = em.dram("xo", [128, KC, TOK], F32, kind="ExternalOutput")
    hxT = em.dram("hxT", [128, KC, TOK], BF16, kind="ExternalOutput")
    cst = Consts(em)
    psr = PsumRing(em, 4)
    x = em.sb("x", [128, KC, TOK], F32)
    h = em.sb("h", [128, KC, TOK], BF16)
    mod = em.sb("modsb", [128, 2, 96], F32)
    modn = em.sb("modnsb", [128, 2, 96], F32)
    g1 = em.sb("g1sb", [128, KC], F32)
    par = em.sb("par", [128, 4, KC], F32)
    em.dma("sp", x[:], xT[:], x, xT)
    em.dma("sp", mod[:], modd[:], mod, modd)
    em.dma("sp", modn[:], modnd[:], modn, modnd)
    em.dma("sp", g1[:], g1d[:], g1, g1d)
    pp = em.pool_("yp_sb", [128, 4, TOK], F32, 3)
    for ei in range(NE):
        for kg in range(4):
            p = pp()
            em.dma("sp", p[:], yp[ei, :, 4 * kg:4 * kg + 4, :], p, yp)
            for kk in range(4):
                k = 4 * kg + kk
                for (s, n, c) in ((0, 1024, 0), (1024, 64, 1)):
                    em.op("dve", lambda e, p=p, kk=kk, k=k, s=s, n=n, c=c: e.scalar_tensor_tensor(
                        out=x[:, k, s:s + n], in0=p[:, kk, s:s + n], scalar=mod[:, c, 80 + k:80 + k + 1], in1=x[:, k, s:s + n],
                        op0=ALU.mult, op1=ALU.add), reads=[p, mod, x], writes=[x])
    em.dma("sp", xo[:], x[:], xo, x)
    load_par(em, par, modn, g1, 0, 1)
    norm_mod(em, cst, psr, x, h, par, TOK_TILES, norm_pools(em))
    em.dma("sp", hxT[:], h[:], hxT, h)
    em.finish()
    return nc


NT = TB // 128
W_NA = 768
W_M = 1028
MASKNEG = -30000.0


def _na_start(r):
    return min(max(r - 4, 0), 56)


def build_B():
    nc = bass.Bass("TRN2", target_bir_lowering=False)
    em = Em(nc)
    io = {}
    hxT = em.dram("hxT", [128, KC, TB], BF16, kind="ExternalInput")
    io["wna"] = em.dram("wna", [D, W_NA], F32, kind="ExternalInput")
    io["wm"] = em.dram("wm", [D, W_M], F32, kind="ExternalInput")
    io["cosT"] = em.dram("cosT", [128, L], F32, kind="ExternalInput")
    io["sinT"] = em.dram("sinT", [128, L], F32, kind="ExternalInput")
    io["nab"] = em.dram("nab", [2, 64, 15, 64], F32, kind="ExternalInput")
    io["cmask"] = em.dram("cmask", [64, 64], F32, kind="ExternalInput")
    io["nqg"] = em.dram("nqg", [128, 1], F32, kind="ExternalInput")
    io["nkg"] = em.dram("nkg", [128, 1], F32, kind="ExternalInput")
    io["mng"] = em.dram("mng", [128, 2], F32, kind="ExternalInput")
    io["gb"] = em.dram("gb", [128, 4], F32, kind="ExternalInput")
    io["mixT"] = em.dram("mixT", [128, 4, 4, TOK], BF16, kind="ExternalOutput")
    io["load_hx"] = lambda hx, s, n: em.dma("sp", hx[:], hxT[:, :, s:s + n], hx, hxT)
    cst = Consts(em)
    psr = PsumRing(em, 7)
    ptr = em.ps("ps_tr", [128, 128], BF16)
    emit_B(em, cst, psr, ptr, io)
    em.finish()
    return nc


def emit_B(em, cst, psr, ptr, io):
    wna, wm, cosd, sind, nabd, cmd = io["wna"], io["wm"], io["cosT"], io["sinT"], io["nab"], io["cmask"]
    nqgd, nkgd, mngd, gbd, mixT = io["nqg"], io["nkg"], io["mng"], io["gb"], io["mixT"]
    load_hx = io["load_hx"]

    def store_mix(c, s, n, tile, buf):
        if s < L:
            em.dma("sp", mixT[:, s // 1024, c, s % 1024:s % 1024 + n], tile, mixT, buf)
        else:
            for r in range(4):
                em.dma("sp", mixT[:, r, c, 1024:TOK], tile[:, 64 * r:64 * r + 64], mixT, buf)

    W = em.sb("W", [128, KC, W_M], BF16)
    QK = em.sb("QK", [128, 4 * TB], BF16)
    VO = em.sb("VO", [128, 2 * TB], BF16)
    qk4 = QK[:].rearrange("p (c t) -> p c t", c=4)
    vna = VO[:].rearrange("p (n h d) -> p n h d", n=NT, h=2)
    sigo = VO[:].rearrange("p (c t) -> p c t", c=2)
    hsum = QK[:].bitcast(F32).rearrange("p (c t) -> p c t", c=2)
    mQ = em.sb("mQ", [128, TB], BF16)
    mK = em.sb("mK", [128, TB], BF16)
    mV = em.sb("mV", [128, NT, 257], BF16)
    G = em.sb("G", [128, NT, 4], F32)
    E = em.sb("E", [128, 2, 15, 64], BF16)
    nqg = em.sb("nqg_sb", [128, 1], F32)
    nkg = em.sb("nkg_sb", [128, 1], F32)
    mng = em.sb("mng_sb", [128, 2], F32)
    gb = em.sb("gb_sb", [128, 4], F32)
    cm = em.sb("cm_sb", [128, 64], F32)
    for (t, dsrc) in ((nqg, nqgd), (nkg, nkgd), (mng, mngd), (gb, gbd)):
        em.dma("sp", t[:], dsrc[:], t, dsrc)
    em.dma("sp", cm[0:64, :], cmd[:], cm, cmd)
    em.dma("sp", cm[64:128, :], cmd[:], cm, cmd)

    hxp = em.pool_("hx_sb", [128, KC, 256], BF16, 2)
    t256 = em.pool_("t256", [128, 256], F32, 6)
    b256 = em.pool_("b256", [128, 256], BF16, 3)

    em.dma("pool", W[:, :, 0:W_NA], wna[:].rearrange("(k p) f -> p k f", p=128), W, wna)
    tok_tiles = [(256 * i, 256) for i in range(TB // 256)]
    for (s, n) in tok_tiles:
        hx = hxp()
        load_hx(hx, s, n)
        for c in range(4):
            pb = psr.next()
            mm_acc(em, pb, pb[:, 0:n], [(W[:, k, 128 * c:128 * c + 128], hx[:, k, :]) for k in range(KC)], [W, hx])
            sq = b256()
            em.op("act", lambda e, pb=pb, sq=sq: e.activation(out=sq[:], in_=pb[:, 0:n], func=AF.Square), reads=[pb], writes=[sq])
            pb2 = psr.next()
            em.op("pe", lambda e, pb2=pb2, sq=sq: e.matmul(pb2[:, 0:n], lhsT=cst.ones_bf[:], rhs=sq[:], start=True, stop=True),
                  reads=[sq, cst.ones_bf], writes=[pb2])
            r = t256()
            em.op("act", lambda e, pb2=pb2, r=r: e.activation(out=r[:], in_=pb2[:, 0:n], func=AF.Sqrt, bias=cst.eps[:, 0:1], scale=1.0 / 128),
                  reads=[pb2, cst.eps], writes=[r])
            em.op("dve", lambda e, r=r: e.reciprocal(out=r[:], in_=r[:]), reads=[r], writes=[r])
            gsb = nqg if c < 2 else nkg
            em.op("dve", lambda e, pb=pb, r=r, c=c, gsb=gsb: e.scalar_tensor_tensor(out=qk4[:, c, s:s + n], in0=pb[:, 0:n], scalar=gsb[:, 0:1],
                                                                                   in1=r[:], op0=ALU.mult, op1=ALU.mult),
                  reads=[pb, r, gsb], writes=[QK])
        for j in range(n // 128):
            pb = psr.next()
            mm_acc(em, pb, pb[:, 0:256], [(hx[:, k, 128 * j:128 * j + 128], W[:, k, 512:768]) for k in range(KC)], [W, hx])
            tt = s // 128 + j
            em.op("act", lambda e, pb=pb, tt=tt: e.activation(out=vna[:, tt, :, :], in_=pb[:, 0:256].rearrange("p (h d) -> p h d", h=2), func=AF.Copy),
                  reads=[pb], writes=[VO])

    ebias = em.sb("ebias", [128, 2, 15, 64], F32)
    for hf in range(2):
        for h in range(2):
            em.dma("sp", ebias[64 * hf:64 * hf + 64, h, :, :], nabd[h], ebias, nabd)
    em.op("act", lambda e: e.activation(out=ebias[:], in_=ebias[:], func=AF.Exp), reads=[ebias], writes=[ebias])
    for h in range(2):
        em.op("dve", lambda e, h=h: e.tensor_tensor(out=E[:, h, :, :], in0=ebias[:, h, :, :], in1=cm[:].unsqueeze(1).to_broadcast([128, 15, 64]), op=ALU.mult),
              reads=[ebias, cm], writes=[E])

    ptp = em.pool_("PT", [128, 7, 128], BF16, 3)
    rdp = em.pool_("rden", [128, 256], F32, 3)
    nop = em.pool_("naout", [128, 256], BF16, 3)
    scale = 128 ** -0.5
    for h in range(2):
        qh = qk4[:, h, :]
        kh = qk4[:, 2 + h, :]
        for i in range(32):
            q0 = 128 * i
            u = min(max(2 * i - 4, 0), 54)
            tiles = []
            for t in range(5):
                val = [[False, False], [False, False]]
                for hf in range(2):
                    rk = u + 2 * t + hf
                    for e_ in range(2):
                        rq = 2 * i + e_
                        val[hf][e_] = (_na_start(rq) <= rk <= _na_start(rq) + 7)
                a0 = val[0][0] or val[0][1]
                a1 = val[1][0] or val[1][1]
                if not a0 and not a1:
                    continue
                assert a0
                tiles.append((t, u // 2 + t, 128 if a1 else 64, val))
            pA = psr.next(); pB = psr.next(); pC = psr.next()
            PT = ptp()
            slot = {}
            for idx, (t, kt, Kp, val) in enumerate(tiles):
                (pbk, col) = (pA, idx) if idx < 4 else (pB, idx - 4)
                slot[t] = (pbk, col)
                em.op("pe", lambda e, pbk=pbk, col=col, kt=kt, Kp=Kp: e.matmul(pbk[0:Kp, 128 * col:128 * col + 128], lhsT=kh[:, 128 * kt:128 * kt + Kp],
                                                                             rhs=qh[:, q0:q0 + 128], start=True, stop=True),
                      reads=[QK], writes=[pbk])
            nb = len(tiles)
            for cc in range(2):
                idx = nb + cc
                (pbk, col) = (pA, idx) if idx < 4 else (pB, idx - 4)
                em.op("pe", lambda e, pbk=pbk, col=col, cc=cc: e.matmul(pbk[:, 128 * col:128 * col + 128], lhsT=kh[:, L + 128 * cc:L + 128 * cc + 128],
                                                                      rhs=qh[:, q0:q0 + 128], start=True, stop=True),
                      reads=[QK], writes=[pbk])
            ntot = nb + 2
            Ks = [tl[2] for tl in tiles] + [128, 128]
            for idx in range(ntot):
                (pbk, col) = (pA, idx) if idx < 4 else (pB, idx - 4)
                Kp = Ks[idx]
                em.op("act", lambda e, pbk=pbk, col=col, idx=idx, Kp=Kp: e.activation(out=PT[0:Kp, idx, :], in_=pbk[0:Kp, 128 * col:128 * col + 128],
                                                                                      func=AF.Exp, scale=scale),
                      reads=[pbk], writes=[PT])
            for idx, (t, kt, Kp, val) in enumerate(tiles):
                for hf in range(2):
                    if hf == 1 and Kp == 64:
                        continue
                    rk = u + 2 * t + hf
                    dd0 = 7 - rk + 2 * i
                    rows = slice(64 * hf, 64 * hf + 64)
                    v0, v1 = val[hf]
                    if v0 and v1:
                        em.op("dve", lambda e, idx=idx, rows=rows, dd0=dd0: e.tensor_tensor(
                            out=PT[rows, idx, :].rearrange("p (a b) -> p a b", a=2), in0=PT[rows, idx, :].rearrange("p (a b) -> p a b", a=2),
                            in1=E[rows, h, dd0:dd0 + 2, :], op=ALU.mult), reads=[PT, E], writes=[PT])
                    else:
                        for e_ in range(2):
                            if val[hf][e_]:
                                em.op("dve", lambda e, idx=idx, rows=rows, dd0=dd0, e_=e_: e.tensor_tensor(
                                    out=PT[rows, idx, 64 * e_:64 * e_ + 64], in0=PT[rows, idx, 64 * e_:64 * e_ + 64],
                                    in1=E[rows, h, dd0 + e_, :], op=ALU.mult), reads=[PT, E], writes=[PT])
                            else:
                                em.op("dve", lambda e, idx=idx, rows=rows, e_=e_: e.memset(PT[rows, idx, 64 * e_:64 * e_ + 64], 0.0), writes=[PT])
            kts = [tl[1] for tl in tiles] + [32, 33]
            for idx in range(ntot):
                Kp = Ks[idx]
                em.op("pe", lambda e, idx=idx, Kp=Kp: e.matmul(pC[:, 0:128], lhsT=vna[0:Kp, kts[idx], h, :], rhs=PT[0:Kp, idx, :],
                                                            start=(idx == 0), stop=(idx == ntot - 1)), reads=[VO, PT], writes=[pC])
            for idx in range(ntot):
                Kp = Ks[idx]
                em.op("pe", lambda e, idx=idx, Kp=Kp: e.matmul(pC[:, 128:256], lhsT=cst.ones_bf[0:Kp, :], rhs=PT[0:Kp, idx, :],
                                                            start=(idx == 0), stop=(idx == ntot - 1)), reads=[cst.ones_bf, PT], writes=[pC])
            rd = rdp()
            em.op("dve", lambda e, rd=rd: e.reciprocal(out=rd[:, 0:128], in_=pC[:, 128:256]), reads=[pC], writes=[rd])
            no = nop()
            em.op("dve", lambda e, rd=rd, no=no: e.tensor_tensor(out=no[:, 0:128], in0=pC[:, 0:128], in1=rd[:, 0:128], op=ALU.mult),
                  reads=[pC, rd], writes=[no])
            store_mix(h, q0, 128, no[:, 0:128], no)
        pA = psr.next(); pC = psr.next()
        PT = ptp()
        PTc = PT[:, 0:4, :].rearrange("p (c x) q -> p c (x q)", c=2)
        for cc in range(2):
            em.op("pe", lambda e, cc=cc: e.matmul(pA[:, 256 * cc:256 * cc + 256], lhsT=kh[:, L + 128 * cc:L + 128 * cc + 128], rhs=qh[:, L:L + 256],
                                                 start=True, stop=True), reads=[QK], writes=[pA])
        em.op("act", lambda e: e.activation(out=PTc, in_=pA[:, 0:512].rearrange("p (c q) -> p c q", c=2), func=AF.Exp, scale=scale),
              reads=[pA], writes=[PT])
        for cc in range(2):
            em.op("pe", lambda e, cc=cc: e.matmul(pC[:, 0:256], lhsT=vna[:, 32 + cc, h, :], rhs=PTc[:, cc, :], start=(cc == 0), stop=(cc == 1)),
                  reads=[VO, PT], writes=[pC])
        for cc in range(2):
            em.op("pe", lambda e, cc=cc: e.matmul(pC[:, 256:512], lhsT=cst.ones_bf[:], rhs=PTc[:, cc, :], start=(cc == 0), stop=(cc == 1)),
                  reads=[cst.ones_bf, PT], writes=[pC])
        rd = rdp()
        em.op("dve", lambda e, rd=rd: e.reciprocal(out=rd[:], in_=pC[:, 256:512]), reads=[pC], writes=[rd])
        no = nop()
        em.op("dve", lambda e, rd=rd, no=no: e.tensor_tensor(out=no[:], in0=pC[:, 0:256], in1=rd[:], op=ALU.mult), reads=[pC, rd], writes=[no])
        store_mix(h, L, 256, no[:], no)

    em.dma("pool", W[:], wm[:].rearrange("(k p) f -> p k f", p=128), W, wm)
    em.op("pool", lambda e: e.memset(mV[:, :, 256:257], 1.0), writes=[mV])
    csp = em.pool_("cs_sb", [128, 2, 256], F32, 2)
    g4p = em.pool_("g4", [128, 4], F32, 4)
    kscale = 128 ** -0.5
    for (s, n) in tok_tiles:
        is_c = s >= L
        hx = hxp()
        load_hx(hx, s, n)
        if not is_c:
            cs = csp()
            em.dma("sp", cs[:, 0, :], cosd[:, s:s + n], cs, cosd)
            em.dma("sp", cs[:, 1, :], sind[:, s:s + n], cs, sind)
        for (c, dst, sc) in ((0, mQ, 1.0), (1, mK, kscale)):
            pb = psr.next()
            mm_acc(em, pb, pb[:, 0:n], [(W[:, k, 128 * c:128 * c + 128], hx[:, k, :]) for k in range(KC)], [W, hx])
            if is_c:
                em.op("act", lambda e, pb=pb, dst=dst, sc=sc: e.activation(out=dst[:, s:s + n], in_=pb[:, 0:n], func=AF.Copy, scale=sc),
                      reads=[pb], writes=[dst])
            else:
                pbs = psr.next()
                mm_acc(em, pbs, pbs[:, 0:n], [(W[:, k, 256 + 128 * c:256 + 128 * c + 128], hx[:, k, :]) for k in range(KC)], [W, hx])
                t1 = t256(); t2 = t256()
                em.op("dve", lambda e, pb=pb, t1=t1, sc=sc, cs=cs: e.scalar_tensor_tensor(out=t1[:], in0=pb[:, 0:n], scalar=sc, in1=cs[:, 0, :],
                                                                                       op0=ALU.mult, op1=ALU.mult), reads=[pb, cs], writes=[t1])
                em.op("dve", lambda e, pbs=pbs, t2=t2, sc=sc, cs=cs: e.scalar_tensor_tensor(out=t2[:], in0=pbs[:, 0:n], scalar=sc, in1=cs[:, 1, :],
                                                                                         op0=ALU.mult, op1=ALU.mult), reads=[pbs, cs], writes=[t2])
                em.op("pool", lambda e, t1=t1, t2=t2, dst=dst: e.tensor_tensor(out=dst[:, s:s + n], in0=t1[:], in1=t2[:], op=ALU.add),
                      reads=[t1, t2], writes=[dst])
        for c in range(2):
            pb = psr.next()
            mm_acc(em, pb, pb[:, 0:n], [(W[:, k, 768 + 128 * c:768 + 128 * c + 128], hx[:, k, :]) for k in range(KC)], [W, hx])
            em.op("act", lambda e, pb=pb, c=c: e.activation(out=sigo[:, c, s:s + n], in_=pb[:, 0:n], func=AF.Sigmoid), reads=[pb], writes=[VO])
        for j in range(n // 128):
            tt = s // 128 + j
            pb = psr.next()
            mm_acc(em, pb, pb[:, 0:256], [(hx[:, k, 128 * j:128 * j + 128], W[:, k, 512:768]) for k in range(KC)], [W, hx])
            em.op("act", lambda e, pb=pb, tt=tt: e.activation(out=mV[:, tt, 0:256], in_=pb[:, 0:256], func=AF.Copy), reads=[pb], writes=[mV])
            pg = psr.next()
            mm_acc(em, pg, pg[:, 0:4], [(hx[:, k, 128 * j:128 * j + 128], W[:, k, 1024:1028]) for k in range(KC)], [W, hx])
            gp_ = g4p(); ge = g4p()
            em.op("dve", lambda e, pg=pg, gp_=gp_: e.tensor_tensor(out=gp_[:], in0=pg[:, 0:4], in1=gb[:], op=ALU.add), reads=[pg, gb], writes=[gp_])
            em.op("act", lambda e, gp_=gp_, ge=ge: e.activation(out=ge[:], in_=gp_[:], func=AF.Exp, scale=-1.0), reads=[gp_], writes=[ge])
            em.op("act", lambda e, ge=ge: e.activation(out=ge[:], in_=ge[:], func=AF.Ln, bias=cst.one[:, 0:1], scale=1.0), reads=[ge, cst.one], writes=[ge])
            for col in (0, 2):
                em.op("pool", lambda e, gp_=gp_, tt=tt, col=col: e.tensor_copy(out=G[:, tt, col:col + 1], in_=gp_[:, col:col + 1]), reads=[gp_], writes=[G])
            for col in (1, 3):
                em.op("dve", lambda e, ge=ge, tt=tt, col=col: e.tensor_scalar(out=G[:, tt, col:col + 1], in0=ge[:, col:col + 1], scalar1=-1.0, scalar2=None,
                                                                            op0=ALU.mult), reads=[ge], writes=[G])

    triF = em.sb("triF", [128, 128], F32); triB = em.sb("triB", [128, 128], F32)
    mskF = em.sb("mskF", [128, 128], F32); mskB = em.sb("mskB", [128, 128], F32)
    for (t_, init, pat, cmul, fill) in ((triF, 1.0, [[1, 128]], -1, 0.0), (triB, 1.0, [[-1, 128]], 1, 0.0),
                                        (mskF, 0.0, [[1, 128]], -1, MASKNEG), (mskB, 0.0, [[-1, 128]], 1, MASKNEG)):
        em.op("pool", lambda e, t_=t_, init=init: e.memset(t_[:], init), writes=[t_])
        em.op("pool", lambda e, t_=t_, pat=pat, cmul=cmul, fill=fill: e.affine_select(out=t_[:], in_=t_[:], pattern=pat, compare_op=ALU.is_ge,
                                                                                   fill=fill, base=0, channel_multiplier=cmul),
              reads=[t_], writes=[t_])
    Cn = em.sb("Cn", [128, 257], F32)
    f128 = em.pool_("f128", [128, 128], F32, 8)
    bf128 = em.pool_("bf128", [128, 128], BF16, 8)
    colp = em.pool_("colp", [128, 1], F32, 12)
    cbp = em.pool_("Cb", [128, 256], BF16, 2)
    for d_ in range(2):
        tri, msk = (triF, mskF) if d_ == 0 else (triB, mskB)
        last = 127 if d_ == 0 else 0
        em.op("pool", lambda e: e.memset(Cn[:], 0.0), writes=[Cn])
        order = [32, 33] + list(range(32)) if d_ == 0 else [33, 32] + list(range(31, -1, -1))
        for tt in order:
            ts_ = slice(128 * tt, 128 * tt + 128)
            icol = G[:, tt, 2 * d_:2 * d_ + 1]
            lf = G[:, tt, 2 * d_ + 1:2 * d_ + 2]
            p1 = psr.next(); p2 = psr.next(); p3 = psr.next(); p4 = psr.next()
            em.op("pe", lambda e, p1=p1: e.matmul(p1[:, 0:1], lhsT=tri[:], rhs=lf, start=True, stop=True), reads=[tri, G], writes=[p1])
            lfb = f128()
            em.op("dve", lambda e, lfb=lfb: e.tensor_scalar(out=lfb[:], in0=cst.ones_f[:], scalar1=lf, scalar2=None, op0=ALU.mult),
                  reads=[cst.ones_f, G], writes=[lfb])
            em.op("pe", lambda e, p2=p2, lfb=lfb: e.matmul(p2[:, 0:128], lhsT=lfb[:], rhs=tri[:], start=True, stop=True), reads=[lfb, tri], writes=[p2])
            imb = colp(); blast = colp(); wcol = colp()
            em.op("dve", lambda e, imb=imb, p1=p1: e.tensor_tensor(out=imb[:], in0=icol, in1=p1[:, 0:1], op=ALU.subtract), reads=[G, p1], writes=[imb])
            dl = f128()
            em.op("dve", lambda e, dl=dl, p2=p2, imb=imb: e.scalar_tensor_tensor(out=dl[:], in0=p2[:, 0:128], scalar=imb[:, 0:1], in1=msk[:],
                                                                               op0=ALU.add, op1=ALU.add), reads=[p2, imb, msk], writes=[dl])
            em.op("act", lambda e, dl=dl: e.activation(out=dl[:], in_=dl[:], func=AF.Exp), reads=[dl], writes=[dl])
            A = f128()
            em.op("act", lambda e, A=A, p2=p2: e.activation(out=A[:], in_=p2[:, 0:128], func=AF.Exp), reads=[p2], writes=[A])
            em.op("dve", lambda e, blast=blast, p2=p2: e.tensor_copy(out=blast[:], in_=p2[:, last:last + 1]), reads=[p2], writes=[blast])
            em.op("act", lambda e, wcol=wcol, imb=imb, blast=blast: e.activation(out=wcol[:], in_=imb[:], func=AF.Exp, bias=blast[:, 0:1], scale=1.0),
                  reads=[imb, blast], writes=[wcol])
            em.op("pe", lambda e, p3=p3: e.matmul(p3[:, 0:128], lhsT=mK[:, ts_], rhs=mQ[:, ts_], start=True, stop=True), reads=[mK, mQ], writes=[p3])
            PTm = bf128()
            em.op("dve", lambda e, PTm=PTm, p3=p3, dl=dl: e.tensor_tensor(out=PTm[:], in0=p3[:, 0:128], in1=dl[:], op=ALU.mult), reads=[p3, dl], writes=[PTm])
            Qa = bf128()
            em.op("pool", lambda e, Qa=Qa, A=A: e.tensor_tensor(out=Qa[:], in0=mQ[:, ts_], in1=A[:], op=ALU.mult), reads=[mQ, A], writes=[Qa])
            Cb = cbp(); Nb = bf128()
            em.op("act", lambda e, Cb=Cb: e.activation(out=Cb[:], in_=Cn[:, 0:256], func=AF.Copy), reads=[Cn], writes=[Cb])
            em.op("pool", lambda e, Nb=Nb: e.tensor_copy(out=Nb[:], in_=Cn[:, 256:257].to_broadcast([128, 128])), reads=[Cn], writes=[Nb])
            for j in range(2):
                em.op("pe", lambda e, j=j, PTm=PTm: e.matmul(p4[:, 128 * j:128 * j + 128], lhsT=mV[:, tt, 128 * j:128 * j + 128], rhs=PTm[:], start=True, stop=False),
                      reads=[mV, PTm], writes=[p4])
                em.op("pe", lambda e, j=j, Cb=Cb, Qa=Qa: e.matmul(p4[:, 128 * j:128 * j + 128], lhsT=Cb[:, 128 * j:128 * j + 128], rhs=Qa[:], start=False, stop=True),
                      reads=[Cb, Qa], writes=[p4])
            em.op("pe", lambda e, PTm=PTm: e.matmul(p4[:, 256:384], lhsT=cst.ones_bf[:], rhs=PTm[:], start=True, stop=False), reads=[cst.ones_bf, PTm], writes=[p4])
            em.op("pe", lambda e, Nb=Nb, Qa=Qa: e.matmul(p4[:, 256:384], lhsT=Nb[:], rhs=Qa[:], start=False, stop=True), reads=[Nb, Qa], writes=[p4])
            rd = f128()
            em.op("act", lambda e, rd=rd: e.activation(out=rd[:], in_=p4[:, 256:384], func=AF.Abs), reads=[p4], writes=[rd])
            em.op("dve", lambda e, rd=rd: e.tensor_scalar(out=rd[:], in0=rd[:], scalar1=1.0, scalar2=None, op0=ALU.max), reads=[rd], writes=[rd])
            em.op("dve", lambda e, rd=rd: e.reciprocal(out=rd[:], in_=rd[:]), reads=[rd], writes=[rd])
            for j in range(2):
                if d_ == 0:
                    em.op("dve", lambda e, j=j, rd=rd: e.tensor_tensor(out=hsum[:, j, ts_], in0=p4[:, 128 * j:128 * j + 128], in1=rd[:], op=ALU.mult),
                          reads=[p4, rd], writes=[QK])
                else:
                    hb = f128()
                    em.op("dve", lambda e, j=j, rd=rd, hb=hb: e.tensor_tensor(out=hb[:], in0=p4[:, 128 * j:128 * j + 128], in1=rd[:], op=ALU.mult),
                          reads=[p4, rd], writes=[hb])
                    em.op("pool", lambda e, j=j, hb=hb: e.tensor_tensor(out=hsum[:, j, ts_], in0=hsum[:, j, ts_], in1=hb[:], op=ALU.add),
                          reads=[hb, QK], writes=[QK])
            em.op("pe", lambda e: e.transpose(ptr[:], mK[:, ts_], cst.ident_bf[:]), reads=[mK, cst.ident_bf], writes=[ptr])
            Kw = bf128()
            em.op("act", lambda e, Kw=Kw, wcol=wcol: e.activation(out=Kw[:], in_=ptr[:], func=AF.Copy, scale=wcol[:, 0:1]), reads=[ptr, wcol], writes=[Kw])
            p5 = psr.next()
            em.op("pe", lambda e, p5=p5, Kw=Kw: e.matmul(p5[:, 0:257], lhsT=Kw[:], rhs=mV[:, tt, :], start=True, stop=True), reads=[Kw, mV], writes=[p5])
            em.op("dve", lambda e, p5=p5, A=A: e.scalar_tensor_tensor(out=Cn[:], in0=Cn[:], scalar=A[:, last:last + 1], in1=p5[:, 0:257],
                                                                    op0=ALU.mult, op1=ALU.add), reads=[Cn, A, p5], writes=[Cn])

    t512 = em.pool_("t512", [128, 512], F32, 4)
    b512 = em.pool_("b512", [128, 512], BF16, 4)
    for (s, n) in tiles_of(TB, 512):
        pb = psr.next()
        for j in range(2):
            sq = b512()
            em.op("act", lambda e, sq=sq, j=j: e.activation(out=sq[:, 0:n], in_=hsum[:, j, s:s + n], func=AF.Square), reads=[QK], writes=[sq])
            em.op("pe", lambda e, sq=sq, j=j: e.matmul(pb[:, 0:n], lhsT=cst.ones_bf[:], rhs=sq[:, 0:n], start=(j == 0), stop=(j == 1)),
                  reads=[sq, cst.ones_bf], writes=[pb])
        r = t512()
        em.op("act", lambda e, r=r: e.activation(out=r[:, 0:n], in_=pb[:, 0:n], func=AF.Sqrt, bias=cst.eps[:, 0:1], scale=1.0 / 256), reads=[pb, cst.eps], writes=[r])
        em.op("dve", lambda e, r=r: e.reciprocal(out=r[:, 0:n], in_=r[:, 0:n]), reads=[r], writes=[r])
        for j in range(2):
            tm = t512()
            em.op("dve", lambda e, tm=tm, j=j, r=r: e.scalar_tensor_tensor(out=tm[:, 0:n], in0=hsum[:, j, s:s + n], scalar=mng[:, j:j + 1], in1=r[:, 0:n],
                                                                         op0=ALU.mult, op1=ALU.mult), reads=[QK, mng, r], writes=[tm])
            ob = b512()
            em.op("pool", lambda e, tm=tm, j=j, ob=ob: e.tensor_tensor(out=ob[:, 0:n], in0=tm[:, 0:n], in1=sigo[:, j, s:s + n], op=ALU.mult),
                  reads=[tm, VO], writes=[ob])
            store_mix(2 + j, s, n, ob[:, 0:n], ob)


def rope_tables():
    t = np.arange(L)
    row = (t // 64).astype(np.float32)
    col = (t % 64).astype(np.float32)
    inv = (np.float32(10000.0) ** (-np.arange(32, dtype=np.float32) / np.float32(32))).astype(np.float32)
    ang = np.concatenate([row[:, None] * inv, col[:, None] * inv], axis=-1).astype(np.float32)
    cos = np.cos(ang).astype(np.float32)
    sin = np.sin(ang).astype(np.float32)
    cosT = np.repeat(cos.T, 2, axis=0)
    sinT = np.repeat(sin.T, 2, axis=0)
    sign = np.where(np.arange(128) % 2 == 0, -1.0, 1.0).astype(np.float32)[:, None]
    return np.ascontiguousarray(cosT), np.ascontiguousarray(sinT * sign)


def col_mask():
    q = np.arange(64)
    cs = np.clip(q - 8, 0, 48)
    k = np.arange(64)
    ok = (k[:, None] >= cs[None, :]) & (k[:, None] < cs[None, :] + 16)
    return ok.astype(np.float32)


def na_bias_table(rpb_h):
    k = np.arange(64)
    dc = np.clip(k[:, None] - k[None, :], -15, 15) + 15
    dr = 14 - np.arange(15)
    return np.ascontiguousarray(rpb_h[dr[None, :, None], dc[:, None, :]])


_SWAP = np.arange(128) ^ 1


def b_inputs(l, b, g, hx_batch, w_in, gate_b, na_q_g, na_k_g, na_rpb, m_norm_g, consts):
    cosT, sinT, cmask = consts
    wl = w_in[l]
    na_cols = np.concatenate([np.arange(o + 256 * g, o + 256 * g + 256) for o in (0, 1024, 2048)])
    mq = 3072 + 128 * g + np.arange(128)
    mk = 3584 + 128 * g + np.arange(128)
    mv = 4096 + 256 * g + np.arange(256)
    mo = 5120 + 256 * g + np.arange(256)
    mg = 6144 + np.array([g, 4 + g, 8 + g, 12 + g])
    m_cols = np.concatenate([mq, mk, mq[_SWAP], mk[_SWAP], mv, mo, mg])
    nab = np.stack([na_bias_table(na_rpb[l, 2 * g + h]) for h in range(2)])
    return {
        "hxT": hx_batch,
        "wna": np.ascontiguousarray(wl[:, na_cols]), "wm": np.ascontiguousarray(wl[:, m_cols]),
        "cosT": cosT, "sinT": sinT, "nab": nab, "cmask": cmask,
        "nqg": np.ascontiguousarray(na_q_g[l].reshape(128, 1)), "nkg": np.ascontiguousarray(na_k_g[l].reshape(128, 1)),
        "mng": np.ascontiguousarray(m_norm_g[l, 256 * g:256 * g + 256].reshape(2, 128).T),
        "gb": np.ascontiguousarray(np.broadcast_to(gate_b[l, [g, 4 + g, 8 + g, 12 + g]][None, :], (128, 4))),
    }


def _batch_hx(hx_cores, b):
    xs = [hx_cores[4 * b + j][:, :, :1024] for j in range(4)]
    cs = [hx_cores[4 * b + j][:, :, 1024:] for j in range(4)]
    return np.ascontiguousarray(np.concatenate(xs + cs, axis=2))


def _mix_for_core(mix_b, j):
    out = np.empty((128, KC, TOK), dtype=mix_b[0].dtype)
    for g in range(4):
        m = mix_b[g]
        for ci, dst in enumerate((2 * g, 2 * g + 1, 8 + 2 * g, 9 + 2 * g)):
            out[:, dst, :1024] = m[:, ci, 1024 * j:1024 * j + 1024]
            out[:, dst, 1024:] = m[:, ci, L + 64 * j:L + 64 * j + 64]
    return out


def kernel_unfused(x, c, ctx, c_ctx, ada_w, ada_b, norm1_g, norm2_g, w_in, gate_b, na_q_g, na_k_g, na_rpb, m_norm_g, w_out,
           ffn_w_gate, ffn_w_up, ffn_w_down, moe_router, moe_w_gate, moe_w_up, moe_w_down):
    f32 = np.float32
    x = np.asarray(x, f32); ctx = np.asarray(ctx, f32)
    mods = host_M(np.asarray(c, f32), np.asarray(c_ctx, f32), np.asarray(ada_w, f32), np.asarray(ada_b, f32))
    xs = shard_tokens(x, ctx)
    res = _run(_prog("A0", build_A0), [{"xT": xs[i], "mod": mod_for_core(mods[0], i // 4), "g1": fm(norm1_g[0])} for i in range(8)])
    hx = [r["hxT"] for r in res]
    consts = (*rope_tables(), col_mask())
    ones_row = np.ones((1, NTOK_ALL), f32)
    for l in range(DEPTH):
        hxb = [_batch_hx(hx, b) for b in range(B)]
        res = _run(_prog("B", build_B), [b_inputs(l, i // 4, i % 4, hxb[i // 4], w_in, gate_b, na_q_g, na_k_g, na_rpb, m_norm_g, consts)
                                         for i in range(8)])
        mix = [r["mixT"] for r in res]
        moe = (l % 2 == 1)
        idx = l // 2
        router = fm(np.asarray(moe_router[idx], f32).T) if moe else np.zeros((128, KC, NE), f32)
        wo = np.asarray(w_out[l], f32)
        n2g = fm(norm2_g[l])
        res = _run(_prog("C", build_C), [{"xT": xs[i], "mixT": _mix_for_core(mix[4 * (i // 4):4 * (i // 4) + 4], i % 4), "wout": wo,
                                          "mod": mod_for_core(mods[l], i // 4), "n2g": n2g, "router": router} for i in range(8)])
        xs = [r["xo"] for r in res]
        h2_all = np.ascontiguousarray(np.concatenate([r["h2T"] for r in res], axis=2))
        if moe:
            gates = np.concatenate([r["gates"].transpose(1, 0, 2).reshape(NT128 * 128, NE)[:TOK] for r in res], axis=0)
            in_maps = [{"h2T": h2_all, "grow": np.ascontiguousarray(gates[:, e][None, :]),
                        "wg": np.asarray(moe_w_gate[idx, e], f32), "wu": np.asarray(moe_w_up[idx, e], f32),
                        "wd": np.asarray(moe_w_down[idx, e], f32)} for e in range(NE)]
            res = _run(_prog("D", build_D, DFF, 4), in_maps)
        else:
            in_maps = []
            for e in range(NE):
                wg = np.zeros((D, 768), f32); wu = np.zeros((D, 768), f32); wd = np.zeros((768, D), f32)
                wg[:, :704] = ffn_w_gate[idx][:, 704 * e:704 * e + 704]
                wu[:, :704] = ffn_w_up[idx][:, 704 * e:704 * e + 704]
                wd[:704] = ffn_w_down[idx][704 * e:704 * e + 704]
                in_maps.append({"h2T": h2_all, "grow": ones_row, "wg": wg, "wu": wu, "wd": wd})
            res = _run(_prog("D", build_D, 768, 3), in_maps)
        ys = [r["yT"] for r in res]
        ln = min(l + 1, DEPTH - 1)
        in_maps = [{"xT": xs[i], "yp": np.ascontiguousarray(np.stack([ys[e][:, :, TOK * i:TOK * i + TOK] for e in range(NE)])),
                    "mod": mod_for_core(mods[l], i // 4), "modn": mod_for_core(mods[ln], i // 4), "g1n": fm(norm1_g[ln])} for i in range(8)]
        res = _run(_prog("E", build_E), in_maps)
        xs = [r["xo"] for r in res]
        hx = [r["hxT"] for r in res]
    out = np.empty((B, L, D), f32)
    for i in range(8):
        b, j = i // 4, i % 4
        out[b, 1024 * j:1024 * j + 1024] = xs[i][:, :, :1024].transpose(2, 1, 0).reshape(1024, D)
    return out


G4 = [[0, 1, 2, 3], [4, 5, 6, 7]]
NCH = 24
FD = 1536


def view(buf, ap):
    b = Buf(buf.name + "_v", ap)
    b.scoped = False
    return b


class AGC:
    KG = 3

    def __init__(self, em, name):
        self.em = em
        self.chunks = []
        for k0 in range(0, KC, self.KG):
            nk = min(self.KG, KC - k0)
            part = em.dram(f"{name}_p{k0}", [128, nk * TOK], BF16)
            allb = em.dram(f"{name}_a{k0}", [512, nk * TOK], BF16)
            self.chunks.append((k0, nk, part, allb, allb.t.rearrange("(r p) (k t) -> p r k t", p=128, k=nk)))

    def store(self, h):
        for (k0, nk, part, allb, v) in self.chunks:
            self.em.dma("sp", part[:].rearrange("p (k t) -> p k t", k=nk), h[:, k0:k0 + nk, :], part, h)

    def gather(self):
        for (k0, nk, part, allb, v) in self.chunks:
            self.em.cc("AllGather", ALU.bypass, G4, allb[:], part[:], allb, part)

    def load(self, dst, dst_ap_fn, r, a, n):
        for (k0, nk, part, allb, v) in self.chunks:
            self.em.dma("sp", dst_ap_fn(k0, nk), v[:, r, :, a:a + n], dst, allb)


def flat_pieces(T0, n):
    out = []
    t = T0
    while t < T0 + n:
        r = t // TOK
        a = t % TOK
        ln = min(TOK - a, T0 + n - t)
        out.append((r, a, t - T0, ln))
        t += ln
    return out


def f_load_mod(em, mod_sb, l, modsel, bidx=None):
    em.dma("sp", mod_sb[:], modsel[:, :, l, :], mod_sb, modsel)


def f_select_mod(em, modsel, mod_all, bidx):
    src = mod_all.t.rearrange("(r p) (c l j) -> p c l r j", p=128, c=3, l=DEPTH)
    for l in range(DEPTH):
        em.dma("sp", modsel[:, 1:2, l, :].rearrange("p o (r j) -> p o r j", r=4), src[:, 2:3, l, :, :], modsel, mod_all)
    dsel = src[:, bass.ts(bidx, 1), :, :, :]
    for l in range(DEPTH):
        em.dma("sp", modsel[:, 0:1, l, :].rearrange("p o (r j) -> p o r j", r=4), dsel[:, :, l, :, :], modsel, mod_all)


def f_C(em, cst, psr, l, xsrc, mix_all, wout_l, modsel, n2g_l, router_l, xres, h2_part, gT_part, jr, bidx):
    x = em.sb("x", [128, KC, TOK], F32)
    mix = em.sb("mix", [128, KC, TOK], BF16)
    h2 = em.sb("h2", [128, KC, TOK], BF16)
    mod = em.sb("modsb", [128, 2, 96], F32)
    n2g = em.sb("n2gsb", [128, KC], F32)
    par = em.sb("par", [128, 4, KC], F32)
    em.dma("sp", x[:], xsrc[:], x, xsrc)
    em.dma("sp", mix[:], mix_all[:].rearrange("p (k t) -> p k t", k=KC), mix, mix_all)
    f_load_mod(em, mod, l, modsel)
    em.dma("sp", n2g[:], n2g_l[:], n2g, n2g_l)
    wp = em.pool_("wout_sb", [128, KC, 512], BF16, 2)
    for cg in range(4):
        w = wp()
        em.dma("pool", w[:], wout_l[:, 512 * cg:512 * cg + 512].rearrange("(k p) f -> p k f", p=128), w, wout_l)
        for dc in range(4):
            d = 4 * cg + dc
            for (s, n, is_c) in TOK_TILES:
                pb = psr.next()
                mm_acc(em, pb, pb[:, 0:n], [(w[:, k, 128 * dc:128 * dc + 128], mix[:, k, s:s + n]) for k in range(KC)], [w, mix])
                gcol = mod[:, 1 if is_c else 0, 32 + d:32 + d + 1]
                em.op("dve", lambda e, pb=pb, d=d, s=s, n=n, gcol=gcol: e.scalar_tensor_tensor(
                    out=x[:, d, s:s + n], in0=pb[:, 0:n], scalar=gcol, in1=x[:, d, s:s + n], op0=ALU.mult, op1=ALU.add),
                    reads=[pb, mod, x], writes=[x])
    em.dma("sp", xres[:], x[:], xres, x)
    load_par(em, par, mod, n2g, 3, 4)
    if router_l is None:
        norm_mod(em, cst, psr, x, h2, par, TOK_TILES, norm_pools(em))
    else:
        rout = em.sb("routsb", [128, KC, NE], F32)
        em.dma("sp", rout[:], router_l[:], rout, router_l)
        gates = em.sb("gatessb", [128, NE], F32)
        gT = em.sb("gTsb", [NE, NT128 * 128], F32)
        small = em.pool_("rt_small", [128, 8], F32, 12)
        lgp = em.pool_("rt_lg", [128, NE], F32, 3)

        def router_cb(ti, s, n, hf):
            for j in range(0, n, 128):
                m = min(128, n - j)
                t128 = (s + j) // 128
                pb = psr.next()
                mm_acc(em, pb, pb[0:m, 0:NE], [(hf[:, k, j:j + m], rout[:, k, :]) for k in range(KC)], [hf, rout])
                lg = lgp()
                em.op("act", lambda e: e.activation(out=lg[0:m, :], in_=pb[0:m, 0:NE], func=AF.Copy), reads=[pb], writes=[lg])
                m1 = small(); eq = small(); lg2 = small(); m2 = small(); sel = small(); nm1 = small(); ex = small(); den = small()
                em.op("dve", lambda e: e.reduce_max(out=m1[0:m, 0:1], in_=lg[0:m, :], axis=AX.X), reads=[lg], writes=[m1])
                em.op("dve", lambda e: e.tensor_scalar(out=eq[0:m, :], in0=lg[0:m, :], scalar1=m1[0:m, 0:1], scalar2=None, op0=ALU.is_equal),
                      reads=[lg, m1], writes=[eq])
                em.op("dve", lambda e: e.scalar_tensor_tensor(out=lg2[0:m, :], in0=eq[0:m, :], scalar=-1e30, in1=lg[0:m, :], op0=ALU.mult, op1=ALU.add),
                      reads=[eq, lg], writes=[lg2])
                em.op("dve", lambda e: e.reduce_max(out=m2[0:m, 0:1], in_=lg2[0:m, :], axis=AX.X), reads=[lg2], writes=[m2])
                em.op("dve", lambda e: e.tensor_scalar(out=sel[0:m, :], in0=lg[0:m, :], scalar1=m2[0:m, 0:1], scalar2=None, op0=ALU.is_ge),
                      reads=[lg, m2], writes=[sel])
                em.op("dve", lambda e: e.tensor_scalar(out=nm1[0:m, 0:1], in0=m1[0:m, 0:1], scalar1=-1.0, scalar2=None, op0=ALU.mult),
                      reads=[m1], writes=[nm1])
                em.op("act", lambda e: e.activation(out=ex[0:m, :], in_=lg[0:m, :], func=AF.Exp, bias=nm1[0:m, 0:1], scale=1.0),
                      reads=[lg, nm1], writes=[ex])
                em.op("dve", lambda e: e.tensor_tensor(out=ex[0:m, :], in0=ex[0:m, :], in1=sel[0:m, :], op=ALU.mult), reads=[ex, sel], writes=[ex])
                em.op("dve", lambda e: e.reduce_sum(out=den[0:m, 0:1], in_=ex[0:m, :], axis=AX.X), reads=[ex], writes=[den])
                em.op("dve", lambda e: e.reciprocal(out=den[0:m, 0:1], in_=den[0:m, 0:1]), reads=[den], writes=[den])
                em.op("dve", lambda e: e.tensor_scalar(out=gates[0:m, :], in0=ex[0:m, :], scalar1=den[0:m, 0:1], scalar2=None, op0=ALU.mult),
                      reads=[ex, den], writes=[gates])
                pt = psr.next()
                em.op("pe", lambda e: e.matmul(pt[0:NE, 0:m], lhsT=gates[0:m, :], rhs=cst.ident_f[0:m, 0:m], start=True, stop=True),
                      reads=[gates, cst.ident_f], writes=[pt])
                em.op("act", lambda e: e.activation(out=gT[:, 128 * t128:128 * t128 + m], in_=pt[0:NE, 0:m], func=AF.Copy), reads=[pt], writes=[gT])

        c_tiles = [(0, 256, False), (256, 256, False), (512, 256, False), (768, 256, False), (1024, 64, True)]
        norm_mod(em, cst, psr, x, h2, par, c_tiles, norm_pools(em, with_hf=True), hf_cb=router_cb)
        em.dma("sp", gT_part[:], gT[:, 0:TOK], gT_part, gT)
    if isinstance(h2_part, AGC):
        h2_part.store(h2)
    else:
        em.dma("sp", h2_part[:], h2[:], h2_part, h2)


D_TILES = [(121 * i, 121) for i in range(8)] + [(968, 120)]


def f_D(em, psr, subs, h2g, gT_all, gsel, yparts, ysums, jr):
    GC = 4
    hp = em.pool_("h_sb", [128, KC, 512], BF16, 2)
    gp = em.pool_("g_sb", [128, 512], F32, 2)
    wgp = em.pool_("wg_sb", [128, KC, 128 * GC], BF16, 2)
    wup = em.pool_("wu_sb", [128, KC, 128 * GC], BF16, 2)
    wdp = em.pool_("wd_sb", [128, GC, D], BF16, 2)
    actp = em.pool_("act_sb", [128, GC, 512], BF16, 2)
    sgp = em.pool_("sg_sb", [128, 512], F32, 3)
    yacc = em.sb("yacc", [128, KC, 512], F32)
    if subs[0][4]:
        for r in range(4):
            em.dma("sp", gsel.t[:, 0, TOK * r:TOK * r + TOK], gT_all.t.rearrange("(r e) t -> r e t", e=NE)[r, bass.ts(jr, 2), :], gsel, gT_all)
    pending = None
    for ti, (a, ln) in enumerate(D_TILES):
        n = 4 * ln
        h = hp()
        for r in range(4):
            h2g.load(h, lambda k0, nk, h=h, r=r: h[:, k0:k0 + nk, r * ln:(r + 1) * ln], r, a, ln)
        ngrp = 0
        prev = None

        def emit_down(wds, act, first):
            for d in range(KC):
                pd = psr.next()
                mm_acc(em, pd, pd[:, 0:n], [(wds[:, c, 128 * d:128 * d + 128], act[:, c, 0:n]) for c in range(GC)], [wds, act])
                if first:
                    em.op("act", lambda e, pd=pd, d=d: e.activation(out=yacc[:, d, 0:n], in_=pd[:, 0:n], func=AF.Copy), reads=[pd], writes=[yacc])
                else:
                    em.op("dve", lambda e, pd=pd, d=d: e.tensor_tensor(out=yacc[:, d, 0:n], in0=pd[:, 0:n], in1=yacc[:, d, 0:n], op=ALU.add),
                          reads=[pd, yacc], writes=[yacc])

        for si, (wg, wu, wd, F, gated) in enumerate(subs):
            if gated:
                g = gp()
                for r in range(4):
                    em.dma("sp", g[:, r * ln:(r + 1) * ln], gsel.t[si, 0:1, TOK * r + a:TOK * r + a + ln].partition_broadcast(128), g, gsel)
            for gi in range(F // (128 * GC)):
                wgs = wgp(); wus = wup(); wds = wdp()
                c0 = 128 * GC * gi
                em.dma("pool", wgs[:], wg[:, c0:c0 + 128 * GC].rearrange("(k p) f -> p k f", p=128), wgs, wg)
                em.dma("pool", wus[:], wu[:, c0:c0 + 128 * GC].rearrange("(k p) f -> p k f", p=128), wus, wu)
                em.dma("pool", wds[:], wd[c0:c0 + 128 * GC, :].rearrange("(c p) f -> p c f", p=128), wds, wd)
                ngrp += 1
                if ngrp == 3 and pending is not None:
                    pending()
                    pending = None
                act = actp()
                for c in range(GC):
                    pg = psr.next(); pu = psr.next()
                    mm_acc(em, pg, pg[:, 0:n], [(wgs[:, k, 128 * c:128 * c + 128], h[:, k, 0:n]) for k in range(KC)], [wgs, h])
                    mm_acc(em, pu, pu[:, 0:n], [(wus[:, k, 128 * c:128 * c + 128], h[:, k, 0:n]) for k in range(KC)], [wus, h])
                    sg = sgp()
                    em.op("act", lambda e, pg=pg, sg=sg: e.activation(out=sg[:, 0:n], in_=pg[:, 0:n], func=AF.Silu), reads=[pg], writes=[sg])
                    if gated:
                        em.op("dve", lambda e, sg=sg, g=g: e.tensor_tensor(out=sg[:, 0:n], in0=sg[:, 0:n], in1=g[:, 0:n], op=ALU.mult),
                              reads=[sg, g], writes=[sg])
                    em.op("dve", lambda e, pu=pu, sg=sg, c=c, act=act: e.tensor_tensor(out=act[:, c, 0:n], in0=pu[:, 0:n], in1=sg[:, 0:n], op=ALU.mult),
                          reads=[pu, sg], writes=[act])
                if prev is not None:
                    emit_down(*prev)
                prev = (wds, act, ngrp == 1)
        emit_down(*prev)
        yp = yparts[ti]
        ypv = yp.t.rearrange("(r p) (k t) -> p r k t", p=128, k=KC)
        for r in range(4):
            em.dma("sp", ypv[:, r, :, :], yacc[:, :, r * ln:(r + 1) * ln], yp, yacc)
        pending = (lambda yp=yp, ys=ysums[ti]: em.cc("ReduceScatter", ALU.add, G4, ys[:], yp[:], ys, yp))
    pending()


def f_Dloc(em, psr, l, wg, wu, wd, h2loc, xres, modsel):
    GC = 4
    mod = em.sb("modsb", [128, 2, 96], F32)
    f_load_mod(em, mod, l, modsel)
    hp = em.pool_("h_sb", [128, KC, 512], BF16, 1)
    xp = em.pool_("xt_sb", [128, KC, 512], F32, 2)
    wgp = em.pool_("wg_sb", [128, KC, 128 * GC], BF16, 2)
    wup = em.pool_("wu_sb", [128, KC, 128 * GC], BF16, 2)
    wdp = em.pool_("wd_sb", [128, GC, D], BF16, 2)
    actp = em.pool_("act_sb", [128, GC, 512], BF16, 2)
    sgp = em.pool_("sg_sb", [128, 512], F32, 3)
    for (s0, n) in ((0, 363), (363, 363), (726, 362)):
        h = hp()
        em.dma("sp", h[:, :, 0:n], h2loc[:, :, s0:s0 + n], h, h2loc)
        xt = xp()
        em.dma("sp", xt[:, :, 0:n], xres[:, :, s0:s0 + n], xt, xres)
        rngs = [(0, min(n, 1024 - s0), 0)] if s0 < 1024 else []
        if s0 + n > 1024:
            rngs.append((max(0, 1024 - s0), n, 1))
        for gi in range(DFF // (128 * GC)):
            wgs = wgp(); wus = wup(); wds = wdp()
            c0 = 128 * GC * gi
            em.dma("pool", wgs[:], wg[:, c0:c0 + 128 * GC].rearrange("(k p) f -> p k f", p=128), wgs, wg)
            em.dma("pool", wus[:], wu[:, c0:c0 + 128 * GC].rearrange("(k p) f -> p k f", p=128), wus, wu)
            em.dma("pool", wds[:], wd[c0:c0 + 128 * GC, :].rearrange("(c p) f -> p c f", p=128), wds, wd)
            act = actp()
            for c in range(GC):
                pg = psr.next(); pu = psr.next()
                mm_acc(em, pg, pg[:, 0:n], [(wgs[:, k, 128 * c:128 * c + 128], h[:, k, 0:n]) for k in range(KC)], [wgs, h])
                mm_acc(em, pu, pu[:, 0:n], [(wus[:, k, 128 * c:128 * c + 128], h[:, k, 0:n]) for k in range(KC)], [wus, h])
                sg = sgp()
                em.op("act", lambda e, pg=pg, sg=sg: e.activation(out=sg[:, 0:n], in_=pg[:, 0:n], func=AF.Silu), reads=[pg], writes=[sg])
                em.op("dve", lambda e, pu=pu, sg=sg, c=c, act=act: e.tensor_tensor(out=act[:, c, 0:n], in0=pu[:, 0:n], in1=sg[:, 0:n], op=ALU.mult),
                      reads=[pu, sg], writes=[act])
            for d in range(KC):
                pd = psr.next()
                mm_acc(em, pd, pd[:, 0:n], [(wds[:, c, 128 * d:128 * d + 128], act[:, c, 0:n]) for c in range(GC)], [wds, act])
                for (lo, hi, mc) in rngs:
                    em.op("dve", lambda e, pd=pd, d=d, xt=xt, lo=lo, hi=hi, mc=mc: e.scalar_tensor_tensor(
                        out=xt[:, d, lo:hi], in0=pd[:, lo:hi], scalar=mod[:, mc, 80 + d:80 + d + 1], in1=xt[:, d, lo:hi],
                        op0=ALU.mult, op1=ALU.add), reads=[pd, mod, xt], writes=[xt])
        em.dma("sp", xres[:, :, s0:s0 + n], xt[:, :, 0:n], xres, xt)


def f_E(em, cst, psr, l, xres, ysum, modsel, g1_next, xdst, hx_part, bidx, last):
    x = em.sb("x", [128, KC, TOK], F32)
    mod = em.sb("modsb", [128, 2, 96], F32)
    em.dma("sp", x[:], xres[:], x, xres)
    f_load_mod(em, mod, l, modsel)
    pp = em.pool_("yp_sb", [128, 4, TOK], F32, 2) if ysum is not None else None
    for kg in (range(4) if ysum is not None else ()):
        p = pp()
        for ti, (a, ln) in enumerate(D_TILES):
            em.dma("sp", p[:, :, a:a + ln], ysum[ti].t.rearrange("p (k t) -> p k t", k=KC)[:, 4 * kg:4 * kg + 4, :], p, ysum[ti])
        for kk in range(4):
            k = 4 * kg + kk
            for (s, n, c) in ((0, 1024, 0), (1024, 64, 1)):
                em.op("dve", lambda e, p=p, kk=kk, k=k, s=s, n=n, c=c: e.scalar_tensor_tensor(
                    out=x[:, k, s:s + n], in0=p[:, kk, s:s + n], scalar=mod[:, c, 80 + k:80 + k + 1], in1=x[:, k, s:s + n],
                    op0=ALU.mult, op1=ALU.add), reads=[p, mod, x], writes=[x])
    em.dma("sp", xdst[:], x[:], xdst, x)
    if not last:
        h = em.sb("h", [128, KC, TOK], BF16)
        modn = em.sb("modnsb", [128, 2, 96], F32)
        g1 = em.sb("g1sb", [128, KC], F32)
        par = em.sb("par", [128, 4, KC], F32)
        f_load_mod(em, modn, l + 1, modsel)
        em.dma("sp", g1[:], g1_next[:], g1, g1_next)
        load_par(em, par, modn, g1, 0, 1)
        norm_mod(em, cst, psr, x, h, par, TOK_TILES, norm_pools(em))
        hx_part.store(h)


def build_fused(nl=DEPTH):
    nc = bass.Bass("TRN2", target_bir_lowering=False)
    em = Em(nc)
    pid = nc.sync.partition_id()
    jr = pid % 4
    bidx = pid // 4
    EI = "ExternalInput"
    xT = em.dram("xT", [128, KC, TOK], F32, kind=EI)
    condT = em.dram("condT", [128, KC, 3], F32, kind=EI)
    adaw = em.dram("adaw", [nl, D, 128 * NCH], F32, kind=EI)
    adab = em.dram("adab", [128, DEPTH, NCH], F32, kind=EI)
    g1d = em.dram("g1", [DEPTH, 128, KC], F32, kind=EI)
    n2gd = em.dram("n2g", [DEPTH, 128, KC], F32, kind=EI)
    wna = em.dram("wna", [nl, D, W_NA], F32, kind=EI)
    wm = em.dram("wm", [nl, D, W_M], F32, kind=EI)
    nab = em.dram("nab", [DEPTH, 2, 64, 15, 64], F32, kind=EI)
    nqg = em.dram("nqg", [DEPTH, 128, 1], F32, kind=EI)
    nkg = em.dram("nkg", [DEPTH, 128, 1], F32, kind=EI)
    mng = em.dram("mng", [DEPTH, 128, 2], F32, kind=EI)
    gb = em.dram("gb", [DEPTH, 128, 4], F32, kind=EI)
    cosT = em.dram("cosT", [128, L], F32, kind=EI)
    sinT = em.dram("sinT", [128, L], F32, kind=EI)
    cmask = em.dram("cmask", [64, 64], F32, kind=EI)
    wout = em.dram("wout", [nl, D, D], F32, kind=EI)
    nd_, nm_ = (nl + 1) // 2, nl // 2
    dwg = em.dram("dwg", [nd_, D, DFF], F32, kind=EI)
    dwu = em.dram("dwu", [nd_, D, DFF], F32, kind=EI)
    dwd = em.dram("dwd", [nd_, DFF, D], F32, kind=EI)
    if nm_ > 0:
        router = em.dram("router", [nm_, 128, KC, NE], F32, kind=EI)
        mwg = em.dram("mwg", [nm_, 2, D, DFF], F32, kind=EI)
        mwu = em.dram("mwu", [nm_, 2, D, DFF], F32, kind=EI)
        mwd = em.dram("mwd", [nm_, 2, DFF, D], F32, kind=EI)
    xo = em.dram("xo", [128, KC, TOK], F32, kind="ExternalOutput")
    NM = 3 * DEPTH * NCH
    mod_part = em.dram("mod_part", [128, NM], F32)
    mod_all = em.dram("mod_all", [512, NM], F32)
    modsel = em.dram("modsel", [128, 2, DEPTH, 96], F32)
    hxg = AGC(em, "hx")
    mix_part = em.dram("mix_part", [128, 4, 4, TOK], BF16)
    mix_rs = em.dram("mix_rs", [512, KC * TOK], BF16)
    mix_own = em.dram("mix_own", [128, KC * TOK], BF16)
    h2g = AGC(em, "h2")
    h2loc = em.dram("h2loc", [128, KC, TOK], BF16)
    gT_part = em.dram("gT_part", [NE, TOK], F32)
    gT_all = em.dram("gT_all", [4 * NE, TOK], F32)
    gsel = em.dram("gsel", [2, 1, 4 * TOK], F32)
    yparts = [em.dram(f"ypart{i}", [512, KC * ln], F32) for i, (a_, ln) in enumerate(D_TILES)]
    ysums = [em.dram(f"ysum{i}", [128, KC * ln], F32) for i, (a_, ln) in enumerate(D_TILES)]
    xres = em.dram("xres", [128, KC, TOK], F32)
    cst = Consts(em)
    psr = PsumRing(em, 7)
    ptr = em.ps("ps_tr", [128, 128], BF16)

    em.scope_begin()
    cf = em.sb("cf", [128, KC, 3], F32)
    cb = em.sb("cb", [128, KC, 3], BF16)
    bt = em.sb("bt", [128, DEPTH, NCH], F32)
    res = em.sb("res", [128, 3, DEPTH, NCH], F32)
    em.op("pool", lambda e: e.memset(res[:], 0.0), writes=[res])
    wp = em.pool_("adaw_sb", [128, KC, 1536], BF16, 2)
    em.dma("sp", cf[:], condT[:], cf, condT)
    em.dma("sp", bt[:], adab[:], bt, adab)
    em.op("act", lambda e: e.activation(out=cb[:], in_=cf[:], func=AF.Silu), reads=[cf], writes=[cb])
    for l in range(nl):
        for hh in range(2):
            w = wp()
            em.dma("pool", w[:], adaw[l, :, 1536 * hh:1536 * hh + 1536].rearrange("(k p) f -> p k f", p=128), w, adaw)
            pb = psr.next()
            for jj in range(12):
                mm_acc(em, pb, pb[:, 3 * jj:3 * jj + 3], [(w[:, k, 128 * jj:128 * jj + 128], cb[:, k, :]) for k in range(KC)], [w, cb])
            em.op("dve", lambda e, l=l, hh=hh, pb=pb: e.tensor_tensor(
                out=res[:, :, l, 12 * hh:12 * hh + 12], in0=pb[:, 0:36].rearrange("p (j c) -> p c j", c=3),
                in1=bt[:, l, 12 * hh:12 * hh + 12].unsqueeze(1).to_broadcast([128, 3, 12]), op=ALU.add), reads=[pb, bt], writes=[res])
    em.dma("sp", mod_part[:], res[:].rearrange("p c l j -> p (c l j)"), mod_part, res)
    em.scope_end()
    em.cc("AllGather", ALU.bypass, G4, mod_all[:], mod_part[:], mod_all, mod_part)
    f_select_mod(em, modsel, mod_all, bidx)

    em.scope_begin()
    x = em.sb("x", [128, KC, TOK], F32)
    h = em.sb("h", [128, KC, TOK], BF16)
    mod = em.sb("modsb", [128, 2, 96], F32)
    g1 = em.sb("g1sb", [128, KC], F32)
    par = em.sb("par", [128, 4, KC], F32)
    em.dma("sp", x[:], xT[:], x, xT)
    f_load_mod(em, mod, 0, modsel)
    em.dma("sp", g1[:], g1d[0], g1, g1d)
    load_par(em, par, mod, g1, 0, 1)
    norm_mod(em, cst, psr, x, h, par, TOK_TILES, norm_pools(em))
    hxg.store(h)
    zt = em.sb("zt", [128, 8704], BF16)
    em.op("pool", lambda e: e.memset(zt[:], 0.0), writes=[zt])
    for r in range(4):
        for hh in range(2):
            em.dma("sp", mix_rs[128 * r:128 * r + 128, 8704 * hh:8704 * hh + 8704], zt[:], mix_rs, zt)
    em.scope_end()
    hxg.gather()

    def load_hx(hx, s, n):
        if s < L:
            hxg.load(hx, lambda k0, nk, hx=hx: hx[:, k0:k0 + nk, :], s // 1024, s % 1024, n)
        else:
            for r in range(4):
                hxg.load(hx, lambda k0, nk, hx=hx, r=r: hx[:, k0:k0 + nk, 64 * r:64 * r + 64], r, 1024, 64)

    for l in range(nl):
        moe = (l % 2 == 1)
        idx = l // 2
        last = (l == nl - 1)
        em.scope_begin()
        io = {"wna": view(wna, wna.t[l]), "wm": view(wm, wm.t[l]), "cosT": cosT, "sinT": sinT, "nab": view(nab, nab.t[l]), "cmask": cmask,
              "nqg": view(nqg, nqg.t[l]), "nkg": view(nkg, nkg.t[l]), "mng": view(mng, mng.t[l]),
              "gb": view(gb, gb.t[l]), "mixT": mix_part, "load_hx": load_hx}
        emit_B(em, cst, psr, ptr, io)
        em.scope_end()
        em.dma("sp", mix_rs.t.rearrange("(d p) (g x) -> p d g x", p=128, g=4)[:, :, bass.ts(jr, 1), :].rearrange("p d g x -> p d (g x)"),
               mix_part[:].rearrange("p d c t -> p d (c t)"), mix_rs, mix_part)
        em.cc("ReduceScatter", ALU.add, G4, mix_own[:], mix_rs[:], mix_own, mix_rs)
        em.scope_begin()
        f_C(em, cst, psr, l, xT if l == 0 else xres, mix_own, view(wout, wout.t[l]), modsel, view(n2gd, n2gd.t[l]),
            view(router, router.t[idx]) if moe else None, xres, h2g if moe else h2loc, gT_part, jr, bidx)
        em.scope_end()
        if moe:
            h2g.gather()
            em.cc("AllGather", ALU.bypass, G4, gT_all[:], gT_part[:], gT_all, gT_part)
            em.scope_begin()
            subs = [(view(mwg, mwg.t[idx, e]), view(mwu, mwu.t[idx, e]), view(mwd, mwd.t[idx, e]), DFF, True) for e in range(2)]
            f_D(em, psr, subs, h2g, gT_all, gsel, yparts, ysums, jr)
            em.scope_end()
        else:
            em.scope_begin()
            f_Dloc(em, psr, l, view(dwg, dwg.t[idx]), view(dwu, dwu.t[idx]), view(dwd, dwd.t[idx]), h2loc, xres, modsel)
            em.scope_end()
        em.scope_begin()
        f_E(em, cst, psr, l, xres, ysums if moe else None, modsel, None if last else view(g1d, g1d.t[l + 1]), xo if last else xres, hxg, bidx, last)
        em.scope_end()
        if not last:
            hxg.gather()
    em.finish()
    return nc


_WOUT_PERM = np.concatenate([np.arange(o, o + 128) for g in range(4) for o in (256 * g, 256 * g + 128, 1024 + 256 * g, 1024 + 256 * g + 128)])


def fused_inputs(i, x_sh, condT, ada_w, ada_b, norm1_g, norm2_g, w_in, gate_b, na_q_g, na_k_g, na_rpb, m_norm_g, w_out,
                 ffn_w_gate, ffn_w_up, ffn_w_down, moe_router, moe_w_gate, moe_w_up, moe_w_down, consts, shared, nl=DEPTH):
    f32 = np.float32
    j = i % 4
    cosT, sinT, cmask = consts
    cols = np.arange(128 * NCH * j, 128 * NCH * (j + 1))
    na_cols = np.concatenate([np.arange(o + 256 * j, o + 256 * j + 256) for o in (0, 1024, 2048)])
    mq = 3072 + 128 * j + np.arange(128)
    mk = 3584 + 128 * j + np.arange(128)
    mv = 4096 + 256 * j + np.arange(256)
    mo = 5120 + 256 * j + np.arange(256)
    mg = 6144 + np.array([j, 4 + j, 8 + j, 12 + j])
    m_cols = np.concatenate([mq, mk, mq[_SWAP], mk[_SWAP], mv, mo, mg])
    dwg, dwu, dwd = ffn_w_gate, ffn_w_up, ffn_w_down
    nd_, nm_ = (nl + 1) // 2, nl // 2
    d = {
        "xT": x_sh[i], "condT": condT,
        "adaw": np.ascontiguousarray(ada_w[:, :, cols]),
        "adab": np.ascontiguousarray(ada_b[:, cols].reshape(DEPTH, NCH, 128).transpose(2, 0, 1)),
        "g1": shared["g1"], "n2g": shared["n2g"],
        "wna": np.ascontiguousarray(w_in[:, :, na_cols]), "wm": np.ascontiguousarray(w_in[:, :, m_cols]),
        "nab": np.stack([np.stack([na_bias_table(na_rpb[l, 2 * j + h]) for h in range(2)]) for l in range(DEPTH)]),
        "nqg": shared["nqg"], "nkg": shared["nkg"],
        "mng": np.ascontiguousarray(np.stack([m_norm_g[l, 256 * j:256 * j + 256].reshape(2, 128).T for l in range(DEPTH)], axis=0)),
        "gb": np.ascontiguousarray(np.broadcast_to(gate_b[:, [j, 4 + j, 8 + j, 12 + j]][:, None, :], (DEPTH, 128, 4))),
        "cosT": cosT, "sinT": sinT, "cmask": cmask, "wout": shared["wout"], "router": shared["router"],
        "dwg": dwg, "dwu": dwu, "dwd": dwd,
        "mwg": np.ascontiguousarray(moe_w_gate[:, 2 * j:2 * j + 2]), "mwu": np.ascontiguousarray(moe_w_up[:, 2 * j:2 * j + 2]),
        "mwd": np.ascontiguousarray(moe_w_down[:, 2 * j:2 * j + 2]),
    }
    for k in ("adaw", "wna", "wm", "wout"):
        d[k] = np.ascontiguousarray(d[k][:nl])
    for k in ("dwg", "dwu", "dwd"):
        d[k] = np.ascontiguousarray(d[k][:nd_])
    for k in ("router", "mwg", "mwu", "mwd"):
        if nm_ == 0:
            del d[k]
        else:
            d[k] = np.ascontiguousarray(d[k][:nm_])
    return d


def kernel(x, c, ctx, c_ctx, ada_w, ada_b, norm1_g, norm2_g, w_in, gate_b, na_q_g, na_k_g, na_rpb, m_norm_g, w_out,
           ffn_w_gate, ffn_w_up, ffn_w_down, moe_router, moe_w_gate, moe_w_up, moe_w_down, _nl=DEPTH):
    f32 = np.float32
    A = lambda v: np.asarray(v, f32)
    x, c, ctx, c_ctx = A(x), A(c), A(ctx), A(c_ctx)
    ada_w, ada_b, norm1_g, norm2_g, w_in, gate_b = A(ada_w), A(ada_b), A(norm1_g), A(norm2_g), A(w_in), A(gate_b)
    na_q_g, na_k_g, na_rpb, m_norm_g, w_out = A(na_q_g), A(na_k_g), A(na_rpb), A(m_norm_g), A(w_out)
    ffn_w_gate, ffn_w_up, ffn_w_down, moe_router = A(ffn_w_gate), A(ffn_w_up), A(ffn_w_down), A(moe_router)
    moe_w_gate, moe_w_up, moe_w_down = A(moe_w_gate), A(moe_w_up), A(moe_w_down)
    x_sh = shard_tokens(x, ctx)
    condT = fm(np.stack([c[0], c[1], c_ctx], axis=0))
    consts = (*rope_tables(), col_mask())
    shared = {
        "g1": np.ascontiguousarray(fm(norm1_g).transpose(2, 0, 1)), "n2g": np.ascontiguousarray(fm(norm2_g).transpose(2, 0, 1)),
        "nqg": np.ascontiguousarray(na_q_g[:, :, None]), "nkg": np.ascontiguousarray(na_k_g[:, :, None]),
        "wout": np.ascontiguousarray(w_out[:, _WOUT_PERM, :]), "router": np.ascontiguousarray(np.stack([fm(moe_router[k].T) for k in range(2)])),
    }
    in_maps = [fused_inputs(i, x_sh, condT, ada_w, ada_b, norm1_g, norm2_g, w_in, gate_b, na_q_g, na_k_g, na_rpb, m_norm_g, w_out,
                            ffn_w_gate, ffn_w_up, ffn_w_down, moe_router, moe_w_gate, moe_w_up, moe_w_down, consts, shared, _nl) for i in range(8)]
    res = _run(_prog("fused", build_fused, _nl), in_maps)
    out = np.empty((B, L, D), f32)
    for i in range(8):
        b, j = i // 4, i % 4
        out[b, 1024 * j:1024 * j + 1024] = res[i]["xo"][:, :, :1024].transpose(2, 1, 0).reshape(1024, D)
    return out
```

```python
import numpy as np
import concourse.bass as bass
import concourse.mybir as mybir
from concourse.bass_utils import run_bass_kernel_spmd

F32 = mybir.dt.float32
BF16 = mybir.dt.bfloat16
ALU = mybir.AluOpType
AF = mybir.ActivationFunctionType
AX = mybir.AxisListType

D = 2048
KC = D // 128
B = 2
L = 4096
CTX = 256
TB = L + CTX
DEPTH = 4
DFF = 5632
FC = DFF // 128
NE = 8
EPS = 1e-6
TOK = 1088


class Buf:
    def __init__(self, name, t=None, psum=False):
        self.name = name
        self.t = t
        self.psum = psum
        self.w = None
        self.r = []
        self.sem = None
        self.dma_total = 0

    def __getitem__(self, k):
        return self.t[k]


class Em:
    ENG = ("pe", "act", "dve", "pool", "sp")

    def __init__(self, nc):
        self.nc = nc
        self.eng = {"pe": nc.tensor, "act": nc.scalar, "dve": nc.vector, "pool": nc.gpsimd, "sp": nc.sync}
        self.sem = {k: nc.alloc_semaphore("sem_" + k) for k in self.ENG}
        self.cnt = {k: 0 for k in self.ENG}
        self.obs = {k: {} for k in self.ENG}
        self.out_bufs = []
        self.guards = []
        self.free_sems = []
        self.scope_bufs = []
        self.persist_bufs = []
        self.in_scope = False
        self.mark = 0
        self.uid = 0

    def _nm(self, name):
        self.uid += 1
        return f"{name}_{self.uid}"

    def sb(self, name, shape, dtype=F32):
        g = self.nc.sbuf_tensor(self._nm(name), list(shape), dtype)
        t = g.__enter__()
        self.guards.append(g)
        b = Buf(name, t)
        b.scoped = self.in_scope
        return b

    def ps(self, name, shape, dtype=F32):
        g = self.nc.psum_tensor(self._nm(name), list(shape), dtype)
        t = g.__enter__()
        self.guards.append(g)
        b = Buf(name, t, psum=True)
        b.scoped = self.in_scope
        return b

    def dram(self, name, shape, dtype, kind="Internal"):
        t = self.nc.dram_tensor(name, list(shape), dtype, kind=kind)
        b = Buf(name, t.ap())
        b.scoped = False
        if kind == "ExternalOutput":
            self.out_bufs.append(b)
        return b

    def _get_sem(self, dst):
        if dst.sem is None:
            if self.free_sems:
                dst.sem, dst.dma_total = self.free_sems.pop()
            else:
                dst.sem = self.nc.alloc_semaphore(self._nm("dsem"))
            (self.scope_bufs if getattr(dst, "scoped", False) else self.persist_bufs).append(dst)

    def scope_begin(self):
        self.in_scope = True
        self.mark = len(self.guards)
        self.scope_bufs = []

    def barrier(self):
        evs = [(self.sem[k], self.cnt[k]) for k in self.ENG if self.cnt[k] > 0]
        evs += [(b.sem, b.dma_total) for b in self.scope_bufs + self.persist_bufs if b.dma_total > 0]
        for e in self.ENG:
            self._wait(e, evs)

    def scope_end(self):
        self.barrier()
        for b in self.scope_bufs:
            self.free_sems.append((b.sem, b.dma_total))
        self.scope_bufs = []
        while len(self.guards) > self.mark:
            self.guards.pop().__exit__(None, None, None)
        self.in_scope = False

    def pool_(self, name, shape, dtype, n):
        bufs = [self.sb(f"{name}{i}", shape, dtype) for i in range(n)]
        st = {"i": 0}

        def nxt():
            b = bufs[st["i"] % n]
            st["i"] += 1
            return b
        return nxt

    def _wait(self, e, evs):
        need = {}
        for ev in evs:
            if ev is None:
                continue
            s, v = ev
            k = s.num
            if k not in need or need[k][1] < v:
                need[k] = (s, v)
        for k, (s, v) in need.items():
            if e == "pe" and s is self.sem["pe"]:
                continue
            if self.obs[e].get(k, 0) >= v:
                continue
            self.eng[e].wait_ge(s, v)
            self.obs[e][k] = v

    def op(self, e, fn, reads=(), writes=(), signal=True):
        evs = []
        for b in reads:
            evs.append(b.w)
            if b.psum:
                evs.extend(b.r)
        for b in writes:
            evs.append(b.w)
            evs.extend(b.r)
        self._wait(e, evs)
        ins = fn(self.eng[e])
        if signal:
            self.cnt[e] += 1
            ins.then_inc(self.sem[e], 1)
            ev = (self.sem[e], self.cnt[e])
        else:
            ev = (self.sem[e], self.cnt[e] + 1)
        for b in writes:
            b.w = ev
            b.r = []
        for b in reads:
            if b not in writes:
                b.r = [x for x in b.r if x[0] is not ev[0]] + [ev]
        return ins

    def dma(self, q, out_ap, in_ap, dst, src):
        self._get_sem(dst)
        evs = [src.w]
        evs.extend(dst.r)
        if dst.w is not None and dst.w[0] is not dst.sem:
            evs.append(dst.w)
        self._wait(q, evs)
        ins = self.eng[q].dma_start(out=out_ap, in_=in_ap)
        ins.then_inc(dst.sem, 16)
        dst.dma_total += 16
        ev = (dst.sem, dst.dma_total)
        dst.w = ev
        dst.r = []
        src.r = [x for x in src.r if x[0] is not ev[0]] + [ev]
        return ins

    def cc(self, kind, op, groups, out_ap, in_ap, dst, src):
        self._get_sem(dst)
        evs = [src.w, dst.w]
        evs.extend(dst.r)
        evs.extend(src.r)
        self._wait("pool", evs)
        ins = self.eng["pool"].collective_compute(kind, op, replica_groups=groups, ins=[in_ap.opt()], outs=[out_ap.opt()])
        ins.then_inc(dst.sem)
        dst.dma_total += 1
        ev = (dst.sem, dst.dma_total)
        dst.w = ev
        dst.r = []
        src.r = [x for x in src.r if x[0] is not ev[0]] + [ev]
        return ins

    def finish(self):
        evs = [b.w for b in self.out_bufs]
        self._wait("sp", evs)
        self._wait("sp", [(self.sem[k], self.cnt[k]) for k in ("pe", "act", "dve", "pool") if self.cnt[k] > 0])


def tiles_of(n, t=512):
    return [(s, min(t, n - s)) for s in range(0, n, t)]


class Consts:
    def __init__(self, em):
        self.ones_bf = em.sb("c_ones_bf", [128, 128], BF16)
        self.ones_f = em.sb("c_ones_f", [128, 128], F32)
        self.ident_f = em.sb("c_ident_f", [128, 128], F32)
        self.ident_bf = em.sb("c_ident_bf", [128, 128], BF16)
        self.eps = em.sb("c_eps", [128, 1], F32)
        self.one = em.sb("c_one", [128, 1], F32)
        em.op("pool", lambda e: e.memset(self.ones_f[:], 1.0), writes=[self.ones_f])
        em.op("pool", lambda e: e.memset(self.ones_bf[:], 1.0), writes=[self.ones_bf])
        em.op("pool", lambda e: e.memset(self.eps[:], EPS), writes=[self.eps])
        em.op("pool", lambda e: e.memset(self.one[:], 1.0), writes=[self.one])
        em.op("pool", lambda e: e.memset(self.ident_f[:], 0.0), writes=[self.ident_f])
        em.op("pool", lambda e: e.affine_select(out=self.ident_f[:], in_=self.ident_f[:], pattern=[[-1, 128]],
                                                 compare_op=ALU.not_equal, fill=1.0, base=0, channel_multiplier=1),
              reads=[self.ident_f], writes=[self.ident_f])
        em.op("dve", lambda e: e.tensor_copy(out=self.ident_bf[:], in_=self.ident_f[:]), reads=[self.ident_f],
              writes=[self.ident_bf])


class PsumRing:
    def __init__(self, em, n=8):
        self.banks = [em.ps(f"psb{i}", [128, 512], F32) for i in range(n)]
        self.i = 0

    def next(self):
        b = self.banks[self.i % len(self.banks)]
        self.i += 1
        return b


def mm_acc(em, pbank, out_ap, pairs, reads, sparse=True):
    n = len(pairs)
    for i, (l, r) in enumerate(pairs):
        em.op("pe", lambda e, l=l, r=r, i=i: e.matmul(out_ap, lhsT=l, rhs=r, start=(i == 0), stop=(i == n - 1)),
              reads=reads, writes=[pbank], signal=(i == n - 1) or not sparse)


def make_affine(em, a_out, g, scale_ap, a_buf_reads):
    em.op("dve", lambda e: e.scalar_tensor_tensor(out=a_out, in0=scale_ap, scalar=1.0, in1=g, op0=ALU.add, op1=ALU.mult),
          reads=a_buf_reads, writes=[a_buf_reads[0]])


def norm_mod(em, cst, psr, x, out, par, tiles, pools, hf_cb=None):
    sq_p, r_p, tmp_p, hf_p = pools
    for ti, (s, n, is_c) in enumerate(tiles):
        pb = psr.next()
        sqs = []
        for k in range(KC):
            sq = sq_p()
            em.op("act", lambda e, sq=sq, k=k: e.activation(out=sq[:, 0:n], in_=x[:, k, s:s + n], func=AF.Square),
                  reads=[x], writes=[sq])
            em.op("pe", lambda e, sq=sq, k=k: e.matmul(pb[:, 0:n], lhsT=cst.ones_bf[:], rhs=sq[:, 0:n], start=(k == 0),
                                                       stop=(k == KC - 1)),
                  reads=[sq, cst.ones_bf], writes=[pb])
        r = r_p()
        em.op("act", lambda e: e.activation(out=r[:, 0:n], in_=pb[:, 0:n], func=AF.Sqrt, bias=cst.eps[:, 0:1], scale=1.0 / D),
              reads=[pb, cst.eps], writes=[r])
        em.op("dve", lambda e: e.reciprocal(out=r[:, 0:n], in_=r[:, 0:n]), reads=[r], writes=[r])
        hf = hf_p() if hf_cb is not None else None
        o = 2 if is_c else 0
        for k in range(KC):
            if hf is None:
                tmp = tmp_p()
                tv = tmp[:, 0:n]
                tb = tmp
            else:
                tv = hf[:, k, 0:n]
                tb = hf
            em.op("dve", lambda e, k=k, tv=tv: e.tensor_tensor(out=tv, in0=x[:, k, s:s + n], in1=r[:, 0:n], op=ALU.mult),
                  reads=[x, r], writes=[tb])
            if hf is None:
                em.op("act", lambda e, k=k, tv=tv: e.activation(out=out[:, k, s:s + n], in_=tv, func=AF.Identity,
                                                                scale=par[:, o, k:k + 1], bias=par[:, o + 1, k:k + 1]),
                      reads=[tb, par], writes=[out])
            else:
                em.op("act", lambda e, k=k, tv=tv: e.activation(out=tv, in_=tv, func=AF.Identity,
                                                                scale=par[:, o, k:k + 1], bias=par[:, o + 1, k:k + 1]),
                      reads=[tb, par], writes=[tb])
                em.op("pool", lambda e, k=k, tv=tv: e.tensor_copy(out=out[:, k, s:s + n], in_=tv), reads=[tb], writes=[out])
        if hf_cb is not None:
            hf_cb(ti, s, n, hf)


def load_par(em, par, mod, g, split_shift, split_scale):
    for c in range(2):
        em.op("dve", lambda e, c=c: e.scalar_tensor_tensor(out=par[:, 2 * c, :], in0=mod[:, c, split_scale * 16:split_scale * 16 + 16],
                                                           scalar=1.0, in1=g[:], op0=ALU.add, op1=ALU.mult),
              reads=[mod, g], writes=[par])
        em.op("dve", lambda e, c=c: e.tensor_copy(out=par[:, 2 * c + 1, :], in_=mod[:, c, split_shift * 16:split_shift * 16 + 16]),
              reads=[mod], writes=[par])


TOK_TILES = [(0, 512, False), (512, 512, False), (1024, 64, True)]


def norm_pools(em, with_hf=False, hf_n=256):
    sq_p = em.pool_("nm_sq", [128, 512], BF16, 3)
    r_p = em.pool_("nm_r", [128, 512], F32, 2)
    tmp_p = em.pool_("nm_tmp", [128, 512], F32, 3)
    hf_p = em.pool_("nm_hf", [128, KC, hf_n], F32, 1) if with_hf else None
    return (sq_p, r_p, tmp_p, hf_p)


def build_M():
    nc = bass.Bass("TRN2", target_bir_lowering=False)
    em = Em(nc)
    condT = em.dram("condT", [128, KC, 3], F32, kind="ExternalInput")
    adaw = em.dram("adaw", [DEPTH, D, 1536], F32, kind="ExternalInput")
    adab = em.dram("adab", [128, DEPTH, 12], F32, kind="ExternalInput")
    mod = em.dram("mod", [128, DEPTH, 12, 3], F32, kind="ExternalOutput")
    psr = PsumRing(em, 4)
    cf = em.sb("cf", [128, KC, 3], F32)
    cb = em.sb("cb", [128, KC, 3], BF16)
    bt = em.sb("bt", [128, DEPTH, 12], F32)
    res = em.sb("res", [128, DEPTH, 12, 3], F32)
    wp = em.pool_("adaw_sb", [128, KC, 1536], BF16, 2)
    em.dma("sp", cf[:], condT[:], cf, condT)
    em.dma("sp", bt[:], adab[:], bt, adab)
    em.op("act", lambda e: e.activation(out=cb[:], in_=cf[:], func=AF.Silu), reads=[cf], writes=[cb])
    for l in range(DEPTH):
        w = wp()
        em.dma("pool", w[:], adaw[l].rearrange("(k p) f -> p k f", p=128), w, adaw)
        pb = psr.next()
        for j in range(12):
            mm_acc(em, pb, pb[:, 3 * j:3 * j + 3], [(w[:, k, 128 * j:128 * j + 128], cb[:, k, :]) for k in range(KC)], [w, cb])
        em.op("dve", lambda e, l=l: e.tensor_tensor(out=res[:, l, :, :], in0=pb[:, 0:36].rearrange("p (j c) -> p j c", c=3),
                                                    in1=bt[:, l, :].unsqueeze(2).to_broadcast([128, 12, 3]), op=ALU.add),
              reads=[pb, bt], writes=[res])
    em.dma("sp", mod[:], res[:], mod, res)
    em.finish()
    return nc


def build_A0():
    nc = bass.Bass("TRN2", target_bir_lowering=False)
    em = Em(nc)
    xT = em.dram("xT", [128, KC, TOK], F32, kind="ExternalInput")
    modd = em.dram("mod", [128, 2, 96], F32, kind="ExternalInput")
    g1d = em.dram("g1", [128, KC], F32, kind="ExternalInput")
    hxT = em.dram("hxT", [128, KC, TOK], BF16, kind="ExternalOutput")
    cst = Consts(em)
    psr = PsumRing(em, 4)
    x = em.sb("x", [128, KC, TOK], F32)
    h = em.sb("h", [128, KC, TOK], BF16)
    mod = em.sb("modsb", [128, 2, 96], F32)
    g1 = em.sb("g1sb", [128, KC], F32)
    par = em.sb("par", [128, 4, KC], F32)
    em.dma("sp", x[:], xT[:], x, xT)
    em.dma("sp", mod[:], modd[:], mod, modd)
    em.dma("sp", g1[:], g1d[:], g1, g1d)
    load_par(em, par, mod, g1, 0, 1)
    norm_mod(em, cst, psr, x, h, par, TOK_TILES, norm_pools(em))
    em.dma("sp", hxT[:], h[:], hxT, h)
    em.finish()
    return nc


_PROGS = {}


def _prog(name, builder, *args):
    key = (name,) + tuple(args)
    if key not in _PROGS:
        _PROGS[key] = builder(*args)
    return _PROGS[key]


def _run(nc, in_maps):
    res = run_bass_kernel_spmd(nc, in_maps, core_ids=list(range(len(in_maps))))
    return res.results


def fm(v):
    v = np.asarray(v)
    lead = v.shape[:-1]
    r = v.reshape(lead + (KC, 128))
    r = np.moveaxis(r, -1, 0)
    r = np.moveaxis(r, -1, 1)
    return np.ascontiguousarray(r)


def host_M(c, c_ctx, ada_w, ada_b):
    cond = np.stack([c[0], c[1], c_ctx], axis=0)
    condT = fm(cond)
    in_maps = []
    for i in range(8):
        cols = np.concatenate([np.arange(128 * (12 * i), 128 * (12 * i + 12))])
        aw = np.ascontiguousarray(ada_w[:, :, cols])
        ab = ada_b[:, cols].reshape(DEPTH, 12, 128).transpose(2, 0, 1)
        in_maps.append({"condT": condT, "adaw": aw, "adab": np.ascontiguousarray(ab)})
    res = _run(_prog("M", build_M), in_maps)
    mod = np.concatenate([r["mod"] for r in res], axis=2)
    return [np.ascontiguousarray(mod[:, l]) for l in range(DEPTH)]


def mod_for_core(modl, b):
    return np.ascontiguousarray(np.stack([modl[:, :, b], modl[:, :, 2]], axis=1))


def shard_tokens(x, ctx):
    outs = []
    for b in range(B):
        for j in range(4):
            t = np.concatenate([x[b, 1024 * j:1024 * (j + 1)], ctx[b, 64 * j:64 * (j + 1)]], axis=0)
            outs.append(np.ascontiguousarray(t.reshape(TOK, KC, 128).transpose(2, 1, 0)))
    return outs


NT128 = 9


def build_C():
    nc = bass.Bass("TRN2", target_bir_lowering=False)
    em = Em(nc)
    xT = em.dram("xT", [128, KC, TOK], F32, kind="ExternalInput")
    mixT = em.dram("mixT", [128, KC, TOK], BF16, kind="ExternalInput")
    wout = em.dram("wout", [D, D], F32, kind="ExternalInput")
    modd = em.dram("mod", [128, 2, 96], F32, kind="ExternalInput")
    n2gd = em.dram("n2g", [128, KC], F32, kind="ExternalInput")
    routd = em.dram("router", [128, KC, NE], F32, kind="ExternalInput")
    xo = em.dram("xo", [128, KC, TOK], F32, kind="ExternalOutput")
    h2T = em.dram("h2T", [128, KC, TOK], BF16, kind="ExternalOutput")
    gout = em.dram("gates", [128, NT128, NE], F32, kind="ExternalOutput")
    cst = Consts(em)
    psr = PsumRing(em, 6)
    x = em.sb("x", [128, KC, TOK], F32)
    mix = em.sb("mix", [128, KC, TOK], BF16)
    h2 = em.sb("h2", [128, KC, TOK], BF16)
    mod = em.sb("modsb", [128, 2, 96], F32)
    n2g = em.sb("n2gsb", [128, KC], F32)
    rout = em.sb("routsb", [128, KC, NE], F32)
    par = em.sb("par", [128, 4, KC], F32)
    gates = em.sb("gatessb", [128, NT128, NE], F32)
    em.dma("sp", x[:], xT[:], x, xT)
    em.dma("sp", mix[:], mixT[:], mix, mixT)
    em.dma("sp", mod[:], modd[:], mod, modd)
    em.dma("sp", n2g[:], n2gd[:], n2g, n2gd)
    em.dma("sp", rout[:], routd[:], rout, routd)
    em.op("pool", lambda e: e.memset(gates[:], 0.0), writes=[gates])
    wp = em.pool_("wout_sb", [128, KC, 512], BF16, 2)
    for cg in range(4):
        w = wp()
        em.dma("pool", w[:], wout[:, 512 * cg:512 * cg + 512].rearrange("(k p) f -> p k f", p=128), w, wout)
        for dc in range(4):
            d = 4 * cg + dc
            for (s, n, is_c) in TOK_TILES:
                pb = psr.next()
                mm_acc(em, pb, pb[:, 0:n], [(w[:, k, 128 * dc:128 * dc + 128], mix[:, k, s:s + n]) for k in range(KC)], [w, mix])
                gcol = mod[:, 1 if is_c else 0, 32 + d:32 + d + 1]
                em.op("dve", lambda e, pb=pb, d=d, s=s, n=n, gcol=gcol: e.scalar_tensor_tensor(
                    out=x[:, d, s:s + n], in0=pb[:, 0:n], scalar=gcol, in1=x[:, d, s:s + n], op0=ALU.mult, op1=ALU.add),
                    reads=[pb, mod, x], writes=[x])
    em.dma("sp", xo[:], x[:], xo, x)
    load_par(em, par, mod, n2g, 3, 4)
    small = em.pool_("rt_small", [128, 8], F32, 12)
    lgp = em.pool_("rt_lg", [128, NE], F32, 3)

    def router_cb(ti, s, n, hf):
        for j in range(0, n, 128):
            m = min(128, n - j)
            t128 = (s + j) // 128
            pb = psr.next()
            mm_acc(em, pb, pb[0:m, 0:NE], [(hf[:, k, j:j + m], rout[:, k, :]) for k in range(KC)], [hf, rout])
            lg = lgp()
            em.op("act", lambda e: e.activation(out=lg[0:m, :], in_=pb[0:m, 0:NE], func=AF.Copy), reads=[pb], writes=[lg])
            m1 = small(); eq = small(); lg2 = small(); m2 = small(); sel = small(); nm1 = small(); ex = small(); den = small()
            em.op("dve", lambda e: e.reduce_max(out=m1[0:m, 0:1], in_=lg[0:m, :], axis=AX.X), reads=[lg], writes=[m1])
            em.op("dve", lambda e: e.tensor_scalar(out=eq[0:m, :], in0=lg[0:m, :], scalar1=m1[0:m, 0:1], scalar2=None, op0=ALU.is_equal),
                  reads=[lg, m1], writes=[eq])
            em.op("dve", lambda e: e.scalar_tensor_tensor(out=lg2[0:m, :], in0=eq[0:m, :], scalar=-1e30, in1=lg[0:m, :], op0=ALU.mult, op1=ALU.add),
                  reads=[eq, lg], writes=[lg2])
            em.op("dve", lambda e: e.reduce_max(out=m2[0:m, 0:1], in_=lg2[0:m, :], axis=AX.X), reads=[lg2], writes=[m2])
            em.op("dve", lambda e: e.tensor_scalar(out=sel[0:m, :], in0=lg[0:m, :], scalar1=m2[0:m, 0:1], scalar2=None, op0=ALU.is_ge),
                  reads=[lg, m2], writes=[sel])
            em.op("dve", lambda e: e.tensor_scalar(out=nm1[0:m, 0:1], in0=m1[0:m, 0:1], scalar1=-1.0, scalar2=None, op0=ALU.mult),
                  reads=[m1], writes=[nm1])
            em.op("act", lambda e: e.activation(out=ex[0:m, :], in_=lg[0:m, :], func=AF.Exp, bias=nm1[0:m, 0:1], scale=1.0),
                  reads=[lg, nm1], writes=[ex])
            em.op("dve", lambda e: e.tensor_tensor(out=ex[0:m, :], in0=ex[0:m, :], in1=sel[0:m, :], op=ALU.mult), reads=[ex, sel], writes=[ex])
            em.op("dve", lambda e: e.reduce_sum(out=den[0:m, 0:1], in_=ex[0:m, :], axis=AX.X), reads=[ex], writes=[den])
            em.op("dve", lambda e: e.reciprocal(out=den[0:m, 0:1], in_=den[0:m, 0:1]), reads=[den], writes=[den])
            em.op("dve", lambda e: e.tensor_scalar(out=gates[0:m, t128, :], in0=ex[0:m, :], scalar1=den[0:m, 0:1], scalar2=None, op0=ALU.mult),
                  reads=[ex, den], writes=[gates])

    c_tiles = [(0, 256, False), (256, 256, False), (512, 256, False), (768, 256, False), (1024, 64, True)]
    norm_mod(em, cst, psr, x, h2, par, c_tiles, norm_pools(em, with_hf=True), hf_cb=router_cb)
    em.dma("sp", h2T[:], h2[:], h2T, h2)
    em.dma("sp", gout[:], gates[:], gout, gates)
    em.finish()
    return nc


NTOK_ALL = 8 * TOK


def build_D(F, GC):
    nc = bass.Bass("TRN2", target_bir_lowering=False)
    em = Em(nc)
    NG = F // (128 * GC)
    h2T = em.dram("h2T", [128, KC, NTOK_ALL], BF16, kind="ExternalInput")
    grow = em.dram("grow", [1, NTOK_ALL], F32, kind="ExternalInput")
    wg = em.dram("wg", [D, F], F32, kind="ExternalInput")
    wu = em.dram("wu", [D, F], F32, kind="ExternalInput")
    wd = em.dram("wd", [F, D], F32, kind="ExternalInput")
    yT = em.dram("yT", [128, KC, NTOK_ALL], F32, kind="ExternalOutput")
    psr = PsumRing(em, 8)
    hp = em.pool_("h_sb", [128, KC, 512], BF16, 2)
    gp = em.pool_("g_sb", [128, 512], F32, 2)
    wgp = em.pool_("wg_sb", [128, KC, 128 * GC], BF16, 2)
    wup = em.pool_("wu_sb", [128, KC, 128 * GC], BF16, 2)
    wdp = em.pool_("wd_sb", [128, GC, D], BF16, 2)
    actp = em.pool_("act_sb", [128, GC, 512], BF16, 2)
    sgp = em.pool_("sg_sb", [128, 512], F32, 3)
    yacc = em.sb("yacc", [128, KC, 512], F32)
    for tt in range(NTOK_ALL // 512):
        s = 512 * tt
        h = hp()
        em.dma("sp", h[:], h2T[:, :, s:s + 512], h, h2T)
        g = gp()
        em.dma("sp", g[:], grow[0:1, s:s + 512].partition_broadcast(128), g, grow)
        for gi in range(NG):
            wgs = wgp(); wus = wup(); wds = wdp()
            c0 = 128 * GC * gi
            em.dma("pool", wgs[:], wg[:, c0:c0 + 128 * GC].rearrange("(k p) f -> p k f", p=128), wgs, wg)
            em.dma("pool", wus[:], wu[:, c0:c0 + 128 * GC].rearrange("(k p) f -> p k f", p=128), wus, wu)
            em.dma("pool", wds[:], wd[c0:c0 + 128 * GC, :].rearrange("(c p) f -> p c f", p=128), wds, wd)
            act = actp()
            for c in range(GC):
                pg = psr.next(); pu = psr.next()
                mm_acc(em, pg, pg[:, :], [(wgs[:, k, 128 * c:128 * c + 128], h[:, k, :]) for k in range(KC)], [wgs, h])
                mm_acc(em, pu, pu[:, :], [(wus[:, k, 128 * c:128 * c + 128], h[:, k, :]) for k in range(KC)], [wus, h])
                sg = sgp()
                em.op("act", lambda e, pg=pg, sg=sg: e.activation(out=sg[:], in_=pg[:, :], func=AF.Silu), reads=[pg], writes=[sg])
                em.op("dve", lambda e, pu=pu, sg=sg, c=c: e.tensor_tensor(out=act[:, c, :], in0=pu[:, :], in1=sg[:], op=ALU.mult),
                      reads=[pu, sg], writes=[act])
            for d in range(KC):
                pd = psr.next()
                mm_acc(em, pd, pd[:, :], [(wds[:, c, 128 * d:128 * d + 128], act[:, c, :]) for c in range(GC)], [wds, act])
                if gi == 0:
                    em.op("act", lambda e, pd=pd, d=d: e.activation(out=yacc[:, d, :], in_=pd[:, :], func=AF.Copy), reads=[pd], writes=[yacc])
                else:
                    em.op("dve", lambda e, pd=pd, d=d: e.tensor_tensor(out=yacc[:, d, :], in0=pd[:, :], in1=yacc[:, d, :], op=ALU.add),
                          reads=[pd, yacc], writes=[yacc])
        for d in range(KC):
            em.op("pool", lambda e, d=d: e.tensor_tensor(out=yacc[:, d, :], in0=yacc[:, d, :], in1=g[:], op=ALU.mult), reads=[yacc, g], writes=[yacc])
        em.dma("sp", yT[:, :, s:s + 512], yacc[:], yT, yacc)
    em.finish()
    return nc


def build_E():
    nc = bass.Bass("TRN2", target_bir_lowering=False)
    em = Em(nc)
    xT = em.dram("xT", [128, KC, TOK], F32, kind="ExternalInput")
    yp = em.dram("yp", [NE, 128, KC, TOK], F32, kind="ExternalInput")
    modd = em.dram("mod", [128, 2, 96], F32, kind="ExternalInput")
    modnd = em.dram("modn", [128, 2, 96], F32, kind="ExternalInput")
    g1d = em.dram("g1n", [128, KC], F32, kind="ExternalInput")
    xo = em.dram("xo", [128, KC, TOK], F32, kind="ExternalOutput")
    hxT = em.dram("hxT", [128, KC, TOK], BF16, kind="ExternalOutput")
    cst = Consts(em)
    psr = PsumRing(em, 4)
    x = em.sb("x", [128, KC, TOK], F32)
    h = em.sb("h", [128, KC, TOK], BF16)
    mod = em.sb("modsb", [128, 2, 96], F32)
    modn = em.sb("modnsb", [128, 2, 96], F32)
    g1 = em.sb("g1sb", [128, KC], F32)
    par = em.sb("par", [128, 4, KC], F32)
    em.dma("sp", x[:], xT[:], x, xT)
    em.dma("sp", mod[:], modd[:], mod, modd)
    em.dma("sp", modn[:], modnd[:], modn, modnd)
    em.dma("sp", g1[:], g1d[:], g1, g1d)
    pp = em.pool_("yp_sb", [128, 4, TOK], F32, 3)
    for ei in range(NE):
        for kg in range(4):
            p = pp()
            em.dma("sp", p[:], yp[ei, :, 4 * kg:4 * kg + 4, :], p, yp)
            for kk in range(4):
                k = 4 * kg + kk
                for (s, n, c) in ((0, 1024, 0), (1024, 64, 1)):
                    em.op("dve", lambda e, p=p, kk=kk, k=k, s=s, n=n, c=c: e.scalar_tensor_tensor(
                        out=x[:, k, s:s + n], in0=p[:, kk, s:s + n], scalar=mod[:, c, 80 + k:80 + k + 1], in1=x[:, k, s:s + n],
                        op0=ALU.mult, op1=ALU.add), reads=[p, mod, x], writes=[x])
    em.dma("sp", xo[:], x[:], xo, x)
    load_par(em, par, modn, g1, 0, 1)
    norm_mod(em, cst, psr, x, h, par, TOK_TILES, norm_pools(em))
    em.dma("sp", hxT[:], h[:], hxT, h)
    em.finish()
    return nc


NT = TB // 128
W_NA = 768
W_M = 1028
MASKNEG = -30000.0


def _na_start(r):
    return min(max(r - 4, 0), 56)


def build_B():
    nc = bass.Bass("TRN2", target_bir_lowering=False)
    em = Em(nc)
    io = {}
    hxT = em.dram("hxT", [128, KC, TB], BF16, kind="ExternalInput")
    io["wna"] = em.dram("wna", [D, W_NA], F32, kind="ExternalInput")
    io["wm"] = em.dram("wm", [D, W_M], F32, kind="ExternalInput")
    io["cosT"] = em.dram("cosT", [128, L], F32, kind="ExternalInput")
    io["sinT"] = em.dram("sinT", [128, L], F32, kind="ExternalInput")
    io["nab"] = em.dram("nab", [2, 64, 15, 64], F32, kind="ExternalInput")
    io["cmask"] = em.dram("cmask", [64, 64], F32, kind="ExternalInput")
    io["nqg"] = em.dram("nqg", [128, 1], F32, kind="ExternalInput")
    io["nkg"] = em.dram("nkg", [128, 1], F32, kind="ExternalInput")
    io["mng"] = em.dram("mng", [128, 2], F32, kind="ExternalInput")
    io["gb"] = em.dram("gb", [128, 4], F32, kind="ExternalInput")
    io["mixT"] = em.dram("mixT", [128, 4, 4, TOK], BF16, kind="ExternalOutput")
    io["load_hx"] = lambda hx, s, n: em.dma("sp", hx[:], hxT[:, :, s:s + n], hx, hxT)
    cst = Consts(em)
    psr = PsumRing(em, 7)
    ptr = em.ps("ps_tr", [128, 128], BF16)
    emit_B(em, cst, psr, ptr, io)
    em.finish()
    return nc


def emit_B(em, cst, psr, ptr, io):
    wna, wm, cosd, sind, nabd, cmd = io["wna"], io["wm"], io["cosT"], io["sinT"], io["nab"], io["cmask"]
    nqgd, nkgd, mngd, gbd, mixT = io["nqg"], io["nkg"], io["mng"], io["gb"], io["mixT"]
    load_hx = io["load_hx"]

    def store_mix(c, s, n, tile, buf):
        if s < L:
            em.dma("sp", mixT[:, s // 1024, c, s % 1024:s % 1024 + n], tile, mixT, buf)
        else:
            for r in range(4):
                em.dma("sp", mixT[:, r, c, 1024:TOK], tile[:, 64 * r:64 * r + 64], mixT, buf)

    W = em.sb("W", [128, KC, W_M], BF16)
    QK = em.sb("QK", [128, 4 * TB], BF16)
    VO = em.sb("VO", [128, 2 * TB], BF16)
    qk4 = QK[:].rearrange("p (c t) -> p c t", c=4)
    vna = VO[:].rearrange("p (n h d) -> p n h d", n=NT, h=2)
    sigo = VO[:].rearrange("p (c t) -> p c t", c=2)
    hsum = QK[:].bitcast(F32).rearrange("p (c t) -> p c t", c=2)
    mQ = em.sb("mQ", [128, TB], BF16)
    mK = em.sb("mK", [128, TB], BF16)
    mV = em.sb("mV", [128, NT, 257], BF16)
    G = em.sb("G", [128, NT, 4], F32)
    E = em.sb("E", [128, 2, 15, 64], BF16)
    nqg = em.sb("nqg_sb", [128, 1], F32)
    nkg = em.sb("nkg_sb", [128, 1], F32)
    mng = em.sb("mng_sb", [128, 2], F32)
    gb = em.sb("gb_sb", [128, 4], F32)
    cm = em.sb("cm_sb", [128, 64], F32)
    for (t, dsrc) in ((nqg, nqgd), (nkg, nkgd), (mng, mngd), (gb, gbd)):
        em.dma("sp", t[:], dsrc[:], t, dsrc)
    em.dma("sp", cm[0:64, :], cmd[:], cm, cmd)
    em.dma("sp", cm[64:128, :], cmd[:], cm, cmd)

    hxp = em.pool_("hx_sb", [128, KC, 256], BF16, 2)
    t256 = em.pool_("t256", [128, 256], F32, 6)
    b256 = em.pool_("b256", [128, 256], BF16, 3)

    em.dma("pool", W[:, :, 0:W_NA], wna[:].rearrange("(k p) f -> p k f", p=128), W, wna)
    tok_tiles = [(256 * i, 256) for i in range(TB // 256)]
    for (s, n) in tok_tiles:
        hx = hxp()
        load_hx(hx, s, n)
        for c in range(4):
            pb = psr.next()
            mm_acc(em, pb, pb[:, 0:n], [(W[:, k, 128 * c:128 * c + 128], hx[:, k, :]) for k in range(KC)], [W, hx])
            sq = b256()
            em.op("act", lambda e, pb=pb, sq=sq: e.activation(out=sq[:], in_=pb[:, 0:n], func=AF.Square), reads=[pb], writes=[sq])
            pb2 = psr.next()
            em.op("pe", lambda e, pb2=pb2, sq=sq: e.matmul(pb2[:, 0:n], lhsT=cst.ones_bf[:], rhs=sq[:], start=True, stop=True),
                  reads=[sq, cst.ones_bf], writes=[pb2])
            r = t256()
            em.op("act", lambda e, pb2=pb2, r=r: e.activation(out=r[:], in_=pb2[:, 0:n], func=AF.Sqrt, bias=cst.eps[:, 0:1], scale=1.0 / 128),
                  reads=[pb2, cst.eps], writes=[r])
            em.op("dve", lambda e, r=r: e.reciprocal(out=r[:], in_=r[:]), reads=[r], writes=[r])
            gsb = nqg if c < 2 else nkg
            em.op("dve", lambda e, pb=pb, r=r, c=c, gsb=gsb: e.scalar_tensor_tensor(out=qk4[:, c, s:s + n], in0=pb[:, 0:n], scalar=gsb[:, 0:1],
                                                                                   in1=r[:], op0=ALU.mult, op1=ALU.mult),
                  reads=[pb, r, gsb], writes=[QK])
        for j in range(n // 128):
            pb = psr.next()
            mm_acc(em, pb, pb[:, 0:256], [(hx[:, k, 128 * j:128 * j + 128], W[:, k, 512:768]) for k in range(KC)], [W, hx])
            tt = s // 128 + j
            em.op("act", lambda e, pb=pb, tt=tt: e.activation(out=vna[:, tt, :, :], in_=pb[:, 0:256].rearrange("p (h d) -> p h d", h=2), func=AF.Copy),
                  reads=[pb], writes=[VO])

    ebias = em.sb("ebias", [128, 2, 15, 64], F32)
    for hf in range(2):
        for h in range(2):
            em.dma("sp", ebias[64 * hf:64 * hf + 64, h, :, :], nabd[h], ebias, nabd)
    em.op("act", lambda e: e.activation(out=ebias[:], in_=ebias[:], func=AF.Exp), reads=[ebias], writes=[ebias])
    for h in range(2):
        em.op("dve", lambda e, h=h: e.tensor_tensor(out=E[:, h, :, :], in0=ebias[:, h, :, :], in1=cm[:].unsqueeze(1).to_broadcast([128, 15, 64]), op=ALU.mult),
              reads=[ebias, cm], writes=[E])

    ptp = em.pool_("PT", [128, 7, 128], BF16, 3)
    rdp = em.pool_("rden", [128, 256], F32, 3)
    nop = em.pool_("naout", [128, 256], BF16, 3)
    scale = 128 ** -0.5
    for h in range(2):
        qh = qk4[:, h, :]
        kh = qk4[:, 2 + h, :]
        for i in range(32):
            q0 = 128 * i
            u = min(max(2 * i - 4, 0), 54)
            tiles = []
            for t in range(5):
                val = [[False, False], [False, False]]
                for hf in range(2):
                    rk = u + 2 * t + hf
                    for e_ in range(2):
                        rq = 2 * i + e_
                        val[hf][e_] = (_na_start(rq) <= rk <= _na_start(rq) + 7)
                a0 = val[0][0] or val[0][1]
                a1 = val[1][0] or val[1][1]
                if not a0 and not a1:
                    continue
                assert a0
                tiles.append((t, u // 2 + t, 128 if a1 else 64, val))
            pA = psr.next(); pB = psr.next(); pC = psr.next()
            PT = ptp()
            slot = {}
            for idx, (t, kt, Kp, val) in enumerate(tiles):
                (pbk, col) = (pA, idx) if idx < 4 else (pB, idx - 4)
                slot[t] = (pbk, col)
                em.op("pe", lambda e, pbk=pbk, col=col, kt=kt, Kp=Kp: e.matmul(pbk[0:Kp, 128 * col:128 * col + 128], lhsT=kh[:, 128 * kt:128 * kt + Kp],
                                                                             rhs=qh[:, q0:q0 + 128], start=True, stop=True),
                      reads=[QK], writes=[pbk])
            nb = len(tiles)
            for cc in range(2):
                idx = nb + cc
                (pbk, col) = (pA, idx) if idx < 4 else (pB, idx - 4)
                em.op("pe", lambda e, pbk=pbk, col=col, cc=cc: e.matmul(pbk[:, 128 * col:128 * col + 128], lhsT=kh[:, L + 128 * cc:L + 128 * cc + 128],
                                                                      rhs=qh[:, q0:q0 + 128], start=True, stop=True),
                      reads=[QK], writes=[pbk])
            ntot = nb + 2
            Ks = [tl[2] for tl in tiles] + [128, 128]
            for idx in range(ntot):
                (pbk, col) = (pA, idx) if idx < 4 else (pB, idx - 4)
                Kp = Ks[idx]
                em.op("act", lambda e, pbk=pbk, col=col, idx=idx, Kp=Kp: e.activation(out=PT[0:Kp, idx, :], in_=pbk[0:Kp, 128 * col:128 * col + 128],
                                                                                      func=AF.Exp, scale=scale),
                      reads=[pbk], writes=[PT])
            for idx, (t, kt, Kp, val) in enumerate(tiles):
                for hf in range(2):
                    if hf == 1 and Kp == 64:
                        continue
                    rk = u + 2 * t + hf
                    dd0 = 7 - rk + 2 * i
                    rows = slice(64 * hf, 64 * hf + 64)
                    v0, v1 = val[hf]
                    if v0 and v1:
                        em.op("dve", lambda e, idx=idx, rows=rows, dd0=dd0: e.tensor_tensor(
                            out=PT[rows, idx, :].rearrange("p (a b) -> p a b", a=2), in0=PT[rows, idx, :].rearrange("p (a b) -> p a b", a=2),
                            in1=E[rows, h, dd0:dd0 + 2, :], op=ALU.mult), reads=[PT, E], writes=[PT])
                    else:
                        for e_ in range(2):
                            if val[hf][e_]:
                                em.op("dve", lambda e, idx=idx, rows=rows, dd0=dd0, e_=e_: e.tensor_tensor(
                                    out=PT[rows, idx, 64 * e_:64 * e_ + 64], in0=PT[rows, idx, 64 * e_:64 * e_ + 64],
                                    in1=E[rows, h, dd0 + e_, :], op=ALU.mult), reads=[PT, E], writes=[PT])
                            else:
                                em.op("dve", lambda e, idx=idx, rows=rows, e_=e_: e.memset(PT[rows, idx, 64 * e_:64 * e_ + 64], 0.0), writes=[PT])
            kts = [tl[1] for tl in tiles] + [32, 33]
            for idx in range(ntot):
                Kp = Ks[idx]
                em.op("pe", lambda e, idx=idx, Kp=Kp: e.matmul(pC[:, 0:128], lhsT=vna[0:Kp, kts[idx], h, :], rhs=PT[0:Kp, idx, :],
                                                            start=(idx == 0), stop=(idx == ntot - 1)), reads=[VO, PT], writes=[pC])
            for idx in range(ntot):
                Kp = Ks[idx]
                em.op("pe", lambda e, idx=idx, Kp=Kp: e.matmul(pC[:, 128:256], lhsT=cst.ones_bf[0:Kp, :], rhs=PT[0:Kp, idx, :],
                                                            start=(idx == 0), stop=(idx == ntot - 1)), reads=[cst.ones_bf, PT], writes=[pC])
            rd = rdp()
            em.op("dve", lambda e, rd=rd: e.reciprocal(out=rd[:, 0:128], in_=pC[:, 128:256]), reads=[pC], writes=[rd])
            no = nop()
            em.op("dve", lambda e, rd=rd, no=no: e.tensor_tensor(out=no[:, 0:128], in0=pC[:, 0:128], in1=rd[:, 0:128], op=ALU.mult),
                  reads=[pC, rd], writes=[no])
            store_mix(h, q0, 128, no[:, 0:128], no)
        pA = psr.next(); pC = psr.next()
        PT = ptp()
        PTc = PT[:, 0:4, :].rearrange("p (c x) q -> p c (x q)", c=2)
        for cc in range(2):
            em.op("pe", lambda e, cc=cc: e.matmul(pA[:, 256 * cc:256 * cc + 256], lhsT=kh[:, L + 128 * cc:L + 128 * cc + 128], rhs=qh[:, L:L + 256],
                                                 start=True, stop=True), reads=[QK], writes=[pA])
        em.op("act", lambda e: e.activation(out=PTc, in_=pA[:, 0:512].rearrange("p (c q) -> p c q", c=2), func=AF.Exp, scale=scale),
              reads=[pA], writes=[PT])
        for cc in range(2):
            em.op("pe", lambda e, cc=cc: e.matmul(pC[:, 0:256], lhsT=vna[:, 32 + cc, h, :], rhs=PTc[:, cc, :], start=(cc == 0), stop=(cc == 1)),
                  reads=[VO, PT], writes=[pC])
        for cc in range(2):
            em.op("pe", lambda e, cc=cc: e.matmul(pC[:, 256:512], lhsT=cst.ones_bf[:], rhs=PTc[:, cc, :], start=(cc == 0), stop=(cc == 1)),
                  reads=[cst.ones_bf, PT], writes=[pC])
        rd = rdp()
        em.op("dve", lambda e, rd=rd: e.reciprocal(out=rd[:], in_=pC[:, 256:512]), reads=[pC], writes=[rd])
        no = nop()
        em.op("dve", lambda e, rd=rd, no=no: e.tensor_tensor(out=no[:], in0=pC[:, 0:256], in1=rd[:], op=ALU.mult), reads=[pC, rd], writes=[no])
        store_mix(h, L, 256, no[:], no)

    em.dma("pool", W[:], wm[:].rearrange("(k p) f -> p k f", p=128), W, wm)
    em.op("pool", lambda e: e.memset(mV[:, :, 256:257], 1.0), writes=[mV])
    csp = em.pool_("cs_sb", [128, 2, 256], F32, 2)
    g4p = em.pool_("g4", [128, 4], F32, 4)
    kscale = 128 ** -0.5
    for (s, n) in tok_tiles:
        is_c = s >= L
        hx = hxp()
        load_hx(hx, s, n)
        if not is_c:
            cs = csp()
            em.dma("sp", cs[:, 0, :], cosd[:, s:s + n], cs, cosd)
            em.dma("sp", cs[:, 1, :], sind[:, s:s + n], cs, sind)
        for (c, dst, sc) in ((0, mQ, 1.0), (1, mK, kscale)):
            pb = psr.next()
            mm_acc(em, pb, pb[:, 0:n], [(W[:, k, 128 * c:128 * c + 128], hx[:, k, :]) for k in range(KC)], [W, hx])
            if is_c:
                em.op("act", lambda e, pb=pb, dst=dst, sc=sc: e.activation(out=dst[:, s:s + n], in_=pb[:, 0:n], func=AF.Copy, scale=sc),
                      reads=[pb], writes=[dst])
            else:
                pbs = psr.next()
                mm_acc(em, pbs, pbs[:, 0:n], [(W[:, k, 256 + 128 * c:256 + 128 * c + 128], hx[:, k, :]) for k in range(KC)], [W, hx])
                t1 = t256(); t2 = t256()
                em.op("dve", lambda e, pb=pb, t1=t1, sc=sc, cs=cs: e.scalar_tensor_tensor(out=t1[:], in0=pb[:, 0:n], scalar=sc, in1=cs[:, 0, :],
                                                                                       op0=ALU.mult, op1=ALU.mult), reads=[pb, cs], writes=[t1])
                em.op("dve", lambda e, pbs=pbs, t2=t2, sc=sc, cs=cs: e.scalar_tensor_tensor(out=t2[:], in0=pbs[:, 0:n], scalar=sc, in1=cs[:, 1, :],
                                                                                         op0=ALU.mult, op1=ALU.mult), reads=[pbs, cs], writes=[t2])
                em.op("pool", lambda e, t1=t1, t2=t2, dst=dst: e.tensor_tensor(out=dst[:, s:s + n], in0=t1[:], in1=t2[:], op=ALU.add),
                      reads=[t1, t2], writes=[dst])
        for c in range(2):
            pb = psr.next()
            mm_acc(em, pb, pb[:, 0:n], [(W[:, k, 768 + 128 * c:768 + 128 * c + 128], hx[:, k, :]) for k in range(KC)], [W, hx])
            em.op("act", lambda e, pb=pb, c=c: e.activation(out=sigo[:, c, s:s + n], in_=pb[:, 0:n], func=AF.Sigmoid), reads=[pb], writes=[VO])
        for j in range(n // 128):
            tt = s // 128 + j
            pb = psr.next()
            mm_acc(em, pb, pb[:, 0:256], [(hx[:, k, 128 * j:128 * j + 128], W[:, k, 512:768]) for k in range(KC)], [W, hx])
            em.op("act", lambda e, pb=pb, tt=tt: e.activation(out=mV[:, tt, 0:256], in_=pb[:, 0:256], func=AF.Copy), reads=[pb], writes=[mV])
            pg = psr.next()
            mm_acc(em, pg, pg[:, 0:4], [(hx[:, k, 128 * j:128 * j + 128], W[:, k, 1024:1028]) for k in range(KC)], [W, hx])
            gp_ = g4p(); ge = g4p()
            em.op("dve", lambda e, pg=pg, gp_=gp_: e.tensor_tensor(out=gp_[:], in0=pg[:, 0:4], in1=gb[:], op=ALU.add), reads=[pg, gb], writes=[gp_])
            em.op("act", lambda e, gp_=gp_, ge=ge: e.activation(out=ge[:], in_=gp_[:], func=AF.Exp, scale=-1.0), reads=[gp_], writes=[ge])
            em.op("act", lambda e, ge=ge: e.activation(out=ge[:], in_=ge[:], func=AF.Ln, bias=cst.one[:, 0:1], scale=1.0), reads=[ge, cst.one], writes=[ge])
            for col in (0, 2):
                em.op("pool", lambda e, gp_=gp_, tt=tt, col=col: e.tensor_copy(out=G[:, tt, col:col + 1], in_=gp_[:, col:col + 1]), reads=[gp_], writes=[G])
            for col in (1, 3):
                em.op("dve", lambda e, ge=ge, tt=tt, col=col: e.tensor_scalar(out=G[:, tt, col:col + 1], in0=ge[:, col:col + 1], scalar1=-1.0, scalar2=None,
                                                                            op0=ALU.mult), reads=[ge], writes=[G])

    triF = em.sb("triF", [128, 128], F32); triB = em.sb("triB", [128, 128], F32)
    mskF = em.sb("mskF", [128, 128], F32); mskB = em.sb("mskB", [128, 128], F32)
    for (t_, init, pat, cmul, fill) in ((triF, 1.0, [[1, 128]], -1, 0.0), (triB, 1.0, [[-1, 128]], 1, 0.0),
                                        (mskF, 0.0, [[1, 128]], -1, MASKNEG), (mskB, 0.0, [[-1, 128]], 1, MASKNEG)):
        em.op("pool", lambda e, t_=t_, init=init: e.memset(t_[:], init), writes=[t_])
        em.op("pool", lambda e, t_=t_, pat=pat, cmul=cmul, fill=fill: e.affine_select(out=t_[:], in_=t_[:], pattern=pat, compare_op=ALU.is_ge,
                                                                                   fill=fill, base=0, channel_multiplier=cmul),
              reads=[t_], writes=[t_])
    Cn = em.sb("Cn", [128, 257], F32)
    f128 = em.pool_("f128", [128, 128], F32, 8)
    bf128 = em.pool_("bf128", [128, 128], BF16, 8)
    colp = em.pool_("colp", [128, 1], F32, 12)
    cbp = em.pool_("Cb", [128, 256], BF16, 2)
    for d_ in range(2):
        tri, msk = (triF, mskF) if d_ == 0 else (triB, mskB)
        last = 127 if d_ == 0 else 0
        em.op("pool", lambda e: e.memset(Cn[:], 0.0), writes=[Cn])
        order = [32, 33] + list(range(32)) if d_ == 0 else [33, 32] + list(range(31, -1, -1))
        for tt in order:
            ts_ = slice(128 * tt, 128 * tt + 128)
            icol = G[:, tt, 2 * d_:2 * d_ + 1]
            lf = G[:, tt, 2 * d_ + 1:2 * d_ + 2]
            p1 = psr.next(); p2 = psr.next(); p3 = psr.next(); p4 = psr.next()
            em.op("pe", lambda e, p1=p1: e.matmul(p1[:, 0:1], lhsT=tri[:], rhs=lf, start=True, stop=True), reads=[tri, G], writes=[p1])
            lfb = f128()
            em.op("dve", lambda e, lfb=lfb: e.tensor_scalar(out=lfb[:], in0=cst.ones_f[:], scalar1=lf, scalar2=None, op0=ALU.mult),
                  reads=[cst.ones_f, G], writes=[lfb])
            em.op("pe", lambda e, p2=p2, lfb=lfb: e.matmul(p2[:, 0:128], lhsT=lfb[:], rhs=tri[:], start=True, stop=True), reads=[lfb, tri], writes=[p2])
            imb = colp(); blast = colp(); wcol = colp()
            em.op("dve", lambda e, imb=imb, p1=p1: e.tensor_tensor(out=imb[:], in0=icol, in1=p1[:, 0:1], op=ALU.subtract), reads=[G, p1], writes=[imb])
            dl = f128()
            em.op("dve", lambda e, dl=dl, p2=p2, imb=imb: e.scalar_tensor_tensor(out=dl[:], in0=p2[:, 0:128], scalar=imb[:, 0:1], in1=msk[:],
                                                                               op0=ALU.add, op1=ALU.add), reads=[p2, imb, msk], writes=[dl])
            em.op("act", lambda e, dl=dl: e.activation(out=dl[:], in_=dl[:], func=AF.Exp), reads=[dl], writes=[dl])
            A = f128()
            em.op("act", lambda e, A=A, p2=p2: e.activation(out=A[:], in_=p2[:, 0:128], func=AF.Exp), reads=[p2], writes=[A])
            em.op("dve", lambda e, blast=blast, p2=p2: e.tensor_copy(out=blast[:], in_=p2[:, last:last + 1]), reads=[p2], writes=[blast])
            em.op("act", lambda e, wcol=wcol, imb=imb, blast=blast: e.activation(out=wcol[:], in_=imb[:], func=AF.Exp, bias=blast[:, 0:1], scale=1.0),
                  reads=[imb, blast], writes=[wcol])
            em.op("pe", lambda e, p3=p3: e.matmul(p3[:, 0:128], lhsT=mK[:, ts_], rhs=mQ[:, ts_], start=True, stop=True), reads=[mK, mQ], writes=[p3])
            PTm = bf128()
            em.op("dve", lambda e, PTm=PTm, p3=p3, dl=dl: e.tensor_tensor(out=PTm[:], in0=p3[:, 0:128], in1=dl[:], op=ALU.mult), reads=[p3, dl], writes=[PTm])
            Qa = bf128()
            em.op("pool", lambda e, Qa=Qa, A=A: e.tensor_tensor(out=Qa[:], in0=mQ[:, ts_], in1=A[:], op=ALU.mult), reads=[mQ, A], writes=[Qa])
            Cb = cbp(); Nb = bf128()
            em.op("act", lambda e, Cb=Cb: e.activation(out=Cb[:], in_=Cn[:, 0:256], func=AF.Copy), reads=[Cn], writes=[Cb])
            em.op("pool", lambda e, Nb=Nb: e.tensor_copy(out=Nb[:], in_=Cn[:, 256:257].to_broadcast([128, 128])), reads=[Cn], writes=[Nb])
            for j in range(2):
                em.op("pe", lambda e, j=j, PTm=PTm: e.matmul(p4[:, 128 * j:128 * j + 128], lhsT=mV[:, tt, 128 * j:128 * j + 128], rhs=PTm[:], start=True, stop=False),
                      reads=[mV, PTm], writes=[p4])
                em.op("pe", lambda e, j=j, Cb=Cb, Qa=Qa: e.matmul(p4[:, 128 * j:128 * j + 128], lhsT=Cb[:, 128 * j:128 * j + 128], rhs=Qa[:], start=False, stop=True),
                      reads=[Cb, Qa], writes=[p4])
            em.op("pe", lambda e, PTm=PTm: e.matmul(p4[:, 256:384], lhsT=cst.ones_bf[:], rhs=PTm[:], start=True, stop=False), reads=[cst.ones_bf, PTm], writes=[p4])
            em.op("pe", lambda e, Nb=Nb, Qa=Qa: e.matmul(p4[:, 256:384], lhsT=Nb[:], rhs=Qa[:], start=False, stop=True), reads=[Nb, Qa], writes=[p4])
            rd = f128()
            em.op("act", lambda e, rd=rd: e.activation(out=rd[:], in_=p4[:, 256:384], func=AF.Abs), reads=[p4], writes=[rd])
            em.op("dve", lambda e, rd=rd: e.tensor_scalar(out=rd[:], in0=rd[:], scalar1=1.0, scalar2=None, op0=ALU.max), reads=[rd], writes=[rd])
            em.op("dve", lambda e, rd=rd: e.reciprocal(out=rd[:], in_=rd[:]), reads=[rd], writes=[rd])
            for j in range(2):
                if d_ == 0:
                    em.op("dve", lambda e, j=j, rd=rd: e.tensor_tensor(out=hsum[:, j, ts_], in0=p4[:, 128 * j:128 * j + 128], in1=rd[:], op=ALU.mult),
                          reads=[p4, rd], writes=[QK])
                else:
                    hb = f128()
                    em.op("dve", lambda e, j=j, rd=rd, hb=hb: e.tensor_tensor(out=hb[:], in0=p4[:, 128 * j:128 * j + 128], in1=rd[:], op=ALU.mult),
                          reads=[p4, rd], writes=[hb])
                    em.op("pool", lambda e, j=j, hb=hb: e.tensor_tensor(out=hsum[:, j, ts_], in0=hsum[:, j, ts_], in1=hb[:], op=ALU.add),
                          reads=[hb, QK], writes=[QK])
            em.op("pe", lambda e: e.transpose(ptr[:], mK[:, ts_], cst.ident_bf[:]), reads=[mK, cst.ident_bf], writes=[ptr])
            Kw = bf128()
            em.op("act", lambda e, Kw=Kw, wcol=wcol: e.activation(out=Kw[:], in_=ptr[:], func=AF.Copy, scale=wcol[:, 0:1]), reads=[ptr, wcol], writes=[Kw])
            p5 = psr.next()
            em.op("pe", lambda e, p5=p5, Kw=Kw: e.matmul(p5[:, 0:257], lhsT=Kw[:], rhs=mV[:, tt, :], start=True, stop=True), reads=[Kw, mV], writes=[p5])
            em.op("dve", lambda e, p5=p5, A=A: e.scalar_tensor_tensor(out=Cn[:], in0=Cn[:], scalar=A[:, last:last + 1], in1=p5[:, 0:257],
                                                                    op0=ALU.mult, op1=ALU.add), reads=[Cn, A, p5], writes=[Cn])

    t512 = em.pool_("t512", [128, 512], F32, 4)
    b512 = em.pool_("b512", [128, 512], BF16, 4)
    for (s, n) in tiles_of(TB, 512):
        pb = psr.next()
        for j in range(2):
            sq = b512()
            em.op("act", lambda e, sq=sq, j=j: e.activation(out=sq[:, 0:n], in_=hsum[:, j, s:s + n], func=AF.Square), reads=[QK], writes=[sq])
            em.op("pe", lambda e, sq=sq, j=j: e.matmul(pb[:, 0:n], lhsT=cst.ones_bf[:], rhs=sq[:, 0:n], start=(j == 0), stop=(j == 1)),
                  reads=[sq, cst.ones_bf], writes=[pb])
        r = t512()
        em.op("act", lambda e, r=r: e.activation(out=r[:, 0:n], in_=pb[:, 0:n], func=AF.Sqrt, bias=cst.eps[:, 0:1], scale=1.0 / 256), reads=[pb, cst.eps], writes=[r])
        em.op("dve", lambda e, r=r: e.reciprocal(out=r[:, 0:n], in_=r[:, 0:n]), reads=[r], writes=[r])
        for j in range(2):
            tm = t512()
            em.op("dve", lambda e, tm=tm, j=j, r=r: e.scalar_tensor_tensor(out=tm[:, 0:n], in0=hsum[:, j, s:s + n], scalar=mng[:, j:j + 1], in1=r[:, 0:n],
                                                                         op0=ALU.mult, op1=ALU.mult), reads=[QK, mng, r], writes=[tm])
            ob = b512()
            em.op("pool", lambda e, tm=tm, j=j, ob=ob: e.tensor_tensor(out=ob[:, 0:n], in0=tm[:, 0:n], in1=sigo[:, j, s:s + n], op=ALU.mult),
                  reads=[tm, VO], writes=[ob])
            store_mix(2 + j, s, n, ob[:, 0:n], ob)


def rope_tables():
    t = np.arange(L)
    row = (t // 64).astype(np.float32)
    col = (t % 64).astype(np.float32)
    inv = (np.float32(10000.0) ** (-np.arange(32, dtype=np.float32) / np.float32(32))).astype(np.float32)
    ang = np.concatenate([row[:, None] * inv, col[:, None] * inv], axis=-1).astype(np.float32)
    cos = np.cos(ang).astype(np.float32)
    sin = np.sin(ang).astype(np.float32)
    cosT = np.repeat(cos.T, 2, axis=0)
    sinT = np.repeat(sin.T, 2, axis=0)
    sign = np.where(np.arange(128) % 2 == 0, -1.0, 1.0).astype(np.float32)[:, None]
    return np.ascontiguousarray(cosT), np.ascontiguousarray(sinT * sign)


def col_mask():
    q = np.arange(64)
    cs = np.clip(q - 8, 0, 48)
    k = np.arange(64)
    ok = (k[:, None] >= cs[None, :]) & (k[:, None] < cs[None, :] + 16)
    return ok.astype(np.float32)


def na_bias_table(rpb_h):
    k = np.arange(64)
    dc = np.clip(k[:, None] - k[None, :], -15, 15) + 15
    dr = 14 - np.arange(15)
    return np.ascontiguousarray(rpb_h[dr[None, :, None], dc[:, None, :]])


_SWAP = np.arange(128) ^ 1


def b_inputs(l, b, g, hx_batch, w_in, gate_b, na_q_g, na_k_g, na_rpb, m_norm_g, consts):
    cosT, sinT, cmask = consts
    wl = w_in[l]
    na_cols = np.concatenate([np.arange(o + 256 * g, o + 256 * g + 256) for o in (0, 1024, 2048)])
    mq = 3072 + 128 * g + np.arange(128)
    mk = 3584 + 128 * g + np.arange(128)
    mv = 4096 + 256 * g + np.arange(256)
    mo = 5120 + 256 * g + np.arange(256)
    mg = 6144 + np.array([g, 4 + g, 8 + g, 12 + g])
    m_cols = np.concatenate([mq, mk, mq[_SWAP], mk[_SWAP], mv, mo, mg])
    nab = np.stack([na_bias_table(na_rpb[l, 2 * g + h]) for h in range(2)])
    return {
        "hxT": hx_batch,
        "wna": np.ascontiguousarray(wl[:, na_cols]), "wm": np.ascontiguousarray(wl[:, m_cols]),
        "cosT": cosT, "sinT": sinT, "nab": nab, "cmask": cmask,
        "nqg": np.ascontiguousarray(na_q_g[l].reshape(128, 1)), "nkg": np.ascontiguousarray(na_k_g[l].reshape(128, 1)),
        "mng": np.ascontiguousarray(m_norm_g[l, 256 * g:256 * g + 256].reshape(2, 128).T),
        "gb": np.ascontiguousarray(np.broadcast_to(gate_b[l, [g, 4 + g, 8 + g, 12 + g]][None, :], (128, 4))),
    }


def _batch_hx(hx_cores, b):
    xs = [hx_cores[4 * b + j][:, :, :1024] for j in range(4)]
    cs = [hx_cores[4 * b + j][:, :, 1024:] for j in range(4)]
    return np.ascontiguousarray(np.concatenate(xs + cs, axis=2))


def _mix_for_core(mix_b, j):
    out = np.empty((128, KC, TOK), dtype=mix_b[0].dtype)
    for g in range(4):
        m = mix_b[g]
        for ci, dst in enumerate((2 * g, 2 * g + 1, 8 + 2 * g, 9 + 2 * g)):
            out[:, dst, :1024] = m[:, ci, 1024 * j:1024 * j + 1024]
            out[:, dst, 1024:] = m[:, ci, L + 64 * j:L + 64 * j + 64]
    return out


def kernel_unfused(x, c, ctx, c_ctx, ada_w, ada_b, norm1_g, norm2_g, w_in, gate_b, na_q_g, na_k_g, na_rpb, m_norm_g, w_out,
           ffn_w_gate, ffn_w_up, ffn_w_down, moe_router, moe_w_gate, moe_w_up, moe_w_down):
    f32 = np.float32
    x = np.asarray(x, f32); ctx = np.asarray(ctx, f32)
    mods = host_M(np.asarray(c, f32), np.asarray(c_ctx, f32), np.asarray(ada_w, f32), np.asarray(ada_b, f32))
    xs = shard_tokens(x, ctx)
    res = _run(_prog("A0", build_A0), [{"xT": xs[i], "mod": mod_for_core(mods[0], i // 4), "g1": fm(norm1_g[0])} for i in range(8)])
    hx = [r["hxT"] for r in res]
    consts = (*rope_tables(), col_mask())
    ones_row = np.ones((1, NTOK_ALL), f32)
    for l in range(DEPTH):
        hxb = [_batch_hx(hx, b) for b in range(B)]
        res = _run(_prog("B", build_B), [b_inputs(l, i // 4, i % 4, hxb[i // 4], w_in, gate_b, na_q_g, na_k_g, na_rpb, m_norm_g, consts)
                                         for i in range(8)])
        mix = [r["mixT"] for r in res]
        moe = (l % 2 == 1)
        idx = l // 2
        router = fm(np.asarray(moe_router[idx], f32).T) if moe else np.zeros((128, KC, NE), f32)
        wo = np.asarray(w_out[l], f32)
        n2g = fm(norm2_g[l])
        res = _run(_prog("C", build_C), [{"xT": xs[i], "mixT": _mix_for_core(mix[4 * (i // 4):4 * (i // 4) + 4], i % 4), "wout": wo,
                                          "mod": mod_for_core(mods[l], i // 4), "n2g": n2g, "router": router} for i in range(8)])
        xs = [r["xo"] for r in res]
        h2_all = np.ascontiguousarray(np.concatenate([r["h2T"] for r in res], axis=2))
        if moe:
            gates = np.concatenate([r["gates"].transpose(1, 0, 2).reshape(NT128 * 128, NE)[:TOK] for r in res], axis=0)
            in_maps = [{"h2T": h2_all, "grow": np.ascontiguousarray(gates[:, e][None, :]),
                        "wg": np.asarray(moe_w_gate[idx, e], f32), "wu": np.asarray(moe_w_up[idx, e], f32),
                        "wd": np.asarray(moe_w_down[idx, e], f32)} for e in range(NE)]
            res = _run(_prog("D", build_D, DFF, 4), in_maps)
        else:
            in_maps = []
            for e in range(NE):
                wg = np.zeros((D, 768), f32); wu = np.zeros((D, 768), f32); wd = np.zeros((768, D), f32)
                wg[:, :704] = ffn_w_gate[idx][:, 704 * e:704 * e + 704]
                wu[:, :704] = ffn_w_up[idx][:, 704 * e:704 * e + 704]
                wd[:704] = ffn_w_down[idx][704 * e:704 * e + 704]
                in_maps.append({"h2T": h2_all, "grow": ones_row, "wg": wg, "wu": wu, "wd": wd})
            res = _run(_prog("D", build_D, 768, 3), in_maps)
        ys = [r["yT"] for r in res]
        ln = min(l + 1, DEPTH - 1)
        in_maps = [{"xT": xs[i], "yp": np.ascontiguousarray(np.stack([ys[e][:, :, TOK * i:TOK * i + TOK] for e in range(NE)])),
                    "mod": mod_for_core(mods[l], i // 4), "modn": mod_for_core(mods[ln], i // 4), "g1n": fm(norm1_g[ln])} for i in range(8)]
        res = _run(_prog("E", build_E), in_maps)
        xs = [r["xo"] for r in res]
        hx = [r["hxT"] for r in res]
    out = np.empty((B, L, D), f32)
    for i in range(8):
        b, j = i // 4, i % 4
        out[b, 1024 * j:1024 * j + 1024] = xs[i][:, :, :1024].transpose(2, 1, 0).reshape(1024, D)
    return out


G4 = [[0, 1, 2, 3], [4, 5, 6, 7]]
NCH = 24
FD = 1536


def view(buf, ap):
    b = Buf(buf.name + "_v", ap)
    b.scoped = False
    return b


class AGC:
    KG = 3

    def __init__(self, em, name):
        self.em = em
        self.chunks = []
        for k0 in range(0, KC, self.KG):
            nk = min(self.KG, KC - k0)
            part = em.dram(f"{name}_p{k0}", [128, nk * TOK], BF16)
            allb = em.dram(f"{name}_a{k0}", [512, nk * TOK], BF16)
            self.chunks.append((k0, nk, part, allb, allb.t.rearrange("(r p) (k t) -> p r k t", p=128, k=nk)))

    def store(self, h):
        for (k0, nk, part, allb, v) in self.chunks:
            self.em.dma("sp", part[:].rearrange("p (k t) -> p k t", k=nk), h[:, k0:k0 + nk, :], part, h)

    def gather(self):
        for (k0, nk, part, allb, v) in self.chunks:
            self.em.cc("AllGather", ALU.bypass, G4, allb[:], part[:], allb, part)

    def load(self, dst, dst_ap_fn, r, a, n):
        for (k0, nk, part, allb, v) in self.chunks:
            self.em.dma("sp", dst_ap_fn(k0, nk), v[:, r, :, a:a + n], dst, allb)


def flat_pieces(T0, n):
    out = []
    t = T0
    while t < T0 + n:
        r = t // TOK
        a = t % TOK
        ln = min(TOK - a, T0 + n - t)
        out.append((r, a, t - T0, ln))
        t += ln
    return out


def f_load_mod(em, mod_sb, l, modsel, bidx=None):
    em.dma("sp", mod_sb[:], modsel[:, :, l, :], mod_sb, modsel)


def f_select_mod(em, modsel, mod_all, bidx):
    src = mod_all.t.rearrange("(r p) (c l j) -> p c l r j", p=128, c=3, l=DEPTH)
    for l in range(DEPTH):
        em.dma("sp", modsel[:, 1:2, l, :].rearrange("p o (r j) -> p o r j", r=4), src[:, 2:3, l, :, :], modsel, mod_all)
    dsel = src[:, bass.ts(bidx, 1), :, :, :]
    for l in range(DEPTH):
        em.dma("sp", modsel[:, 0:1, l, :].rearrange("p o (r j) -> p o r j", r=4), dsel[:, :, l, :, :], modsel, mod_all)


def f_C(em, cst, psr, l, xsrc, mix_all, wout_l, modsel, n2g_l, router_l, xres, h2_part, gT_part, jr, bidx):
    x = em.sb("x", [128, KC, TOK], F32)
    mix = em.sb("mix", [128, KC, TOK], BF16)
    h2 = em.sb("h2", [128, KC, TOK], BF16)
    mod = em.sb("modsb", [128, 2, 96], F32)
    n2g = em.sb("n2gsb", [128, KC], F32)
    par = em.sb("par", [128, 4, KC], F32)
    em.dma("sp", x[:], xsrc[:], x, xsrc)
    em.dma("sp", mix[:], mix_all[:].rearrange("p (k t) -> p k t", k=KC), mix, mix_all)
    f_load_mod(em, mod, l, modsel)
    em.dma("sp", n2g[:], n2g_l[:], n2g, n2g_l)
    wp = em.pool_("wout_sb", [128, KC, 512], BF16, 2)
    for cg in range(4):
        w = wp()
        em.dma("pool", w[:], wout_l[:, 512 * cg:512 * cg + 512].rearrange("(k p) f -> p k f", p=128), w, wout_l)
        for dc in range(4):
            d = 4 * cg + dc
            for (s, n, is_c) in TOK_TILES:
                pb = psr.next()
                mm_acc(em, pb, pb[:, 0:n], [(w[:, k, 128 * dc:128 * dc + 128], mix[:, k, s:s + n]) for k in range(KC)], [w, mix])
                gcol = mod[:, 1 if is_c else 0, 32 + d:32 + d + 1]
                em.op("dve", lambda e, pb=pb, d=d, s=s, n=n, gcol=gcol: e.scalar_tensor_tensor(
                    out=x[:, d, s:s + n], in0=pb[:, 0:n], scalar=gcol, in1=x[:, d, s:s + n], op0=ALU.mult, op1=ALU.add),
                    reads=[pb, mod, x], writes=[x])
    em.dma("sp", xres[:], x[:], xres, x)
    load_par(em, par, mod, n2g, 3, 4)
    if router_l is None:
        norm_mod(em, cst, psr, x, h2, par, TOK_TILES, norm_pools(em))
    else:
        rout = em.sb("routsb", [128, KC, NE], F32)
        em.dma("sp", rout[:], router_l[:], rout, router_l)
        gates = em.sb("gatessb", [128, NE], F32)
        gT = em.sb("gTsb", [NE, NT128 * 128], F32)
        small = em.pool_("rt_small", [128, 8], F32, 12)
        lgp = em.pool_("rt_lg", [128, NE], F32, 3)

        def router_cb(ti, s, n, hf):
            for j in range(0, n, 128):
                m = min(128, n - j)
                t128 = (s + j) // 128
                pb = psr.next()
                mm_acc(em, pb, pb[0:m, 0:NE], [(hf[:, k, j:j + m], rout[:, k, :]) for k in range(KC)], [hf, rout])
                lg = lgp()
                em.op("act", lambda e: e.activation(out=lg[0:m, :], in_=pb[0:m, 0:NE], func=AF.Copy), reads=[pb], writes=[lg])
                m1 = small(); eq = small(); lg2 = small(); m2 = small(); sel = small(); nm1 = small(); ex = small(); den = small()
                em.op("dve", lambda e: e.reduce_max(out=m1[0:m, 0:1], in_=lg[0:m, :], axis=AX.X), reads=[lg], writes=[m1])
                em.op("dve", lambda e: e.tensor_scalar(out=eq[0:m, :], in0=lg[0:m, :], scalar1=m1[0:m, 0:1], scalar2=None, op0=ALU.is_equal),
                      reads=[lg, m1], writes=[eq])
                em.op("dve", lambda e: e.scalar_tensor_tensor(out=lg2[0:m, :], in0=eq[0:m, :], scalar=-1e30, in1=lg[0:m, :], op0=ALU.mult, op1=ALU.add),
                      reads=[eq, lg], writes=[lg2])
                em.op("dve", lambda e: e.reduce_max(out=m2[0:m, 0:1], in_=lg2[0:m, :], axis=AX.X), reads=[lg2], writes=[m2])
                em.op("dve", lambda e: e.tensor_scalar(out=sel[0:m, :], in0=lg[0:m, :], scalar1=m2[0:m, 0:1], scalar2=None, op0=ALU.is_ge),
                      reads=[lg, m2], writes=[sel])
                em.op("dve", lambda e: e.tensor_scalar(out=nm1[0:m, 0:1], in0=m1[0:m, 0:1], scalar1=-1.0, scalar2=None, op0=ALU.mult),
                      reads=[m1], writes=[nm1])
                em.op("act", lambda e: e.activation(out=ex[0:m, :], in_=lg[0:m, :], func=AF.Exp, bias=nm1[0:m, 0:1], scale=1.0),
                      reads=[lg, nm1], writes=[ex])
                em.op("dve", lambda e: e.tensor_tensor(out=ex[0:m, :], in0=ex[0:m, :], in1=sel[0:m, :], op=ALU.mult), reads=[ex, sel], writes=[ex])
                em.op("dve", lambda e: e.reduce_sum(out=den[0:m, 0:1], in_=ex[0:m, :], axis=AX.X), reads=[ex], writes=[den])
                em.op("dve", lambda e: e.reciprocal(out=den[0:m, 0:1], in_=den[0:m, 0:1]), reads=[den], writes=[den])
                em.op("dve", lambda e: e.tensor_scalar(out=gates[0:m, :], in0=ex[0:m, :], scalar1=den[0:m, 0:1], scalar2=None, op0=ALU.mult),
                      reads=[ex, den], writes=[gates])
                pt = psr.next()
                em.op("pe", lambda e: e.matmul(pt[0:NE, 0:m], lhsT=gates[0:m, :], rhs=cst.ident_f[0:m, 0:m], start=True, stop=True),
                      reads=[gates, cst.ident_f], writes=[pt])
                em.op("act", lambda e: e.activation(out=gT[:, 128 * t128:128 * t128 + m], in_=pt[0:NE, 0:m], func=AF.Copy), reads=[pt], writes=[gT])

        c_tiles = [(0, 256, False), (256, 256, False), (512, 256, False), (768, 256, False), (1024, 64, True)]
        norm_mod(em, cst, psr, x, h2, par, c_tiles, norm_pools(em, with_hf=True), hf_cb=router_cb)
        em.dma("sp", gT_part[:], gT[:, 0:TOK], gT_part, gT)
    if isinstance(h2_part, AGC):
        h2_part.store(h2)
    else:
        em.dma("sp", h2_part[:], h2[:], h2_part, h2)


D_TILES = [(128 * i, 128) for i in range(8)] + [(1024, 64)]


def f_D(em, psr, subs, h2g, gT_all, gsel, yparts, ysums, jr):
    GC = 4
    hp = em.pool_("h_sb", [128, KC, 512], BF16, 2)
    gp = em.pool_("g_sb", [128, 512], F32, 2)
    wgp = em.pool_("wg_sb", [128, KC, 128 * GC], BF16, 2)
    wup = em.pool_("wu_sb", [128, KC, 128 * GC], BF16, 2)
    wdp = em.pool_("wd_sb", [128, GC, D], BF16, 2)
    actp = em.pool_("act_sb", [128, GC, 512], BF16, 2)
    sgp = em.pool_("sg_sb", [128, 512], F32, 3)
    yacc = em.sb("yacc", [128, KC, 512], F32)
    if subs[0][4]:
        for r in range(4):
            em.dma("sp", gsel.t[:, 0, TOK * r:TOK * r + TOK], gT_all.t.rearrange("(r e) t -> r e t", e=NE)[r, bass.ts(jr, 2), :], gsel, gT_all)
    pending = None
    for ti, (a, ln) in enumerate(D_TILES):
        n = 4 * ln
        h = hp()
        for r in range(4):
            h2g.load(h, lambda k0, nk, h=h, r=r: h[:, k0:k0 + nk, r * ln:(r + 1) * ln], r, a, ln)
        first = True
        ngrp = 0
        for si, (wg, wu, wd, F, gated) in enumerate(subs):
            if gated:
                g = gp()
                for r in range(4):
                    em.dma("sp", g[:, r * ln:(r + 1) * ln], gsel.t[si, 0:1, TOK * r + a:TOK * r + a + ln].partition_broadcast(128), g, gsel)
            for gi in range(F // (128 * GC)):
                wgs = wgp(); wus = wup(); wds = wdp()
                c0 = 128 * GC * gi
                em.dma("pool", wgs[:], wg[:, c0:c0 + 128 * GC].rearrange("(k p) f -> p k f", p=128), wgs, wg)
                em.dma("pool", wus[:], wu[:, c0:c0 + 128 * GC].rearrange("(k p) f -> p k f", p=128), wus, wu)
                em.dma("pool", wds[:], wd[c0:c0 + 128 * GC, :].rearrange("(c p) f -> p c f", p=128), wds, wd)
                ngrp += 1
                if ngrp == 3 and pending is not None:
                    pending()
                    pending = None
                act = actp()
                for c in range(GC):
                    pg = psr.next(); pu = psr.next()
                    mm_acc(em, pg, pg[:, 0:n], [(wgs[:, k, 128 * c:128 * c + 128], h[:, k, 0:n]) for k in range(KC)], [wgs, h])
                    mm_acc(em, pu, pu[:, 0:n], [(wus[:, k, 128 * c:128 * c + 128], h[:, k, 0:n]) for k in range(KC)], [wus, h])
                    sg = sgp()
                    em.op("act", lambda e, pg=pg, sg=sg: e.activation(out=sg[:, 0:n], in_=pg[:, 0:n], func=AF.Silu), reads=[pg], writes=[sg])
                    if gated:
                        em.op("dve", lambda e, sg=sg, g=g: e.tensor_tensor(out=sg[:, 0:n], in0=sg[:, 0:n], in1=g[:, 0:n], op=ALU.mult),
                              reads=[sg, g], writes=[sg])
                    em.op("dve", lambda e, pu=pu, sg=sg, c=c, act=act: e.tensor_tensor(out=act[:, c, 0:n], in0=pu[:, 0:n], in1=sg[:, 0:n], op=ALU.mult),
                          reads=[pu, sg], writes=[act])
                for d in range(KC):
                    pd = psr.next()
                    mm_acc(em, pd, pd[:, 0:n], [(wds[:, c, 128 * d:128 * d + 128], act[:, c, 0:n]) for c in range(GC)], [wds, act])
                    if first:
                        em.op("act", lambda e, pd=pd, d=d: e.activation(out=yacc[:, d, 0:n], in_=pd[:, 0:n], func=AF.Copy), reads=[pd], writes=[yacc])
                    else:
                        em.op("dve", lambda e, pd=pd, d=d: e.tensor_tensor(out=yacc[:, d, 0:n], in0=pd[:, 0:n], in1=yacc[:, d, 0:n], op=ALU.add),
                              reads=[pd, yacc], writes=[yacc])
                first = False
        yp = yparts[ti]
        ypv = yp.t.rearrange("(r p) (k t) -> p r k t", p=128, k=KC)
        for r in range(4):
            em.dma("sp", ypv[:, r, :, :], yacc[:, :, r * ln:(r + 1) * ln], yp, yacc)
        pending = (lambda yp=yp, ys=ysums[ti]: em.cc("ReduceScatter", ALU.add, G4, ys[:], yp[:], ys, yp))
    pending()


def f_Dloc(em, psr, l, wg, wu, wd, h2loc, xres, modsel):
    GC = 4
    mod = em.sb("modsb", [128, 2, 96], F32)
    f_load_mod(em, mod, l, modsel)
    hp = em.pool_("h_sb", [128, KC, 512], BF16, 1)
    xp = em.pool_("xt_sb", [128, KC, 512], F32, 2)
    wgp = em.pool_("wg_sb", [128, KC, 128 * GC], BF16, 2)
    wup = em.pool_("wu_sb", [128, KC, 128 * GC], BF16, 2)
    wdp = em.pool_("wd_sb", [128, GC, D], BF16, 2)
    actp = em.pool_("act_sb", [128, GC, 512], BF16, 2)
    sgp = em.pool_("sg_sb", [128, 512], F32, 3)
    for (s0, n) in ((0, 384), (384, 384), (768, 320)):
        h = hp()
        em.dma("sp", h[:, :, 0:n], h2loc[:, :, s0:s0 + n], h, h2loc)
        xt = xp()
        em.dma("sp", xt[:, :, 0:n], xres[:, :, s0:s0 + n], xt, xres)
        rngs = [(0, min(n, 1024 - s0), 0)] if s0 < 1024 else []
        if s0 + n > 1024:
            rngs.append((max(0, 1024 - s0), n, 1))
        for gi in range(DFF // (128 * GC)):
            wgs = wgp(); wus = wup(); wds = wdp()
            c0 = 128 * GC * gi
            em.dma("pool", wgs[:], wg[:, c0:c0 + 128 * GC].rearrange("(k p) f -> p k f", p=128), wgs, wg)
            em.dma("pool", wus[:], wu[:, c0:c0 + 128 * GC].rearrange("(k p) f -> p k f", p=128), wus, wu)
            em.dma("pool", wds[:], wd[c0:c0 + 128 * GC, :].rearrange("(c p) f -> p c f", p=128), wds, wd)
            act = actp()
            for c in range(GC):
                pg = psr.next(); pu = psr.next()
                mm_acc(em, pg, pg[:, 0:n], [(wgs[:, k, 128 * c:128 * c + 128], h[:, k, 0:n]) for k in range(KC)], [wgs, h])
                mm_acc(em, pu, pu[:, 0:n], [(wus[:, k, 128 * c:128 * c + 128], h[:, k, 0:n]) for k in range(KC)], [wus, h])
                sg = sgp()
                em.op("act", lambda e, pg=pg, sg=sg: e.activation(out=sg[:, 0:n], in_=pg[:, 0:n], func=AF.Silu), reads=[pg], writes=[sg])
                em.op("dve", lambda e, pu=pu, sg=sg, c=c, act=act: e.tensor_tensor(out=act[:, c, 0:n], in0=pu[:, 0:n], in1=sg[:, 0:n], op=ALU.mult),
                      reads=[pu, sg], writes=[act])
            for d in range(KC):
                pd = psr.next()
                mm_acc(em, pd, pd[:, 0:n], [(wds[:, c, 128 * d:128 * d + 128], act[:, c, 0:n]) for c in range(GC)], [wds, act])
                for (lo, hi, mc) in rngs:
                    em.op("dve", lambda e, pd=pd, d=d, xt=xt, lo=lo, hi=hi, mc=mc: e.scalar_tensor_tensor(
                        out=xt[:, d, lo:hi], in0=pd[:, lo:hi], scalar=mod[:, mc, 80 + d:80 + d + 1], in1=xt[:, d, lo:hi],
                        op0=ALU.mult, op1=ALU.add), reads=[pd, mod, xt], writes=[xt])
        em.dma("sp", xres[:, :, s0:s0 + n], xt[:, :, 0:n], xres, xt)


def f_E(em, cst, psr, l, xres, ysum, modsel, g1_next, xdst, hx_part, bidx, last):
    x = em.sb("x", [128, KC, TOK], F32)
    mod = em.sb("modsb", [128, 2, 96], F32)
    em.dma("sp", x[:], xres[:], x, xres)
    f_load_mod(em, mod, l, modsel)
    pp = em.pool_("yp_sb", [128, 4, TOK], F32, 2) if ysum is not None else None
    for kg in (range(4) if ysum is not None else ()):
        p = pp()
        for ti, (a, ln) in enumerate(D_TILES):
            em.dma("sp", p[:, :, a:a + ln], ysum[ti].t.rearrange("p (k t) -> p k t", k=KC)[:, 4 * kg:4 * kg + 4, :], p, ysum[ti])
        for kk in range(4):
            k = 4 * kg + kk
            for (s, n, c) in ((0, 1024, 0), (1024, 64, 1)):
                em.op("dve", lambda e, p=p, kk=kk, k=k, s=s, n=n, c=c: e.scalar_tensor_tensor(
                    out=x[:, k, s:s + n], in0=p[:, kk, s:s + n], scalar=mod[:, c, 80 + k:80 + k + 1], in1=x[:, k, s:s + n],
                    op0=ALU.mult, op1=ALU.add), reads=[p, mod, x], writes=[x])
    em.dma("sp", xdst[:], x[:], xdst, x)
    if not last:
        h = em.sb("h", [128, KC, TOK], BF16)
        modn = em.sb("modnsb", [128, 2, 96], F32)
        g1 = em.sb("g1sb", [128, KC], F32)
        par = em.sb("par", [128, 4, KC], F32)
        f_load_mod(em, modn, l + 1, modsel)
        em.dma("sp", g1[:], g1_next[:], g1, g1_next)
        load_par(em, par, modn, g1, 0, 1)
        norm_mod(em, cst, psr, x, h, par, TOK_TILES, norm_pools(em))
        hx_part.store(h)


def build_fused(nl=DEPTH):
    nc = bass.Bass("TRN2", target_bir_lowering=False)
    em = Em(nc)
    pid = nc.sync.partition_id()
    jr = pid % 4
    bidx = pid // 4
    EI = "ExternalInput"
    xT = em.dram("xT", [128, KC, TOK], F32, kind=EI)
    condT = em.dram("condT", [128, KC, 3], F32, kind=EI)
    adaw = em.dram("adaw", [nl, D, 128 * NCH], F32, kind=EI)
    adab = em.dram("adab", [128, DEPTH, NCH], F32, kind=EI)
    g1d = em.dram("g1", [DEPTH, 128, KC], F32, kind=EI)
    n2gd = em.dram("n2g", [DEPTH, 128, KC], F32, kind=EI)
    wna = em.dram("wna", [nl, D, W_NA], F32, kind=EI)
    wm = em.dram("wm", [nl, D, W_M], F32, kind=EI)
    nab = em.dram("nab", [DEPTH, 2, 64, 15, 64], F32, kind=EI)
    nqg = em.dram("nqg", [DEPTH, 128, 1], F32, kind=EI)
    nkg = em.dram("nkg", [DEPTH, 128, 1], F32, kind=EI)
    mng = em.dram("mng", [DEPTH, 128, 2], F32, kind=EI)
    gb = em.dram("gb", [DEPTH, 128, 4], F32, kind=EI)
    cosT = em.dram("cosT", [128, L], F32, kind=EI)
    sinT = em.dram("sinT", [128, L], F32, kind=EI)
    cmask = em.dram("cmask", [64, 64], F32, kind=EI)
    wout = em.dram("wout", [nl, D, D], F32, kind=EI)
    nd_, nm_ = (nl + 1) // 2, nl // 2
    dwg = em.dram("dwg", [nd_, D, DFF], F32, kind=EI)
    dwu = em.dram("dwu", [nd_, D, DFF], F32, kind=EI)
    dwd = em.dram("dwd", [nd_, DFF, D], F32, kind=EI)
    if nm_ > 0:
        router = em.dram("router", [nm_, 128, KC, NE], F32, kind=EI)
        mwg = em.dram("mwg", [nm_, 2, D, DFF], F32, kind=EI)
        mwu = em.dram("mwu", [nm_, 2, D, DFF], F32, kind=EI)
        mwd = em.dram("mwd", [nm_, 2, DFF, D], F32, kind=EI)
    xo = em.dram("xo", [128, KC, TOK], F32, kind="ExternalOutput")
    NM = 3 * DEPTH * NCH
    mod_part = em.dram("mod_part", [128, NM], F32)
    mod_all = em.dram("mod_all", [512, NM], F32)
    modsel = em.dram("modsel", [128, 2, DEPTH, 96], F32)
    hxg = AGC(em, "hx")
    mix_part = em.dram("mix_part", [128, 4, 4, TOK], BF16)
    mix_rs = em.dram("mix_rs", [512, KC * TOK], BF16)
    mix_own = em.dram("mix_own", [128, KC * TOK], BF16)
    h2g = AGC(em, "h2")
    h2loc = em.dram("h2loc", [128, KC, TOK], BF16)
    gT_part = em.dram("gT_part", [NE, TOK], F32)
    gT_all = em.dram("gT_all", [4 * NE, TOK], F32)
    gsel = em.dram("gsel", [2, 1, 4 * TOK], F32)
    yparts = [em.dram(f"ypart{i}", [512, KC * ln], F32) for i, (a_, ln) in enumerate(D_TILES)]
    ysums = [em.dram(f"ysum{i}", [128, KC * ln], F32) for i, (a_, ln) in enumerate(D_TILES)]
    xres = em.dram("xres", [128, KC, TOK], F32)
    cst = Consts(em)
    psr = PsumRing(em, 7)
    ptr = em.ps("ps_tr", [128, 128], BF16)

    em.scope_begin()
    cf = em.sb("cf", [128, KC, 3], F32)
    cb = em.sb("cb", [128, KC, 3], BF16)
    bt = em.sb("bt", [128, DEPTH, NCH], F32)
    res = em.sb("res", [128, 3, DEPTH, NCH], F32)
    em.op("pool", lambda e: e.memset(res[:], 0.0), writes=[res])
    wp = em.pool_("adaw_sb", [128, KC, 1536], BF16, 2)
    em.dma("sp", cf[:], condT[:], cf, condT)
    em.dma("sp", bt[:], adab[:], bt, adab)
    em.op("act", lambda e: e.activation(out=cb[:], in_=cf[:], func=AF.Silu), reads=[cf], writes=[cb])
    for l in range(nl):
        for hh in range(2):
            w = wp()
            em.dma("pool", w[:], adaw[l, :, 1536 * hh:1536 * hh + 1536].rearrange("(k p) f -> p k f", p=128), w, adaw)
            pb = psr.next()
            for jj in range(12):
                mm_acc(em, pb, pb[:, 3 * jj:3 * jj + 3], [(w[:, k, 128 * jj:128 * jj + 128], cb[:, k, :]) for k in range(KC)], [w, cb])
            em.op("dve", lambda e, l=l, hh=hh, pb=pb: e.tensor_tensor(
                out=res[:, :, l, 12 * hh:12 * hh + 12], in0=pb[:, 0:36].rearrange("p (j c) -> p c j", c=3),
                in1=bt[:, l, 12 * hh:12 * hh + 12].unsqueeze(1).to_broadcast([128, 3, 12]), op=ALU.add), reads=[pb, bt], writes=[res])
    em.dma("sp", mod_part[:], res[:].rearrange("p c l j -> p (c l j)"), mod_part, res)
    em.scope_end()
    em.cc("AllGather", ALU.bypass, G4, mod_all[:], mod_part[:], mod_all, mod_part)
    f_select_mod(em, modsel, mod_all, bidx)

    em.scope_begin()
    x = em.sb("x", [128, KC, TOK], F32)
    h = em.sb("h", [128, KC, TOK], BF16)
    mod = em.sb("modsb", [128, 2, 96], F32)
    g1 = em.sb("g1sb", [128, KC], F32)
    par = em.sb("par", [128, 4, KC], F32)
    em.dma("sp", x[:], xT[:], x, xT)
    f_load_mod(em, mod, 0, modsel)
    em.dma("sp", g1[:], g1d[0], g1, g1d)
    load_par(em, par, mod, g1, 0, 1)
    norm_mod(em, cst, psr, x, h, par, TOK_TILES, norm_pools(em))
    hxg.store(h)
    zt = em.sb("zt", [128, 8704], BF16)
    em.op("pool", lambda e: e.memset(zt[:], 0.0), writes=[zt])
    for r in range(4):
        for hh in range(2):
            em.dma("sp", mix_rs[128 * r:128 * r + 128, 8704 * hh:8704 * hh + 8704], zt[:], mix_rs, zt)
    em.scope_end()
    hxg.gather()

    def load_hx(hx, s, n):
        if s < L:
            hxg.load(hx, lambda k0, nk, hx=hx: hx[:, k0:k0 + nk, :], s // 1024, s % 1024, n)
        else:
            for r in range(4):
                hxg.load(hx, lambda k0, nk, hx=hx, r=r: hx[:, k0:k0 + nk, 64 * r:64 * r + 64], r, 1024, 64)

    for l in range(nl):
        moe = (l % 2 == 1)
        idx = l // 2
        last = (l == nl - 1)
        em.scope_begin()
        io = {"wna": view(wna, wna.t[l]), "wm": view(wm, wm.t[l]), "cosT": cosT, "sinT": sinT, "nab": view(nab, nab.t[l]), "cmask": cmask,
              "nqg": view(nqg, nqg.t[l]), "nkg": view(nkg, nkg.t[l]), "mng": view(mng, mng.t[l]),
              "gb": view(gb, gb.t[l]), "mixT": mix_part, "load_hx": load_hx}
        emit_B(em, cst, psr, ptr, io)
        em.scope_end()
        em.dma("sp", mix_rs.t.rearrange("(d p) (g x) -> p d g x", p=128, g=4)[:, :, bass.ts(jr, 1), :].rearrange("p d g x -> p d (g x)"),
               mix_part[:].rearrange("p d c t -> p d (c t)"), mix_rs, mix_part)
        em.cc("ReduceScatter", ALU.add, G4, mix_own[:], mix_rs[:], mix_own, mix_rs)
        em.scope_begin()
        f_C(em, cst, psr, l, xT if l == 0 else xres, mix_own, view(wout, wout.t[l]), modsel, view(n2gd, n2gd.t[l]),
            view(router, router.t[idx]) if moe else None, xres, h2g if moe else h2loc, gT_part, jr, bidx)
        em.scope_end()
        if moe:
            h2g.gather()
            em.cc("AllGather", ALU.bypass, G4, gT_all[:], gT_part[:], gT_all, gT_part)
            em.scope_begin()
            subs = [(view(mwg, mwg.t[idx, e]), view(mwu, mwu.t[idx, e]), view(mwd, mwd.t[idx, e]), DFF, True) for e in range(2)]
            f_D(em, psr, subs, h2g, gT_all, gsel, yparts, ysums, jr)
            em.scope_end()
        else:
            em.scope_begin()
            f_Dloc(em, psr, l, view(dwg, dwg.t[idx]), view(dwu, dwu.t[idx]), view(dwd, dwd.t[idx]), h2loc, xres, modsel)
            em.scope_end()
        em.scope_begin()
        f_E(em, cst, psr, l, xres, ysums if moe else None, modsel, None if last else view(g1d, g1d.t[l + 1]), xo if last else xres, hxg, bidx, last)
        em.scope_end()
        if not last:
            hxg.gather()
    em.finish()
    return nc


_WOUT_PERM = np.concatenate([np.arange(o, o + 128) for g in range(4) for o in (256 * g, 256 * g + 128, 1024 + 256 * g, 1024 + 256 * g + 128)])


def fused_inputs(i, x_sh, condT, ada_w, ada_b, norm1_g, norm2_g, w_in, gate_b, na_q_g, na_k_g, na_rpb, m_norm_g, w_out,
                 ffn_w_gate, ffn_w_up, ffn_w_down, moe_router, moe_w_gate, moe_w_up, moe_w_down, consts, shared, nl=DEPTH):
    f32 = np.float32
    j = i % 4
    cosT, sinT, cmask = consts
    cols = np.arange(128 * NCH * j, 128 * NCH * (j + 1))
    na_cols = np.concatenate([np.arange(o + 256 * j, o + 256 * j + 256) for o in (0, 1024, 2048)])
    mq = 3072 + 128 * j + np.arange(128)
    mk = 3584 + 128 * j + np.arange(128)
    mv = 4096 + 256 * j + np.arange(256)
    mo = 5120 + 256 * j + np.arange(256)
    mg = 6144 + np.array([j, 4 + j, 8 + j, 12 + j])
    m_cols = np.concatenate([mq, mk, mq[_SWAP], mk[_SWAP], mv, mo, mg])
    dwg, dwu, dwd = ffn_w_gate, ffn_w_up, ffn_w_down
    nd_, nm_ = (nl + 1) // 2, nl // 2
    d = {
        "xT": x_sh[i], "condT": condT,
        "adaw": np.ascontiguousarray(ada_w[:, :, cols]),
        "adab": np.ascontiguousarray(ada_b[:, cols].reshape(DEPTH, NCH, 128).transpose(2, 0, 1)),
        "g1": shared["g1"], "n2g": shared["n2g"],
        "wna": np.ascontiguousarray(w_in[:, :, na_cols]), "wm": np.ascontiguousarray(w_in[:, :, m_cols]),
        "nab": np.stack([np.stack([na_bias_table(na_rpb[l, 2 * j + h]) for h in range(2)]) for l in range(DEPTH)]),
        "nqg": shared["nqg"], "nkg": shared["nkg"],
        "mng": np.ascontiguousarray(np.stack([m_norm_g[l, 256 * j:256 * j + 256].reshape(2, 128).T for l in range(DEPTH)], axis=0)),
        "gb": np.ascontiguousarray(np.broadcast_to(gate_b[:, [j, 4 + j, 8 + j, 12 + j]][:, None, :], (DEPTH, 128, 4))),
        "cosT": cosT, "sinT": sinT, "cmask": cmask, "wout": shared["wout"], "router": shared["router"],
        "dwg": dwg, "dwu": dwu, "dwd": dwd,
        "mwg": np.ascontiguousarray(moe_w_gate[:, 2 * j:2 * j + 2]), "mwu": np.ascontiguousarray(moe_w_up[:, 2 * j:2 * j + 2]),
        "mwd": np.ascontiguousarray(moe_w_down[:, 2 * j:2 * j + 2]),
    }
    for k in ("adaw", "wna", "wm", "wout"):
        d[k] = np.ascontiguousarray(d[k][:nl])
    for k in ("dwg", "dwu", "dwd"):
        d[k] = np.ascontiguousarray(d[k][:nd_])
    for k in ("router", "mwg", "mwu", "mwd"):
        if nm_ == 0:
            del d[k]
        else:
            d[k] = np.ascontiguousarray(d[k][:nm_])
    return d


def kernel(x, c, ctx, c_ctx, ada_w, ada_b, norm1_g, norm2_g, w_in, gate_b, na_q_g, na_k_g, na_rpb, m_norm_g, w_out,
           ffn_w_gate, ffn_w_up, ffn_w_down, moe_router, moe_w_gate, moe_w_up, moe_w_down, _nl=DEPTH):
    f32 = np.float32
    A = lambda v: np.asarray(v, f32)
    x, c, ctx, c_ctx = A(x), A(c), A(ctx), A(c_ctx)
    ada_w, ada_b, norm1_g, norm2_g, w_in, gate_b = A(ada_w), A(ada_b), A(norm1_g), A(norm2_g), A(w_in), A(gate_b)
    na_q_g, na_k_g, na_rpb, m_norm_g, w_out = A(na_q_g), A(na_k_g), A(na_rpb), A(m_norm_g), A(w_out)
    ffn_w_gate, ffn_w_up, ffn_w_down, moe_router = A(ffn_w_gate), A(ffn_w_up), A(ffn_w_down), A(moe_router)
    moe_w_gate, moe_w_up, moe_w_down = A(moe_w_gate), A(moe_w_up), A(moe_w_down)
    x_sh = shard_tokens(x, ctx)
    condT = fm(np.stack([c[0], c[1], c_ctx], axis=0))
    consts = (*rope_tables(), col_mask())
    shared = {
        "g1": np.ascontiguousarray(fm(norm1_g).transpose(2, 0, 1)), "n2g": np.ascontiguousarray(fm(norm2_g).transpose(2, 0, 1)),
        "nqg": np.ascontiguousarray(na_q_g[:, :, None]), "nkg": np.ascontiguousarray(na_k_g[:, :, None]),
        "wout": np.ascontiguousarray(w_out[:, _WOUT_PERM, :]), "router": np.ascontiguousarray(np.stack([fm(moe_router[k].T) for k in range(2)])),
    }
    in_maps = [fused_inputs(i, x_sh, condT, ada_w, ada_b, norm1_g, norm2_g, w_in, gate_b, na_q_g, na_k_g, na_rpb, m_norm_g, w_out,
                            ffn_w_gate, ffn_w_up, ffn_w_down, moe_router, moe_w_gate, moe_w_up, moe_w_down, consts, shared, _nl) for i in range(8)]
    res = _run(_prog("fused", build_fused, _nl), in_maps)
    out = np.empty((B, L, D), f32)
    for i in range(8):
        b, j = i // 4, i % 4
        out[b, 1024 * j:1024 * j + 1024] = res[i]["xo"][:, :, :1024].transpose(2, 1, 0).reshape(1024, D)
    return out
```
